# Optimizing a Trainium2 kernel written in Bass

```python
import math
import jax, jax.numpy as jnp
from jax import lax
import numpy as np

D_MODEL = 1024
BATCH = 2
SEQ = 8192
DEPTH = 1

ATTN_HEADS = 8
ATTN_KV_HEADS = 2
ATTN_HEAD_DIM = 128
IDX_HEADS = 4
IDX_HEAD_DIM = 64
TOPK_MAX = 256
Q_BLOCK = 128
ROPE_THETA = 500000.0
ROPE_FRACTION = 4
SSD_EXPAND = 2
SSD_D_INNER = SSD_EXPAND * D_MODEL
SSD_HEAD_DIM = 64
SSD_HEADS = SSD_D_INNER // SSD_HEAD_DIM
SSD_GROUPS = 4
SSD_STATE = 128
SSD_CONV = 4
SSD_CHUNK = 128
SSD_CONV_CH = SSD_D_INNER + 2 * SSD_GROUPS * SSD_STATE
MEM_LEN = 256
MEM_HEADS = 4
MEM_HEAD_DIM = D_MODEL // MEM_HEADS
PEER_HEADS = 8
PEER_N_KEYS = 128
PEER_N_EXPERTS = PEER_N_KEYS * PEER_N_KEYS
PEER_KEY_DIM = 128
PEER_TOPK = 16
PEER_TOKEN_BLOCK = 128
N_BRANCHES = 2
EPS = 1e-6

IN_SPLIT_SIZES = (
    ATTN_HEADS * ATTN_HEAD_DIM,
    ATTN_KV_HEADS * ATTN_HEAD_DIM,
    ATTN_KV_HEADS * ATTN_HEAD_DIM,
    IDX_HEADS * IDX_HEAD_DIM,
    IDX_HEAD_DIM,
    IDX_HEADS,
    SSD_D_INNER,
    SSD_CONV_CH,
    SSD_HEADS,
    N_BRANCHES * D_MODEL,
)
IN_WIDTH = sum(IN_SPLIT_SIZES)

kernel_name = "hybrid_dsa_ssd_peer_block"


def _in_offsets():
    offs, acc = [], 0
    for s in IN_SPLIT_SIZES[:-1]:
        acc += s
        offs.append(acc)
    return offs


def rmsnorm(x, g):
    xf = x.astype(jnp.float32)
    y = xf * lax.rsqrt(jnp.mean(xf * xf, axis=-1, keepdims=True) + EPS)
    return (y * g.astype(jnp.float32)).astype(x.dtype)


def partial_rope(x, pos):
    d = x.shape[-1]
    rot = d // ROPE_FRACTION
    half = rot // 2
    inv = ROPE_THETA ** (-2.0 * jnp.arange(half, dtype=jnp.float32) / rot)
    ang = pos.astype(jnp.float32)[..., None] * inv
    cos = jnp.cos(ang)[:, :, None, :]
    sin = jnp.sin(ang)[:, :, None, :]
    xf = x.astype(jnp.float32)
    x1 = xf[..., :half]
    x2 = xf[..., half:rot]
    out = jnp.concatenate([x1 * cos - x2 * sin, x2 * cos + x1 * sin, xf[..., rot:]], axis=-1)
    return out.astype(x.dtype)


def dsa_attention(q, k, v, iq, ik, iw):
    b, t, h, dh = q.shape
    kv = k.shape[2]
    grp = h // kv
    n_sel = min(TOPK_MAX, t // 4)
    nblk = t // Q_BLOCK
    idx_scale = IDX_HEAD_DIM ** -0.5
    w_scale = IDX_HEADS ** -0.5
    att_scale = dh ** -0.5
    key_idx = jnp.arange(t)

    def to_blocks(a):
        return a.reshape(b, nblk, Q_BLOCK, *a.shape[2:]).swapaxes(0, 1)

    def block(args):
        qb, iqb, iwb, start = args
        qpos = start + jnp.arange(Q_BLOCK)
        s = jax.nn.relu(jnp.einsum('bqhd,bsd->bqhs', iqb, ik).astype(jnp.float32) * idx_scale)
        score = jnp.einsum('bqhs,bqh->bqs', s, iwb.astype(jnp.float32) * w_scale)
        causal = key_idx[None, :] <= qpos[:, None]
        score = jnp.where(causal[None], score, -jnp.inf)
        _, sel = lax.top_k(score, n_sel)
        valid = sel <= qpos[None, :, None]
        ks = jax.vmap(lambda a, i: a[i])(k, sel)
        vs = jax.vmap(lambda a, i: a[i])(v, sel)
        qg = qb.reshape(b, Q_BLOCK, kv, grp, dh)
        logits = jnp.einsum('bqcgd,bqncd->bqcgn', qg, ks).astype(jnp.float32) * att_scale
        logits = jnp.where(valid[:, :, None, None, :], logits, -jnp.inf)
        p = jax.nn.softmax(logits, axis=-1).astype(vs.dtype)
        o = jnp.einsum('bqcgn,bqncd->bqcgd', p, vs)
        return o.reshape(b, Q_BLOCK, h * dh)

    starts = jnp.arange(nblk) * Q_BLOCK
    out = lax.map(block, (to_blocks(q), to_blocks(iq), to_blocks(iw), starts))
    return out.swapaxes(0, 1).reshape(b, t, h * dh)


def ssd_chunked(xs, dt, a, bm, cm):
    b, t, h, p = xs.shape
    g, n = bm.shape[2], bm.shape[3]
    j = h // g
    L = SSD_CHUNK
    nc = t // L
    xdt = (xs * dt[..., None]).reshape(b, nc, L, g, j, p)
    adt = (dt * a).reshape(b, nc, L, g, j)
    bc = bm.reshape(b, nc, L, g, n)
    cc = cm.reshape(b, nc, L, g, n)
    acs = jnp.cumsum(adt, axis=2)
    tri = jnp.tril(jnp.ones((L, L), dtype=bool))
    seg = acs[:, :, :, None] - acs[:, :, None, :]
    decay = jnp.exp(jnp.where(tri[None, None, :, :, None, None], seg, -jnp.inf))
    cb = jnp.einsum('bclgn,bcsgn->bclsg', cc, bc)
    y_diag = jnp.einsum('bclsgj,bcsgjp->bclgjp', cb[..., None] * decay, xdt)
    decay_to_end = jnp.exp(acs[:, :, -1:] - acs)
    states = jnp.einsum('bcsgn,bcsgj,bcsgjp->bcgjpn', bc, decay_to_end, xdt)
    chunk_decay = jnp.exp(acs[:, :, -1])

    def step(prev, inp):
        st, dec = inp
        return prev * dec[..., None, None] + st, prev

    init = jnp.zeros((b, g, j, p, n), xs.dtype)
    _, prev_states = lax.scan(step, init, (states.swapaxes(0, 1), chunk_decay.swapaxes(0, 1)))
    prev_states = prev_states.swapaxes(0, 1)
    y_off = jnp.einsum('bclgn,bcgjpn,bclgj->bclgjp', cc, prev_states, jnp.exp(acs))
    return (y_diag + y_off).reshape(b, t, h, p)


def ssd_branch(z, xbc, dt_raw, conv_w, conv_b, dt_bias, a_log, d_skip, norm_g):
    b, t, _ = xbc.shape
    xbc = lax.conv_general_dilated(
        xbc, conv_w, window_strides=(1,), padding=[(SSD_CONV - 1, 0)],
        dimension_numbers=('NWC', 'WIO', 'NWC'), feature_group_count=SSD_CONV_CH) + conv_b
    xbc = jax.nn.silu(xbc)
    xs, bm, cm = jnp.split(xbc, [SSD_D_INNER, SSD_D_INNER + SSD_GROUPS * SSD_STATE], axis=-1)
    xs = xs.reshape(b, t, SSD_HEADS, SSD_HEAD_DIM).astype(jnp.float32)
    bm = bm.reshape(b, t, SSD_GROUPS, SSD_STATE).astype(jnp.float32)
    cm = cm.reshape(b, t, SSD_GROUPS, SSD_STATE).astype(jnp.float32)
    dt = jax.nn.softplus(dt_raw.astype(jnp.float32) + dt_bias.astype(jnp.float32))
    a = -jnp.exp(a_log.astype(jnp.float32))
    y = ssd_chunked(xs, dt, a, bm, cm) + xs * d_skip.astype(jnp.float32)[:, None]
    y = y.reshape(b, t, SSD_D_INNER) * jax.nn.silu(z.astype(jnp.float32))
    return rmsnorm(y, norm_g).astype(z.dtype)


def memory_cross_attention(a, m, w_q, w_kv, w_o):
    b, t, _ = a.shape
    q = (a @ w_q).reshape(b, t, MEM_HEADS, MEM_HEAD_DIM)
    kk, vv = jnp.split(m @ w_kv, 2, axis=-1)
    kk = kk.reshape(b, m.shape[1], MEM_HEADS, MEM_HEAD_DIM)
    vv = vv.reshape(b, m.shape[1], MEM_HEADS, MEM_HEAD_DIM)
    logits = jnp.einsum('bthd,bmhd->bhtm', q, kk).astype(jnp.float32) * MEM_HEAD_DIM ** -0.5
    p = jax.nn.softmax(logits, axis=-1).astype(vv.dtype)
    o = jnp.einsum('bhtm,bmhd->bthd', p, vv).reshape(b, t, D_MODEL)
    return o @ w_o


def peer_ffn(a, w_q, sub_keys, u_tab, v_tab):
    b, t, d = a.shape
    n = b * t
    af = a.reshape(n, d)
    qry = (af @ w_q).reshape(n, PEER_HEADS, 2, PEER_KEY_DIM)
    s = jnp.einsum('nhpd,hpkd->nhpk', qry, sub_keys).astype(jnp.float32)
    s_top, i_top = lax.top_k(s, PEER_TOPK)
    cand = (s_top[:, :, 0, :, None] + s_top[:, :, 1, None, :]).reshape(n, PEER_HEADS, PEER_TOPK * PEER_TOPK)
    sc, ci = lax.top_k(cand, PEER_TOPK)
    e_a = jnp.take_along_axis(i_top[:, :, 0], ci // PEER_TOPK, axis=-1)
    e_b = jnp.take_along_axis(i_top[:, :, 1], ci % PEER_TOPK, axis=-1)
    experts = e_a * PEER_N_KEYS + e_b
    gw = jax.nn.softmax(sc, axis=-1)
    n_sel = PEER_HEADS * PEER_TOPK
    nb = n // PEER_TOKEN_BLOCK

    def block(args):
        xb, eb, gb = args
        u = u_tab[eb]
        act = jax.nn.gelu(jnp.einsum('td,tkd->tk', xb, u).astype(jnp.float32), approximate=False)
        wts = (gb * act).astype(v_tab.dtype)
        return jnp.einsum('tk,tkd->td', wts, v_tab[eb])

    out = lax.map(block, (af.reshape(nb, PEER_TOKEN_BLOCK, d),
                          experts.reshape(nb, PEER_TOKEN_BLOCK, n_sel),
                          gw.reshape(nb, PEER_TOKEN_BLOCK, n_sel)))
    return out.reshape(b, t, d)


def setup_inputs(seed: int = 0) -> dict:
    key = jax.random.key(seed)
    ks = jax.random.split(key, 26)
    f32 = jnp.float32
    L = DEPTH

    def nrm(k, shape, scale):
        return jax.random.normal(k, shape, f32) * scale

    def gain(k, shape):
        return 1.0 + 0.02 * jax.random.normal(k, shape, f32)

    dt0 = jnp.exp(jax.random.uniform(ks[6], (L, SSD_HEADS), f32, math.log(1e-3), math.log(1e-1)))
    return {
        "x": nrm(ks[0], (BATCH, SEQ, D_MODEL), 1.0),
        "mem": nrm(ks[1], (BATCH, MEM_LEN, D_MODEL), 1.0),
        "positions": jnp.broadcast_to(jnp.arange(SEQ, dtype=jnp.int32), (BATCH, SEQ)),
        "norm_mix_g": gain(ks[2], (L, D_MODEL)),
        "w_in": nrm(ks[3], (L, D_MODEL, IN_WIDTH), D_MODEL ** -0.5),
        "conv_w": nrm(ks[4], (L, SSD_CONV, 1, SSD_CONV_CH), SSD_CONV ** -0.5),
        "conv_b": nrm(ks[5], (L, SSD_CONV_CH), 0.02),
        "dt_bias": dt0 + jnp.log(-jnp.expm1(-dt0)),
        "a_log": jnp.log(jax.random.uniform(ks[7], (L, SSD_HEADS), f32, 1.0, 16.0)),
        "d_skip": gain(ks[8], (L, SSD_HEADS)),
        "ssd_norm_g": gain(ks[9], (L, SSD_D_INNER)),
        "w_attn_branch": nrm(ks[10], (L, ATTN_HEADS * ATTN_HEAD_DIM, D_MODEL), (ATTN_HEADS * ATTN_HEAD_DIM) ** -0.5),
        "w_ssd_branch": nrm(ks[11], (L, SSD_D_INNER, D_MODEL), SSD_D_INNER ** -0.5),
        "w_out": nrm(ks[12], (L, D_MODEL, D_MODEL), D_MODEL ** -0.5),
        "norm_cross_g": gain(ks[13], (L, D_MODEL)),
        "norm_mem_g": gain(ks[14], (L, D_MODEL)),
        "w_cross_q": nrm(ks[15], (L, D_MODEL, MEM_HEADS * MEM_HEAD_DIM), D_MODEL ** -0.5),
        "w_cross_kv": nrm(ks[16], (L, D_MODEL, 2 * MEM_HEADS * MEM_HEAD_DIM), D_MODEL ** -0.5),
        "w_cross_out": nrm(ks[17], (L, MEM_HEADS * MEM_HEAD_DIM, D_MODEL), D_MODEL ** -0.5),
        "norm_ffn_g": gain(ks[18], (L, D_MODEL)),
        "w_peer_q": nrm(ks[19], (L, D_MODEL, PEER_HEADS * 2 * PEER_KEY_DIM), D_MODEL ** -0.5),
        "peer_sub_keys": nrm(ks[20], (L, PEER_HEADS, 2, PEER_N_KEYS, PEER_KEY_DIM), PEER_KEY_DIM ** -0.5),
        "peer_u": nrm(ks[21], (L, PEER_N_EXPERTS, D_MODEL), D_MODEL ** -0.5),
        "peer_v": nrm(ks[22], (L, PEER_N_EXPERTS, D_MODEL), 0.3),
        "norm_final_g": gain(ks[23], (D_MODEL,)),
    }


def reference(x, mem, positions, norm_mix_g, w_in, conv_w, conv_b, dt_bias, a_log, d_skip,
              ssd_norm_g, w_attn_branch, w_ssd_branch, w_out, norm_cross_g, norm_mem_g,
              w_cross_q, w_cross_kv, w_cross_out, norm_ffn_g, w_peer_q, peer_sub_keys,
              peer_u, peer_v, norm_final_g):
    b, t, _ = x.shape
    offsets = _in_offsets()
    h = x
    for layer in range(DEPTH):
        a = rmsnorm(h, norm_mix_g[layer])
        proj = a @ w_in[layer]
        q, k, v, iq, ik, iw, z, xbc, dt_raw, gate = jnp.split(proj, offsets, axis=-1)
        q = partial_rope(q.reshape(b, t, ATTN_HEADS, ATTN_HEAD_DIM), positions)
        k = partial_rope(k.reshape(b, t, ATTN_KV_HEADS, ATTN_HEAD_DIM), positions)
        v = v.reshape(b, t, ATTN_KV_HEADS, ATTN_HEAD_DIM)
        iq = partial_rope(iq.reshape(b, t, IDX_HEADS, IDX_HEAD_DIM), positions)
        ik = partial_rope(ik.reshape(b, t, 1, IDX_HEAD_DIM), positions)[:, :, 0]
        attn = dsa_attention(q, k, v, iq, ik, iw)
        ssd = ssd_branch(z, xbc, dt_raw, conv_w[layer], conv_b[layer], dt_bias[layer],
                         a_log[layer], d_skip[layer], ssd_norm_g[layer])
        gates = jax.nn.sigmoid(gate.astype(jnp.float32)).reshape(b, t, N_BRANCHES, D_MODEL).astype(x.dtype)
        merged = (gates[:, :, 0] * (attn @ w_attn_branch[layer])
                  + gates[:, :, 1] * (ssd @ w_ssd_branch[layer]))
        h = h + merged @ w_out[layer]
        h = h + memory_cross_attention(rmsnorm(h, norm_cross_g[layer]), rmsnorm(mem, norm_mem_g[layer]),
                                       w_cross_q[layer], w_cross_kv[layer], w_cross_out[layer])
        h = h + peer_ffn(rmsnorm(h, norm_ffn_g[layer]), w_peer_q[layer], peer_sub_keys[layer],
                         peer_u[layer], peer_v[layer])
    return rmsnorm(h, norm_final_g)
```

```python
import math
import numpy as np
import ml_dtypes
from contextlib import ExitStack
import concourse.bass as bass
import concourse.mybir as mybir
from concourse.bass_utils import run_bass_kernel_spmd

F32 = mybir.dt.float32
BF16 = mybir.dt.bfloat16
I32 = mybir.dt.int32
U32 = mybir.dt.uint32
ALU = mybir.AluOpType
ACT = mybir.ActivationFunctionType
AX = mybir.AxisListType

D = 1024
NCORE = 8
TOWN = 2048
SEGT = 1024
NSEG = 8
NCTX = 6144
EPS = 1e-6
NEG = -1.0e30
OQ, OK_, OV, OIQ, OIK, OIW, OZ, OXBC, ODT, OGATE = 0, 1024, 1280, 1536, 1792, 1856, 1860, 3908, 6980, 7012
TWO_PI = 2.0 * math.pi


class Res:
    __slots__ = ("name", "w", "r")

    def __init__(self, name=""):
        self.name = name
        self.w = None
        self.r = []


class FW:
    NDMA = 24

    def __init__(self, nc, es):
        self.nc = nc
        self.eng = {"pe": nc.tensor, "dve": nc.vector, "act": nc.scalar, "pool": nc.gpsimd, "sp": nc.sync}
        self.sem = {k: es.enter_context(nc.semaphore("s_" + k)) for k in self.eng}
        self.cnt = {k: 0 for k in self.eng}
        self.dsem = [es.enter_context(nc.semaphore(f"d{i}")) for i in range(self.NDMA)]
        self.dcnt = [0] * self.NDMA
        self.dnext = 0
        self.seen = {k: {} for k in self.eng}

    def _wait(self, e, tok):
        sem, val = tok
        if self.seen[e].get(sem.name, 0) >= val:
            return
        self.eng[e].wait_ge(sem, val)
        self.seen[e][sem.name] = val

    def _deps(self, e, reads, writes, skip_self=False):
        toks = []
        for r in reads:
            if r.w is not None:
                toks.append(r.w)
        for w in writes:
            if w.w is not None:
                toks.append(w.w)
            toks.extend(w.r)
        best = {}
        for sem, val in toks:
            if skip_self and sem is self.sem[e]:
                continue
            if sem.name not in best or best[sem.name][1] < val:
                best[sem.name] = (sem, val)
        for tok in best.values():
            self._wait(e, tok)

    def _commit(self, tok, reads, writes):
        for r in reads:
            r.r.append(tok)
            if len(r.r) > 32:
                best = {}
                for s, v in r.r:
                    if s.name not in best or best[s.name][1] < v:
                        best[s.name] = (s, v)
                r.r = list(best.values())
        for w in writes:
            w.w = tok
            w.r = []

    def op(self, e, fn, reads=(), writes=(), skip_self=False):
        self._deps(e, reads, writes, skip_self)
        ins = fn(self.eng[e])
        self.cnt[e] += 1
        ins.then_inc(self.sem[e], 1)
        tok = (self.sem[e], self.cnt[e])
        self._commit(tok, reads, writes)
        return tok

    def dma(self, e, out, in_, reads=(), writes=(), indirect=None, **kw):
        i = self.dnext
        self.dnext = (self.dnext + 1) % self.NDMA
        sem = self.dsem[i]
        if self.dcnt[i] > 0:
            self._wait(e, (sem, self.dcnt[i]))
        self._deps(e, reads, writes)
        if indirect is not None:
            ins = self.eng[e].indirect_dma_start(out=out, out_offset=None, in_=in_, in_offset=indirect)
        else:
            ins = self.eng[e].dma_start(out=out, in_=in_, **kw)
        self.dcnt[i] += 16
        ins.then_inc(sem, 16)
        tok = (sem, self.dcnt[i])
        self._commit(tok, reads, writes)
        return tok

    def barrier(self):
        toks = [(self.sem[k], self.cnt[k]) for k in self.eng if self.cnt[k] > 0]
        toks += [(self.dsem[i], self.dcnt[i]) for i in range(self.NDMA) if self.dcnt[i] > 0]
        for e in self.eng:
            for t in toks:
                if t[0] is self.sem[e]:
                    continue
                self._wait(e, t)


class Pool:
    N = [0]

    def __init__(self, nc, es):
        self.nc, self.es = nc, es

    def sb(self, shape, dt, name=None):
        Pool.N[0] += 1
        t = self.es.enter_context(self.nc.sbuf_tensor(f"{name or 't'}_{Pool.N[0]}", list(shape), dt))
        return t, Res(name or "t")


def build(dbg=False, phases=(1, 2, 3)):
    nc = bass.Bass("TRN2", target_bir_lowering=False)

    def din(name, shape, dt=F32):
        return nc.dram_tensor(name, list(shape), dt, kind="ExternalInput").ap()

    x_seg = din("x_seg", [NSEG, SEGT, D])
    pos_seg = din("pos_seg", [128, NSEG * 8])
    valid_seg = din("valid_seg", [128, NSEG * 8])
    pen_ctx = din("pen_ctx", [128, 6])
    consts = din("consts", [128, 5 * 128])
    invf = din("invf", [128, 24])
    mem = din("mem", [256, D])
    w_in = din("w_in", [D, 9060])
    g_mix = din("g_mix", [128, 8])
    convw = din("convw", [128, 24 * 4])
    convb = din("convb", [128, 24])
    hp3 = din("hp3", [128, 96])
    g_ssd = din("g_ssd", [128, 16])
    w_attn_branch = din("w_attn_branch", [D, D])
    w_ssd_branch = din("w_ssd_branch", [2048, D])
    w_out = din("w_out", [D, D])
    g_cross = din("g_cross", [128, 8])
    g_mem = din("g_mem", [128, 8])
    w_cross_q = din("w_cross_q", [D, D])
    w_cross_kv = din("w_cross_kv", [D, 2048])
    w_cross_out = din("w_cross_out", [D, D])
    g_ffn = din("g_ffn", [128, D])
    g_ffn_c = din("g_ffn_c", [128, 8])
    w_peer_q = din("w_peer_q", [D, 2048])
    sub_keys = din("sub_keys", [16, 128, 128])
    peer_u = din("peer_u", [16384, D])
    peer_v = din("peer_v", [16384, D])
    g_final = din("g_final", [128, D])
    y_out = nc.dram_tensor("y_out", [TOWN, D], F32, kind="ExternalOutput").ap()
    sproj_d = nc.dram_tensor("sproj_d", [TOWN, D], F32, kind="Internal" if 1 in phases else "ExternalInput").ap()
    attn_d = nc.dram_tensor("attn_d", [TOWN, D], F32, kind="Internal" if 2 in phases else "ExternalInput").ap()
    if dbg:
        dbg_sproj = nc.dram_tensor("dbg_sproj", [TOWN, D], F32, kind="ExternalOutput").ap()
        dbg_attn = nc.dram_tensor("dbg_attn", [TOWN, D], F32, kind="ExternalOutput").ap()

    with ExitStack() as es0:
        fw = FW(nc, es0)
        P0 = Pool(nc, es0)
        banks = []
        for i in range(8):
            t = es0.enter_context(nc.psum_tensor(f"bank{i}", [128, 512], F32))
            banks.append((t, Res(f"bank{i}")))
        cst, r_cst = P0.sb([128, 5 * 128], F32, "cst")
        cstb, r_cstb = P0.sb([128, 5 * 128], BF16, "cstb")
        fw.dma("sp", cst[:], consts, writes=[r_cst])
        fw.op("dve", lambda e: e.tensor_copy(out=cstb[:], in_=cst[:]), reads=[r_cst], writes=[r_cstb])
        ident_b = cstb[:, 0:128]
        tri_f = cst[:, 128:256]
        ones_f = cst[:, 256:384]
        tri_pen = cst[:, 384:512]
        ones_b = cstb[:, 256:384]
        gm, r_gm = P0.sb([128, 8], F32, "gm")
        fw.dma("sp", gm[:], g_mix, writes=[r_gm])
        posv, r_posv = P0.sb([128, NSEG * 8], F32, "posv")
        fw.dma("sp", posv[:], pos_seg, writes=[r_posv])
        validv, r_validv = P0.sb([128, NSEG * 8], F32, "validv")
        fw.dma("sp", validv[:], valid_seg, writes=[r_validv])
        invt, r_invt = P0.sb([128, 24], F32, "invt")
        fw.dma("sp", invt[:], invf, writes=[r_invt])

        def w_rows(ap2d):
            return ap2d.rearrange("(c p) n -> p c n", p=128)

        w_in_r = w_rows(w_in)

        def load_weight(pool, dst, r_dst, src_r, c0, ncols, gt, r_gt, nch=8, eng="pool", stg_cols=512):
            stgf, r_stg = pool
            stg_cols = 4096 // nch
            stg = stgf[:, 0:nch * stg_cols].rearrange("p (c n) -> p c n", c=nch)
            done = 0
            while done < ncols:
                n = min(stg_cols, ncols - done)
                fw.dma("sp", stg[:, 0:nch, 0:n], src_r[:, 0:nch, c0 + done:c0 + done + n], writes=[r_stg])
                if gt is None:
                    fw.op(eng, lambda e, n=n, done=done: e.tensor_copy(out=dst[:, 0:nch, done:done + n], in_=stg[:, 0:nch, 0:n]),
                          reads=[r_stg], writes=[r_dst])
                else:
                    fw.op(eng, lambda e, n=n, done=done: e.tensor_tensor(
                        out=dst[:, 0:nch, done:done + n], in0=stg[:, 0:nch, 0:n],
                        in1=gt[:, 0:nch].unsqueeze(2).to_broadcast([128, nch, n]), op=ALU.mult),
                        reads=[r_stg, r_gt], writes=[r_dst])
                done += n

        def build_xT(pool, seg, xT, r_xT, xt_bufs, ntile=8, src=None, gdiv=D):
            for ti in range(ntile):
                xt, r_xt, xb, r_xb, ss, r_ss = xt_bufs[ti % 2]
                srcap = x_seg[seg, ti * 128:(ti + 1) * 128, :] if src is None else src(ti)
                fw.dma("sp", xt[:], srcap, writes=[r_xt])
                fw.op("act", lambda e: e.activation(out=xb[:], in_=xt[:], func=ACT.Square, accum_out=ss[:, 0:1]),
                      reads=[r_xt], writes=[r_xb, r_ss])
                fw.op("dve", lambda e: e.tensor_scalar(out=ss[:, 1:2], in0=ss[:, 0:1], scalar1=1.0 / gdiv, scalar2=EPS,
                                                       op0=ALU.mult, op1=ALU.add), reads=[r_ss], writes=[r_ss])
                fw.op("act", lambda e: e.activation(out=ss[:, 3:4], in_=ss[:, 1:2], func=ACT.Ln), reads=[r_ss], writes=[r_ss])
                fw.op("act", lambda e: e.activation(out=ss[:, 2:3], in_=ss[:, 3:4], func=ACT.Exp, scale=-0.5), reads=[r_ss], writes=[r_ss])
                fw.op("dve", lambda e: e.tensor_scalar(out=xb[:], in0=xt[:], scalar1=ss[:, 2:3], scalar2=None, op0=ALU.mult),
                      reads=[r_xt, r_ss], writes=[r_xb])
                bk, r_bk = banks[ti % 2]
                bkb = bk[:].bitcast(BF16)
                for ch in range(8):
                    fw.op("pe", lambda e, ch=ch: e.transpose(out=bkb[:, ch * 128:(ch + 1) * 128], in_=xb[:, ch * 128:(ch + 1) * 128],
                                                            identity=ident_b),
                          reads=[r_xb, r_cstb], writes=[r_bk], skip_self=True)
                fw.op("act", lambda e, ti=ti: e.activation(out=xT[:, :, ti * 128:(ti + 1) * 128],
                                                          in_=bkb.rearrange("p (c t) -> p c t", c=8), func=ACT.Copy),
                      reads=[r_bk], writes=[r_xT])

        if 1 in phases:
            with ExitStack() as es1:
                P = Pool(nc, es1)
                xT, r_xT = P.sb([128, 8, SEGT], BF16, "xT")
                xt_bufs = []
                for i in range(2):
                    xt, r_xt = P.sb([128, D], F32, "xt")
                    xb, r_xb = P.sb([128, D], BF16, "xb")
                    ss, r_ss = P.sb([128, 4], F32, "ss")
                    xt_bufs.append((xt, r_xt, xb, r_xb, ss, r_ss))
                wstg = P.sb([128, 4096], F32, "wstg")
                cw, r_cw = P.sb([128, 24, 4], F32, "cw")
                cb, r_cb = P.sb([128, 24], F32, "cb")
                fw.dma("sp", cw[:], convw.rearrange("p (r k) -> p r k", k=4), writes=[r_cw])
                fw.dma("sp", cb[:], convb, writes=[r_cb])
                hp, r_hp = P.sb([128, 96], F32, "hp")
                fw.dma("sp", hp[:], hp3, writes=[r_hp])
                gs, r_gs = P.sb([128, 16], F32, "gs")
                fw.dma("sp", gs[:], g_ssd, writes=[r_gs])
                a_bc, r_abc = P.sb([128, 32], F32, "a_bc")
                fw.op("act", lambda e: e.activation(out=a_bc[:], in_=hp[:, 32:64], func=ACT.Exp), reads=[r_hp], writes=[r_abc])
                fw.op("dve", lambda e: e.tensor_scalar(out=a_bc[:], in0=a_bc[:], scalar1=-1.0, scalar2=None, op0=ALU.mult),
                      reads=[r_abc], writes=[r_abc])
                wdt, r_wdt = P.sb([128, 8, 32], BF16, "wdt")
                load_weight(wstg, wdt, r_wdt, w_in_r, ODT, 32, gm, r_gm)
                halo, r_halo = P.sb([128, 24, 3], F32, "halo")
                fw.op("pool", lambda e: e.memset(halo[:], 0.0), writes=[r_halo])
                state, r_state = P.sb([128, 4, 512], F32, "state")
                fw.op("pool", lambda e: e.memset(state[:], 0.0), writes=[r_state])
                state_bf, r_statebf = P.sb([128, 512], BF16, "state_bf")
                NT = SEGT // 128
                dtr, r_dtr = P.sb([128, NT, 32], F32, "dtr")
                tmpa, r_tmpa = P.sb([128, NT, 32], F32, "tmpa")
                tmpb, r_tmpb = P.sb([128, NT, 32], F32, "tmpb")
                dt_all, r_dt = P.sb([128, NT, 32], F32, "dt_all")
                adt, r_adt = P.sb([128, NT, 32], F32, "adt")
                acs, r_acs = P.sb([128, NT, 32], F32, "acs")
                tot, r_tot = P.sb([128, NT, 32], F32, "tot")
                dtdte, r_dtdte = P.sb([128, NT, 32], F32, "dtdte")
                cdec, r_cdec = P.sb([128, NT, 32], F32, "cdec")
                eacs, r_eacs = P.sb([128, NT, 32], F32, "eacs")
                wrow, r_wrow = P.sb([128, 8, 128], BF16, "wrow")
                rowbufs = [P.sb([128, 3 + SEGT], F32, "rowbuf") for _ in range(2)]
                convacc = [P.sb([128, SEGT], F32, "convacc") for _ in range(2)]
                rowact = [P.sb([128, SEGT], BF16, "rowact") for _ in range(2)]
                xs_tok, r_xstok = P.sb([128, NT, 512], BF16, "xs_tok")
                B_tok, r_Btok = P.sb([128, NT, 128], BF16, "B_tok")
                BT, r_BT = P.sb([128, SEGT], BF16, "BT")
                CT, r_CT = P.sb([128, SEGT], BF16, "CT")
                xdt, r_xdt = P.sb([128, 512], BF16, "xdt")
                xdd, r_xdd = P.sb([128, 512], BF16, "xdd")
                y_store, r_ys = P.sb([128, NT, 2048], BF16, "y_store")
                cbm, r_cbm = P.sb([128, 128], F32, "cbm")
                triadt, r_triadt = P.sb([128, 8, 128], F32, "triadt")
                segb, r_segb = P.sb([128, 8, 128], F32, "segb")
                mT, r_mT = P.sb([128, 8, 128], BF16, "mT")
                t1, r_t1 = P.sb([128, 512], F32, "t1")
                t2, r_t2 = P.sb([128, 512], F32, "t2")
                wz, r_wz = P.sb([128, 8, 512], BF16, "wz")
                ws, r_ws = P.sb([128, 16, 1024], BF16, "ws")
                zact, r_zact = P.sb([128, 512], F32, "zact")
                ssq, r_ssq = P.sb([128, NT, 8], F32, "ssq")
                ygT, r_ygT = P.sb([128, 16, 128], BF16, "ygT")
                spt, r_spt = P.sb([128, D], F32, "spt")
                junk, r_junk = P.sb([128, 512], F32, "junk")

                for seg in range(NSEG):
                    own = seg >= 6
                    build_xT(P, seg, xT, r_xT, xt_bufs)
                    bk, r_bk = banks[2]
                    for ti in range(NT):
                        for ch in range(8):
                            fw.op("pe", lambda e, ti=ti, ch=ch: e.matmul(bk[:, ti * 32:(ti + 1) * 32], lhsT=xT[:, ch, ti * 128:(ti + 1) * 128],
                                                                          rhs=wdt[:, ch, :], start=(ch == 0), stop=(ch == 7)),
                                  reads=[r_xT, r_wdt], writes=[r_bk], skip_self=True)
                    bkv = bk[:, 0:NT * 32].rearrange("p (t h) -> p t h", h=32)
                    fw.op("dve", lambda e: e.tensor_tensor(out=dtr[:], in0=bkv, in1=hp[:, 0:32].unsqueeze(1).to_broadcast([128, NT, 32]), op=ALU.add),
                          reads=[r_bk, r_hp], writes=[r_dtr])
                    fw.op("dve", lambda e: e.tensor_scalar(out=tmpa[:], in0=dtr[:], scalar1=-1.0, scalar2=None, op0=ALU.mult), reads=[r_dtr], writes=[r_tmpa])
                    fw.op("dve", lambda e: e.tensor_tensor(out=tmpa[:], in0=tmpa[:], in1=dtr[:], op=ALU.max), reads=[r_dtr, r_tmpa], writes=[r_tmpa])
                    fw.op("act", lambda e: e.activation(out=tmpa[:], in_=tmpa[:], func=ACT.Exp, scale=-1.0), reads=[r_tmpa], writes=[r_tmpa])
                    fw.op("act", lambda e: e.activation(out=tmpa[:], in_=tmpa[:], func=ACT.Ln, bias=1.0), reads=[r_tmpa], writes=[r_tmpa])
                    fw.op("dve", lambda e: e.tensor_single_scalar(out=tmpb[:], in_=dtr[:], scalar=0.0, op=ALU.max), reads=[r_dtr], writes=[r_tmpb])
                    fw.op("dve", lambda e: e.tensor_tensor(out=tmpb[:], in0=tmpb[:], in1=tmpa[:], op=ALU.add), reads=[r_tmpa, r_tmpb], writes=[r_tmpb])
                    fw.op("dve", lambda e, seg=seg: e.tensor_tensor(out=dt_all[:], in0=tmpb[:],
                                                                  in1=validv[:, seg * NT:(seg + 1) * NT].unsqueeze(2).to_broadcast([128, NT, 32]), op=ALU.mult),
                          reads=[r_tmpb, r_validv], writes=[r_dt])
                    fw.op("dve", lambda e: e.tensor_tensor(out=adt[:], in0=dt_all[:], in1=a_bc[:].unsqueeze(1).to_broadcast([128, NT, 32]), op=ALU.mult),
                          reads=[r_dt, r_abc], writes=[r_adt])
                    bk3, r_bk3 = banks[3]
                    for ti in range(NT):
                        fw.op("pe", lambda e, ti=ti: e.matmul(bk[:, ti * 32:(ti + 1) * 32], lhsT=tri_f, rhs=adt[:, ti, :], start=True, stop=True),
                              reads=[r_cst, r_adt], writes=[r_bk], skip_self=True)
                        fw.op("pe", lambda e, ti=ti: e.matmul(bk3[:, ti * 32:(ti + 1) * 32], lhsT=ones_f, rhs=adt[:, ti, :], start=True, stop=True),
                              reads=[r_cst, r_adt], writes=[r_bk3], skip_self=True)
                    fw.op("act", lambda e: e.activation(out=acs[:], in_=bkv, func=ACT.Copy), reads=[r_bk], writes=[r_acs])
                    bk3v = bk3[:, 0:NT * 32].rearrange("p (t h) -> p t h", h=32)
                    fw.op("act", lambda e: e.activation(out=tot[:], in_=bk3v, func=ACT.Copy), reads=[r_bk3], writes=[r_tot])
                    fw.op("dve", lambda e: e.tensor_tensor(out=tmpa[:], in0=tot[:], in1=acs[:], op=ALU.subtract), reads=[r_tot, r_acs], writes=[r_tmpa])
                    fw.op("act", lambda e: e.activation(out=tmpa[:], in_=tmpa[:], func=ACT.Exp), reads=[r_tmpa], writes=[r_tmpa])
                    fw.op("dve", lambda e: e.tensor_tensor(out=dtdte[:], in0=tmpa[:], in1=dt_all[:], op=ALU.mult), reads=[r_tmpa, r_dt], writes=[r_dtdte])
                    fw.op("act", lambda e: e.activation(out=cdec[:], in_=tot[:], func=ACT.Exp), reads=[r_tot], writes=[r_cdec])
                    if own:
                        fw.op("act", lambda e: e.activation(out=eacs[:], in_=acs[:], func=ACT.Exp), reads=[r_acs], writes=[r_eacs])
                        fw.op("pool", lambda e: e.memset(ssq[:], 0.0), writes=[r_ssq])

                    for g in range(4):
                        rows = [(4 * g + i, "xs", i) for i in range(4)] + [(16 + g, "B", 0)] + ([(20 + g, "C", 0)] if seg >= 5 else [])
                        for ri, (r, kind, sub) in enumerate(rows):
                            rb, r_rb = rowbufs[ri % 2]
                            ca, r_ca = convacc[ri % 2]
                            ra, r_ra = rowact[ri % 2]
                            load_weight(wstg, wrow, r_wrow, w_in_r, OXBC + r * 128, 128, gm, r_gm)
                            for tg in range(SEGT // 512):
                                bkp, r_bkp = banks[4 + (tg % 2)]
                                for ch in range(8):
                                    fw.op("pe", lambda e, ch=ch, tg=tg, bkp=bkp: e.matmul(bkp[:], lhsT=wrow[:, ch, :], rhs=xT[:, ch, tg * 512:(tg + 1) * 512],
                                                                                          start=(ch == 0), stop=(ch == 7)),
                                          reads=[r_wrow, r_xT], writes=[r_bkp], skip_self=True)
                                fw.op("act", lambda e, tg=tg, bkp=bkp, rb=rb: e.activation(out=rb[:, 3 + tg * 512:3 + (tg + 1) * 512], in_=bkp[:], func=ACT.Copy),
                                      reads=[r_bkp], writes=[r_rb])
                            fw.op("pool", lambda e, rb=rb, r=r: e.tensor_copy(out=rb[:, 0:3], in_=halo[:, r, :]), reads=[r_halo], writes=[r_rb])
                            fw.op("pool", lambda e, rb=rb, r=r: e.tensor_copy(out=halo[:, r, :], in_=rb[:, SEGT:SEGT + 3]), reads=[r_rb], writes=[r_halo])
                            if kind == "C" and not own:
                                continue
                            fw.op("dve", lambda e, rb=rb, ca=ca, r=r: e.tensor_scalar(out=ca[:], in0=rb[:, 0:SEGT], scalar1=cw[:, r, 0:1], scalar2=None, op0=ALU.mult),
                                  reads=[r_rb, r_cw], writes=[r_ca])
                            for k in range(1, 4):
                                fw.op("dve", lambda e, rb=rb, ca=ca, r=r, k=k: e.scalar_tensor_tensor(out=ca[:], in0=rb[:, k:k + SEGT], scalar=cw[:, r, k:k + 1], in1=ca[:],
                                                                                                     op0=ALU.mult, op1=ALU.add),
                                      reads=[r_rb, r_cw, r_ca], writes=[r_ca])
                            fw.op("act", lambda e, ca=ca, ra=ra, r=r: e.activation(out=ra[:], in_=ca[:], func=ACT.Silu, bias=cb[:, r:r + 1]),
                                  reads=[r_ca, r_cb], writes=[r_ra])
                            if kind == "C":
                                fw.op("pool", lambda e, ra=ra: e.tensor_copy(out=CT[:], in_=ra[:]), reads=[r_ra], writes=[r_CT])
                                continue
                            if kind == "B" and own:
                                fw.op("pool", lambda e, ra=ra: e.tensor_copy(out=BT[:], in_=ra[:]), reads=[r_ra], writes=[r_BT])
                            bkt, r_bkt = banks[6 + (ri % 2)]
                            bktb = bkt[:].bitcast(BF16)
                            for ti in range(NT):
                                fw.op("pe", lambda e, ti=ti, ra=ra, bktb=bktb: e.transpose(out=bktb[:, ti * 128:(ti + 1) * 128], in_=ra[:, ti * 128:(ti + 1) * 128], identity=ident_b),
                                      reads=[r_ra, r_cstb], writes=[r_bkt], skip_self=True)
                            src = bktb[:, 0:NT * 128].rearrange("p (t c) -> p t c", c=128)
                            if kind == "xs":
                                fw.op("act", lambda e, src=src, sub=sub: e.activation(out=xs_tok[:, :, sub * 128:(sub + 1) * 128], in_=src, func=ACT.Copy),
                                      reads=[r_bkt], writes=[r_xstok])
                            else:
                                fw.op("act", lambda e, src=src: e.activation(out=B_tok[:], in_=src, func=ACT.Copy), reads=[r_bkt], writes=[r_Btok])
                        for c in range(NT):
                            hs = slice(8 * g, 8 * g + 8)
                            xsv = xs_tok[:, c, :].rearrange("p (h q) -> p h q", q=64)
                            fw.op("dve", lambda e, c=c, xsv=xsv, hs=hs: e.tensor_tensor(out=xdd[:].rearrange("p (h q) -> p h q", q=64), in0=xsv,
                                                                                        in1=dtdte[:, c, hs].unsqueeze(2).to_broadcast([128, 8, 64]), op=ALU.mult),
                                  reads=[r_xstok, r_dtdte], writes=[r_xdd])
                            bks, r_bks = banks[2]
                            fw.op("pe", lambda e, c=c, bks=bks: e.matmul(bks[:], lhsT=B_tok[:, c, :], rhs=xdd[:], start=True, stop=True),
                                  reads=[r_Btok, r_xdd], writes=[r_bks], skip_self=True)
                            if own:
                                fw.op("act", lambda e, g=g: e.activation(out=state_bf[:], in_=state[:, g, :], func=ACT.Copy), reads=[r_state], writes=[r_statebf])
                                fw.op("dve", lambda e, c=c, xsv=xsv, hs=hs: e.tensor_tensor(out=xdt[:].rearrange("p (h q) -> p h q", q=64), in0=xsv,
                                                                                            in1=dt_all[:, c, hs].unsqueeze(2).to_broadcast([128, 8, 64]), op=ALU.mult),
                                      reads=[r_xstok, r_dt], writes=[r_xdt])
                                cs = slice(c * 128, (c + 1) * 128)
                                bkc, r_bkc = banks[3]
                                fw.op("pe", lambda e, cs=cs, bkc=bkc: e.matmul(bkc[:, 0:128], lhsT=BT[:, cs], rhs=CT[:, cs], start=True, stop=True),
                                      reads=[r_BT, r_CT], writes=[r_bkc], skip_self=True)
                                fw.op("dve", lambda e, bkc=bkc: e.tensor_tensor(out=cbm[:], in0=bkc[:, 0:128], in1=tri_f, op=ALU.mult), reads=[r_bkc, r_cst], writes=[r_cbm])
                                fw.op("pool", lambda e, c=c, hs=hs: e.tensor_tensor(out=triadt[:], in0=tri_f.unsqueeze(1).to_broadcast([128, 8, 128]),
                                                                                   in1=adt[:, c, hs].unsqueeze(2).to_broadcast([128, 8, 128]), op=ALU.mult),
                                      reads=[r_cst, r_adt], writes=[r_triadt])
                                bka, r_bka = banks[4]
                                bkb_, r_bkb_ = banks[5]
                                fw.op("pe", lambda e, bka=bka: e.matmul(bka[:], lhsT=ones_f, rhs=triadt[:, 0:4, :].rearrange("p h l -> p (h l)"), start=True, stop=True),
                                      reads=[r_cst, r_triadt], writes=[r_bka], skip_self=True)
                                fw.op("pe", lambda e, bkb_=bkb_: e.matmul(bkb_[:], lhsT=ones_f, rhs=triadt[:, 4:8, :].rearrange("p h l -> p (h l)"), start=True, stop=True),
                                      reads=[r_cst, r_triadt], writes=[r_bkb_], skip_self=True)
                                for h in range(8):
                                    bsrc, r_bsrc = (bka, r_bka) if h < 4 else (bkb_, r_bkb_)
                                    hh = h % 4
                                    fw.op("dve", lambda e, h=h, hh=hh, bsrc=bsrc, c=c, g=g: e.scalar_tensor_tensor(
                                        out=segb[:, h, :], in0=bsrc[:, hh * 128:(hh + 1) * 128], scalar=acs[:, c, 8 * g + h:8 * g + h + 1], in1=tri_f,
                                        op0=ALU.subtract, op1=ALU.mult), reads=[r_bsrc, r_acs, r_cst], writes=[r_segb])
                                fw.op("act", lambda e: e.activation(out=segb[:], in_=segb[:], func=ACT.Exp), reads=[r_segb], writes=[r_segb])
                                fw.op("dve", lambda e: e.tensor_tensor(out=mT[:], in0=segb[:], in1=cbm[:].unsqueeze(1).to_broadcast([128, 8, 128]), op=ALU.mult),
                                      reads=[r_segb, r_cbm], writes=[r_mT])
                                bky, r_bky = banks[6]
                                for h in range(8):
                                    fw.op("pe", lambda e, h=h, bky=bky: e.matmul(bky[:, h * 64:(h + 1) * 64], lhsT=mT[:, h, :], rhs=xdt[:, h * 64:(h + 1) * 64], start=True, stop=True),
                                          reads=[r_mT, r_xdt], writes=[r_bky], skip_self=True)
                                bko, r_bko = banks[7]
                                fw.op("pe", lambda e, cs=cs, bko=bko: e.matmul(bko[:], lhsT=CT[:, cs], rhs=state_bf[:], start=True, stop=True),
                                      reads=[r_CT, r_statebf], writes=[r_bko], skip_self=True)
                                fw.op("dve", lambda e, c=c, hs=hs, bko=bko: e.tensor_tensor(out=t1[:].rearrange("p (h q) -> p h q", q=64),
                                                                                          in0=bko[:].rearrange("p (h q) -> p h q", q=64),
                                                                                          in1=eacs[:, c, hs].unsqueeze(2).to_broadcast([128, 8, 64]), op=ALU.mult),
                                      reads=[r_bko, r_eacs], writes=[r_t1])
                                fw.op("pool", lambda e, xsv=xsv, hs=hs: e.tensor_tensor(out=t2[:].rearrange("p (h q) -> p h q", q=64), in0=xsv,
                                                                                       in1=hp[:, 64 + hs.start:64 + hs.stop].unsqueeze(2).to_broadcast([128, 8, 64]), op=ALU.mult),
                                      reads=[r_xstok, r_hp], writes=[r_t2])
                                fw.op("dve", lambda e: e.tensor_tensor(out=t1[:], in0=t1[:], in1=t2[:], op=ALU.add), reads=[r_t1, r_t2], writes=[r_t1])
                                fw.op("dve", lambda e, c=c, g=g, bky=bky: e.tensor_tensor(out=y_store[:, c, g * 512:(g + 1) * 512], in0=bky[:], in1=t1[:], op=ALU.add),
                                      reads=[r_bky, r_t1], writes=[r_ys])
                            stv = state[:, g, :].rearrange("p (h q) -> p h q", q=64)
                            fw.op("dve", lambda e, c=c, hs=hs, stv=stv: e.tensor_tensor(out=stv, in0=stv, in1=cdec[:, c, hs].unsqueeze(2).to_broadcast([128, 8, 64]), op=ALU.mult),
                                  reads=[r_state, r_cdec], writes=[r_state])
                            fw.op("dve", lambda e, g=g, bks=bks: e.tensor_tensor(out=state[:, g, :], in0=state[:, g, :], in1=bks[:], op=ALU.add),
                                  reads=[r_state, r_bks], writes=[r_state])
                        if own:
                            load_weight(wstg, wz, r_wz, w_in_r, OZ + g * 512, 512, gm, r_gm)
                            for c in range(NT):
                                bkz, r_bkz = banks[2 + (c % 2)]
                                for ch in range(8):
                                    fw.op("pe", lambda e, c=c, ch=ch, bkz=bkz: e.matmul(bkz[:], lhsT=xT[:, ch, c * 128:(c + 1) * 128], rhs=wz[:, ch, :], start=(ch == 0), stop=(ch == 7)),
                                          reads=[r_xT, r_wz], writes=[r_bkz], skip_self=True)
                                fw.op("act", lambda e, bkz=bkz: e.activation(out=zact[:], in_=bkz[:], func=ACT.Silu), reads=[r_bkz], writes=[r_zact])
                                ysl = y_store[:, c, g * 512:(g + 1) * 512]
                                fw.op("dve", lambda e, ysl=ysl: e.tensor_tensor(out=zact[:], in0=zact[:], in1=ysl, op=ALU.mult), reads=[r_zact, r_ys], writes=[r_zact])
                                fw.op("act", lambda e, c=c, g=g: e.activation(out=junk[:], in_=zact[:], func=ACT.Square, accum_out=ssq[:, c, g:g + 1]),
                                      reads=[r_zact], writes=[r_junk, r_ssq])
                                fw.op("pool", lambda e, ysl=ysl: e.tensor_copy(out=ysl, in_=zact[:]), reads=[r_zact], writes=[r_ys])
                    if own:
                        load_weight(wstg, ws, r_ws, w_rows(w_ssd_branch), 0, 1024, gs, r_gs, nch=16, stg_cols=512)
                        for c in range(NT):
                            fw.op("dve", lambda e, c=c: e.tensor_reduce(out=ssq[:, c, 4:5], in_=ssq[:, c, 0:4], axis=AX.X, op=ALU.add), reads=[r_ssq], writes=[r_ssq])
                            fw.op("dve", lambda e, c=c: e.tensor_scalar(out=ssq[:, c, 5:6], in0=ssq[:, c, 4:5], scalar1=1.0 / 2048, scalar2=EPS, op0=ALU.mult, op1=ALU.add),
                                  reads=[r_ssq], writes=[r_ssq])
                            fw.op("act", lambda e, c=c: e.activation(out=ssq[:, c, 7:8], in_=ssq[:, c, 5:6], func=ACT.Ln), reads=[r_ssq], writes=[r_ssq])
                            fw.op("act", lambda e, c=c: e.activation(out=ssq[:, c, 6:7], in_=ssq[:, c, 7:8], func=ACT.Exp, scale=-0.5), reads=[r_ssq], writes=[r_ssq])
                            for half in range(2):
                                bkt, r_bkt = banks[4 + half]
                                bktb = bkt[:].bitcast(BF16)
                                for i in range(8):
                                    ch = half * 8 + i
                                    fw.op("pe", lambda e, c=c, ch=ch, i=i, bktb=bktb: e.transpose(out=bktb[:, i * 128:(i + 1) * 128], in_=y_store[:, c, ch * 128:(ch + 1) * 128], identity=ident_b),
                                          reads=[r_ys, r_cstb], writes=[r_bkt], skip_self=True)
                                fw.op("act", lambda e, half=half, bktb=bktb: e.activation(out=ygT[:, half * 8:(half + 1) * 8, :], in_=bktb.rearrange("p (c t) -> p c t", c=8), func=ACT.Copy),
                                      reads=[r_bkt], writes=[r_ygT])
                            for cg in range(2):
                                bko, r_bko = banks[6 + cg]
                                for ch in range(16):
                                    fw.op("pe", lambda e, ch=ch, cg=cg, bko=bko: e.matmul(bko[:], lhsT=ygT[:, ch, :], rhs=ws[:, ch, cg * 512:(cg + 1) * 512], start=(ch == 0), stop=(ch == 15)),
                                          reads=[r_ygT, r_ws], writes=[r_bko], skip_self=True)
                                fw.op("dve", lambda e, c=c, cg=cg, bko=bko: e.tensor_scalar(out=spt[:, cg * 512:(cg + 1) * 512], in0=bko[:], scalar1=ssq[:, c, 6:7], scalar2=None, op0=ALU.mult),
                                      reads=[r_bko, r_ssq], writes=[r_spt])
                            t0 = (seg - 6) * SEGT + c * 128
                            fw.dma("sp", sproj_d[t0:t0 + 128, :], spt[:], reads=[r_spt])
                            if dbg:
                                fw.dma("sp", dbg_sproj[t0:t0 + 128, :], spt[:], reads=[r_spt])
                fw.barrier()

        if 2 in phases:
            with ExitStack() as es2:
                P = Pool(nc, es2)
                NT = SEGT // 128
                KT, r_KT = P.sb([128, 2, 8192], BF16, "KT")
                Vx, r_Vx = P.sb([128, 64, 2, 129], BF16, "Vx")
                ikT, r_ikT = P.sb([128, 4096], BF16, "ikT")
                fw.op("pool", lambda e: e.memset(Vx[:], 1.0), writes=[r_Vx])
                wq, r_wq = P.sb([128, 8, 1024], BF16, "wq")
                wiq, r_wiq = P.sb([128, 8, 260], BF16, "wiq")
                penx, r_penx = P.sb([128, 8], F32, "penx")
                fw.op("pool", lambda e: e.memset(penx[:], 0.0), writes=[r_penx])
                fw.dma("sp", penx[:, 0:6], pen_ctx, writes=[r_penx])
                trig, r_trig = P.sb([128, 4, 24], F32, "trig")
                trigi, r_trigi = P.sb([128, 24], I32, "trigi")
                trigk, r_trigk = P.sb([128, 24], F32, "trigk")
                rt = [P.sb([128, 8, 16], F32, "rt") for _ in range(4)]
                kb, r_kb = P.sb([128, 1024], BF16, "kb")
                xt_bufs = []
                for i in range(1):
                    xt, r_xt = P.sb([128, D], F32, "xt")
                    xb, r_xb = P.sb([128, D], BF16, "xb")
                    ss, r_ss = P.sb([128, 4], F32, "ss")
                    xt_bufs.append((xt, r_xt, xb, r_xb, ss, r_ss))
                xt_bufs = xt_bufs * 2
                es2a = ExitStack()
                Pa = Pool(nc, es2a)
                xT, r_xT = Pa.sb([128, 8, SEGT], BF16, "xT2")
                wstg = Pa.sb([128, 4096], F32, "wstg")
                wkv, r_wkv = Pa.sb([128, 8, 512], BF16, "wkv")
                wik, r_wik = Pa.sb([128, 8, 64], BF16, "wik")
                kf, r_kf = Pa.sb([128, 1024], F32, "kf")
                load_weight(wstg, wkv, r_wkv, w_in_r, OK_, 512, gm, r_gm)
                load_weight(wstg, wik, r_wik, w_in_r, OIK, 64, gm, r_gm)
                load_weight(wstg, wq, r_wq, w_in_r, OQ, 1024, gm, r_gm)
                load_weight(wstg, wiq, r_wiq, w_in_r, OIQ, 256, gm, r_gm)
                stgf, r_stg = wstg
                stg4 = stgf[:, 0:32].rearrange("p (c n) -> p c n", c=8)
                fw.dma("sp", stg4, w_in_r[:, :, OIW:OIW + 4], writes=[r_stg])
                fw.op("pool", lambda e: e.tensor_tensor(out=wiq[:, :, 256:260], in0=stg4, in1=gm[:].unsqueeze(2).to_broadcast([128, 8, 4]), op=ALU.mult),
                      reads=[r_stg, r_gm], writes=[r_wiq])

                def trig_tables(col):
                    fw.op("dve", lambda e: e.tensor_scalar(out=trig[:, 0, :], in0=invt[:], scalar1=posv[:, col:col + 1], scalar2=None, op0=ALU.mult),
                          reads=[r_invt, r_posv], writes=[r_trig])
                    for which, shift in ((2, 0.0), (3, 0.25)):
                        fw.op("dve", lambda e, shift=shift: e.tensor_scalar(out=trig[:, 1, :], in0=trig[:, 0, :], scalar1=1.0 / TWO_PI, scalar2=shift, op0=ALU.mult, op1=ALU.add),
                              reads=[r_trig], writes=[r_trig])
                        fw.op("dve", lambda e: e.tensor_copy(out=trigi[:], in_=trig[:, 1, :]), reads=[r_trig], writes=[r_trigi])
                        fw.op("dve", lambda e: e.tensor_copy(out=trigk[:], in_=trigi[:]), reads=[r_trigi], writes=[r_trigk])
                        fw.op("dve", lambda e: e.scalar_tensor_tensor(out=trig[:, 1, :], in0=trigk[:], scalar=-TWO_PI, in1=trig[:, 0, :], op0=ALU.mult, op1=ALU.add),
                              reads=[r_trigk, r_trig], writes=[r_trig])
                        fw.op("dve", lambda e, shift=shift: e.tensor_scalar(out=trig[:, 1, :], in0=trig[:, 1, :], scalar1=shift * TWO_PI, scalar2=-math.pi, op0=ALU.add, op1=ALU.max),
                              reads=[r_trig], writes=[r_trig])
                        fw.op("dve", lambda e: e.tensor_scalar(out=trig[:, 1, :], in0=trig[:, 1, :], scalar1=math.pi, scalar2=None, op0=ALU.min),
                              reads=[r_trig], writes=[r_trig])
                        fw.op("act", lambda e, which=which: e.activation(out=trig[:, which, :], in_=trig[:, 1, :], func=ACT.Sin), reads=[r_trig], writes=[r_trig])

                def rope(buf, r_buf, nh, dh, half, toff):
                    v = buf[:, 0:nh * dh].rearrange("p (h d) -> p h d", d=dh)
                    x1, x2 = v[:, :, 0:half], v[:, :, half:2 * half]
                    sn = trig[:, 2, toff:toff + half].unsqueeze(1).to_broadcast([128, nh, half])
                    cs = trig[:, 3, toff:toff + half].unsqueeze(1).to_broadcast([128, nh, half])
                    tm = [(t[0][:, 0:nh, 0:half], t[1]) for t in rt]
                    fw.op("dve", lambda e: e.tensor_tensor(out=tm[0][0], in0=x1, in1=cs, op=ALU.mult), reads=[r_buf, r_trig], writes=[tm[0][1]])
                    fw.op("dve", lambda e: e.tensor_tensor(out=tm[1][0], in0=x2, in1=sn, op=ALU.mult), reads=[r_buf, r_trig], writes=[tm[1][1]])
                    fw.op("dve", lambda e: e.tensor_tensor(out=tm[2][0], in0=x2, in1=cs, op=ALU.mult), reads=[r_buf, r_trig], writes=[tm[2][1]])
                    fw.op("dve", lambda e: e.tensor_tensor(out=tm[3][0], in0=x1, in1=sn, op=ALU.mult), reads=[r_buf, r_trig], writes=[tm[3][1]])
                    fw.op("dve", lambda e: e.tensor_tensor(out=x1, in0=tm[0][0], in1=tm[1][0], op=ALU.subtract), reads=[tm[0][1], tm[1][1]], writes=[r_buf])
                    fw.op("dve", lambda e: e.tensor_tensor(out=x2, in0=tm[2][0], in1=tm[3][0], op=ALU.add), reads=[tm[2][1], tm[3][1]], writes=[r_buf])

                for seg in range(NSEG):
                    build_xT(P, seg, xT, r_xT, xt_bufs)
                    for ti in range(NT):
                        blk = seg * NT + ti
                        trig_tables(blk)
                        bk, r_bk = banks[2 + (ti % 2)]
                        for ch in range(8):
                            fw.op("pe", lambda e, ch=ch, ti=ti, bk=bk: e.matmul(bk[:], lhsT=xT[:, ch, ti * 128:(ti + 1) * 128], rhs=wkv[:, ch, :], start=(ch == 0), stop=(ch == 7)),
                                  reads=[r_xT, r_wkv], writes=[r_bk], skip_self=True)
                        bki, r_bki = banks[4 + (ti % 2)]
                        for ch in range(8):
                            fw.op("pe", lambda e, ch=ch, ti=ti, bki=bki: e.matmul(bki[:, 0:64], lhsT=xT[:, ch, ti * 128:(ti + 1) * 128], rhs=wik[:, ch, :], start=(ch == 0), stop=(ch == 7)),
                                  reads=[r_xT, r_wik], writes=[r_bki], skip_self=True)
                        fw.op("act", lambda e, bk=bk, blk=blk: e.activation(out=Vx[:, blk, :, 0:128], in_=bk[:, 256:512].rearrange("p (k d) -> p k d", d=128), func=ACT.Copy),
                              reads=[r_bk], writes=[r_Vx])
                        fw.op("act", lambda e, bk=bk: e.activation(out=kf[:, 0:256], in_=bk[:, 0:256], func=ACT.Copy), reads=[r_bk], writes=[r_kf])
                        fw.op("act", lambda e, bki=bki: e.activation(out=kf[:, 256:320], in_=bki[:, 0:64], func=ACT.Copy), reads=[r_bki], writes=[r_kf])
                        rope(kf, r_kf, 2, 128, 16, 0)
                        rope(kf[:, 256:320], r_kf, 1, 64, 8, 16)
                        fw.op("dve", lambda e: e.tensor_copy(out=kf[:, 320:384], in_=kf[:, 256:320]), reads=[r_kf], writes=[r_kf])
                        fw.op("dve", lambda e: e.tensor_copy(out=kb[:, 0:384], in_=kf[:, 0:384]), reads=[r_kf], writes=[r_kb])
                        bkt, r_bkt = banks[6 + (ti % 2)]
                        bktb = bkt[:].bitcast(BF16)
                        for kv in range(2):
                            fw.op("pe", lambda e, kv=kv, bktb=bktb: e.transpose(out=bktb[:, kv * 128:(kv + 1) * 128], in_=kb[:, kv * 128:(kv + 1) * 128], identity=ident_b),
                                  reads=[r_kb, r_cstb], writes=[r_bkt], skip_self=True)
                        fw.op("pe", lambda e, bktb=bktb: e.transpose(out=bktb[:, 256:384], in_=kb[:, 256:384], identity=ident_b),
                              reads=[r_kb, r_cstb], writes=[r_bkt], skip_self=True)
                        fw.op("act", lambda e, bktb=bktb, blk=blk: e.activation(out=KT[:, :, blk * 128:(blk + 1) * 128], in_=bktb[:, 0:256].rearrange("p (k t) -> p k t", k=2), func=ACT.Copy),
                              reads=[r_bkt], writes=[r_KT])
                        pr = slice(0, 64) if blk < 32 else slice(64, 128)
                        fw.op("act", lambda e, bktb=bktb, blk=blk, pr=pr: e.activation(out=ikT[pr, (blk % 32) * 128:(blk % 32 + 1) * 128], in_=bktb[pr, 256:384], func=ACT.Copy),
                              reads=[r_bkt], writes=[r_ikT])

                fw.barrier()
                es2a.close()
                xT, r_xT = P.sb([128, 8, 128], BF16, "xTq")
                S, r_S = P.sb([128, 8192], F32, "S")
                mk, r_mk = P.sb([128, 8192], BF16, "mk")
                maskT, r_maskT = P.sb([128, 64, 128], BF16, "maskT")
                QT, r_QT = P.sb([128, 8, 128], BF16, "QT")
                iqT, r_iqT = P.sb([128, 4, 128], BF16, "iqT")
                qf, r_qf = P.sb([128, 1024], F32, "qf")
                wsg, r_wsg = P.sb([128, 16], F32, "wsg")
                bis, r_bis = P.sb([128, 8], F32, "bis")
                rl = [P.sb([128, 512], F32, "rl") for _ in range(2)]
                eL = [P.sb([128, 512], BF16, "eL") for _ in range(2)]
                PT = [P.sb([128, 512], BF16, "PT") for _ in range(2)]
                rden, r_rden = P.sb([128, 16], F32, "rden")
                at, r_at = P.sb([128, D], F32, "at")
                NITER = 24
                for it in range(16):
                    seg = 6 + it // 8
                    ti = it % 8
                    build_xT(P, seg, xT, r_xT, xt_bufs, ntile=1, src=lambda _t, seg=seg, ti=ti: x_seg[seg, ti * 128:(ti + 1) * 128, :])
                    trig_tables(seg * 8 + ti)
                    for cg in range(2):
                        bk, r_bk = banks[2 + cg]
                        for ch in range(8):
                            fw.op("pe", lambda e, ch=ch, cg=cg, bk=bk: e.matmul(bk[:], lhsT=xT[:, ch, :], rhs=wq[:, ch, cg * 512:(cg + 1) * 512], start=(ch == 0), stop=(ch == 7)),
                                  reads=[r_xT, r_wq], writes=[r_bk], skip_self=True)
                        fw.op("act", lambda e, cg=cg, bk=bk: e.activation(out=qf[:, cg * 512:(cg + 1) * 512], in_=bk[:], func=ACT.Copy), reads=[r_bk], writes=[r_qf])
                    rope(qf, r_qf, 8, 128, 16, 0)
                    fw.op("dve", lambda e: e.tensor_scalar(out=kb[:], in0=qf[:], scalar1=128.0 ** -0.5, scalar2=None, op0=ALU.mult), reads=[r_qf], writes=[r_kb])
                    bkt, r_bkt = banks[4]
                    bktb = bkt[:].bitcast(BF16)
                    for h in range(8):
                        fw.op("pe", lambda e, h=h, bktb=bktb: e.transpose(out=bktb[:, h * 128:(h + 1) * 128], in_=kb[:, h * 128:(h + 1) * 128], identity=ident_b),
                              reads=[r_kb, r_cstb], writes=[r_bkt], skip_self=True)
                    fw.op("act", lambda e, bktb=bktb: e.activation(out=QT[:], in_=bktb.rearrange("p (h t) -> p h t", h=8), func=ACT.Copy), reads=[r_bkt], writes=[r_QT])
                    bk, r_bk = banks[5]
                    for ch in range(8):
                        fw.op("pe", lambda e, ch=ch, bk=bk: e.matmul(bk[:, 0:260], lhsT=xT[:, ch, :], rhs=wiq[:, ch, :], start=(ch == 0), stop=(ch == 7)),
                              reads=[r_xT, r_wiq], writes=[r_bk], skip_self=True)
                    fw.op("act", lambda e, bk=bk: e.activation(out=qf[:, 0:260], in_=bk[:, 0:260], func=ACT.Copy), reads=[r_bk], writes=[r_qf])
                    rope(qf, r_qf, 4, 64, 8, 16)
                    fw.op("dve", lambda e: e.tensor_scalar(out=wsg[:, 0:4], in0=qf[:, 256:260], scalar1=0.0625, scalar2=None, op0=ALU.mult), reads=[r_qf], writes=[r_wsg])
                    fw.op("dve", lambda e: e.tensor_scalar(out=wsg[:, 12:16], in0=wsg[:, 0:4], scalar1=-1.0, scalar2=None, op0=ALU.mult), reads=[r_wsg], writes=[r_wsg])
                    fw.op("dve", lambda e: e.tensor_tensor(out=wsg[:, 4:8], in0=wsg[:, 0:4], in1=wsg[:, 12:16], op=ALU.max), reads=[r_wsg], writes=[r_wsg])
                    fw.op("dve", lambda e: e.tensor_scalar(out=wsg[:, 8:12], in0=wsg[:, 0:4], scalar1=0.0, scalar2=2.0, op0=ALU.is_ge, op1=ALU.mult), reads=[r_wsg], writes=[r_wsg])
                    fw.op("dve", lambda e: e.tensor_scalar(out=wsg[:, 8:12], in0=wsg[:, 8:12], scalar1=-1.0, scalar2=None, op0=ALU.add), reads=[r_wsg], writes=[r_wsg])
                    fw.op("dve", lambda e: e.tensor_tensor(out=kb[:, 0:512].rearrange("p (h r d) -> p h r d", h=4, r=2),
                                                           in0=qf[:, 0:256].rearrange("p (h d) -> p h d", d=64).unsqueeze(2).to_broadcast([128, 4, 2, 64]),
                                                           in1=wsg[:, 4:8].unsqueeze(2).unsqueeze(3).to_broadcast([128, 4, 2, 64]), op=ALU.mult), reads=[r_qf, r_wsg], writes=[r_kb])
                    bkt, r_bkt = banks[6]
                    bktb = bkt[:].bitcast(BF16)
                    for h in range(4):
                        fw.op("pe", lambda e, h=h, bktb=bktb: e.transpose(out=bktb[:, h * 128:(h + 1) * 128], in_=kb[:, h * 128:(h + 1) * 128], identity=ident_b),
                              reads=[r_kb, r_cstb], writes=[r_bkt], skip_self=True)
                    fw.op("act", lambda e, bktb=bktb: e.activation(out=iqT[:], in_=bktb[:, 0:512].rearrange("p (h t) -> p h t", h=4), func=ACT.Copy), reads=[r_bkt], writes=[r_iqT])
                    Ls = NCTX + 128 * (it + 1)
                    ngr = (Ls + 511) // 512
                    for sg in range(ngr):
                        wd = min(512, Ls - sg * 512)
                        pr = slice(0, 64) if sg < 8 else slice(64, 128)
                        c0 = (sg % 8) * 512
                        for h in range(4):
                            bk, r_bk = banks[2 + ((sg * 4 + h) % 2)]
                            rlb, r_rlb = rl[(sg * 4 + h) % 2]
                            fw.op("pe", lambda e, h=h, wd=wd, bk=bk, pr=pr, c0=c0: e.matmul(bk[:, 0:wd], lhsT=iqT[pr, h, :], rhs=ikT[pr, c0:c0 + wd], start=True, stop=True),
                                  reads=[r_iqT, r_ikT], writes=[r_bk], skip_self=True)
                            fw.op("act", lambda e, wd=wd, bk=bk, rlb=rlb: e.activation(out=rlb[:, 0:wd], in_=bk[:, 0:wd], func=ACT.Relu), reads=[r_bk], writes=[r_rlb])
                            if h == 0:
                                fw.op("dve", lambda e, sg=sg, wd=wd, rlb=rlb: e.tensor_scalar(out=S[:, sg * 512:sg * 512 + wd], in0=rlb[:, 0:wd], scalar1=wsg[:, 8:9], scalar2=None, op0=ALU.mult),
                                      reads=[r_rlb, r_wsg], writes=[r_S])
                            else:
                                fw.op("dve", lambda e, sg=sg, wd=wd, rlb=rlb, h=h: e.scalar_tensor_tensor(out=S[:, sg * 512:sg * 512 + wd], in0=rlb[:, 0:wd], scalar=wsg[:, 8 + h:9 + h],
                                                                                                         in1=S[:, sg * 512:sg * 512 + wd], op0=ALU.mult, op1=ALU.add),
                                      reads=[r_rlb, r_wsg, r_S], writes=[r_S])
                    fw.op("dve", lambda e, Ls=Ls: e.tensor_reduce(out=bis[:, 0:1], in_=S[:, 0:Ls], axis=AX.X, op=ALU.max), reads=[r_S], writes=[r_bis])
                    fw.op("dve", lambda e, Ls=Ls: e.tensor_reduce(out=bis[:, 1:2], in_=S[:, 0:Ls], axis=AX.X, op=ALU.min), reads=[r_S], writes=[r_bis])
                    for cs in range(6):
                        fw.op("pool", lambda e, cs=cs: e.tensor_scalar(out=S[:, cs * 1024:(cs + 1) * 1024], in0=S[:, cs * 1024:(cs + 1) * 1024], scalar1=penx[:, cs:cs + 1], scalar2=None, op0=ALU.add),
                              reads=[r_S, r_penx], writes=[r_S])
                    fw.op("dve", lambda e, Ls=Ls: e.tensor_tensor(out=S[:, Ls - 128:Ls], in0=S[:, Ls - 128:Ls], in1=tri_pen, op=ALU.add), reads=[r_S, r_cst], writes=[r_S])
                    fw.op("dve", lambda e: e.tensor_tensor(out=bis[:, 3:4], in0=bis[:, 0:1], in1=bis[:, 1:2], op=ALU.subtract), reads=[r_bis], writes=[r_bis])
                    fw.op("dve", lambda e: e.tensor_scalar(out=bis[:, 3:4], in0=bis[:, 3:4], scalar1=0.501, scalar2=1e-6, op0=ALU.mult, op1=ALU.add), reads=[r_bis], writes=[r_bis])
                    fw.op("dve", lambda e: e.scalar_tensor_tensor(out=bis[:, 2:3], in0=bis[:, 3:4], scalar=-0.002, in1=bis[:, 1:2], op0=ALU.mult, op1=ALU.add), reads=[r_bis], writes=[r_bis])
                    fw.op("dve", lambda e: e.tensor_scalar(out=bis[:, 2:3], in0=bis[:, 2:3], scalar1=-1e-6, scalar2=None, op0=ALU.add), reads=[r_bis], writes=[r_bis])
                    fw.op("dve", lambda e: e.tensor_scalar(out=bis[:, 3:4], in0=bis[:, 3:4], scalar1=2.0, scalar2=None, op0=ALU.mult), reads=[r_bis], writes=[r_bis])
                    for itn in range(NITER):
                        fw.op("dve", lambda e: e.tensor_scalar(out=bis[:, 3:4], in0=bis[:, 3:4], scalar1=0.5, scalar2=None, op0=ALU.mult), reads=[r_bis], writes=[r_bis])
                        fw.op("dve", lambda e: e.tensor_tensor(out=bis[:, 4:5], in0=bis[:, 2:3], in1=bis[:, 3:4], op=ALU.add), reads=[r_bis], writes=[r_bis])
                        fw.op("dve", lambda e, Ls=Ls: e.tensor_scalar(out=mk[:, 0:Ls], in0=S[:, 0:Ls], scalar1=bis[:, 4:5], scalar2=None, op0=ALU.is_ge, op1=ALU.add, accum_out=bis[:, 5:6]),
                              reads=[r_S, r_bis], writes=[r_mk, r_bis])
                        fw.op("dve", lambda e: e.tensor_single_scalar(out=bis[:, 6:7], in_=bis[:, 5:6], scalar=255.5, op=ALU.is_ge), reads=[r_bis], writes=[r_bis])
                        fw.op("dve", lambda e: e.scalar_tensor_tensor(out=bis[:, 2:3], in0=bis[:, 6:7], scalar=bis[:, 3:4], in1=bis[:, 2:3], op0=ALU.mult, op1=ALU.add), reads=[r_bis], writes=[r_bis])
                    fw.op("dve", lambda e, Ls=Ls: e.tensor_scalar(out=mk[:, 0:Ls], in0=S[:, 0:Ls], scalar1=bis[:, 2:3], scalar2=None, op0=ALU.is_ge), reads=[r_S, r_bis], writes=[r_mk])
                    nb = Ls // 128
                    for b0 in range(0, nb, 8):
                        nn = min(8, nb - b0)
                        bkt, r_bkt = banks[4 + ((b0 // 8) % 2)]
                        bktb = bkt[:].bitcast(BF16)
                        for bb in range(nn):
                            fw.op("pe", lambda e, bb=bb, b0=b0, bktb=bktb: e.transpose(out=bktb[:, bb * 128:(bb + 1) * 128], in_=mk[:, (b0 + bb) * 128:(b0 + bb + 1) * 128], identity=ident_b),
                                  reads=[r_mk, r_cstb], writes=[r_bkt], skip_self=True)
                        fw.op("act", lambda e, b0=b0, nn=nn, bktb=bktb: e.activation(out=maskT[:, b0:b0 + nn, :], in_=bktb[:, 0:nn * 128].rearrange("p (b t) -> p b t", t=128), func=ACT.Copy),
                              reads=[r_bkt], writes=[r_maskT])
                    nblk = nb
                    def oslot(h):
                        bko, r_bko = banks[h // 3]
                        return bko[:, (h % 3) * 129:(h % 3) * 129 + 129], r_bko
                    cnt = 0
                    for sb in range(nblk):
                        for hq in range(2):
                            bkq, r_bkq = banks[6 + (cnt % 2)]
                            eLb, r_eLb = eL[cnt % 2]
                            PTb, r_PTb = PT[cnt % 2]
                            cnt += 1
                            for j in range(4):
                                h = hq * 4 + j
                                fw.op("pe", lambda e, h=h, j=j, sb=sb, hq=hq, bkq=bkq: e.matmul(bkq[:, j * 128:(j + 1) * 128], lhsT=KT[:, hq, sb * 128:(sb + 1) * 128], rhs=QT[:, h, :], start=True, stop=True),
                                      reads=[r_KT, r_QT], writes=[r_bkq], skip_self=True)
                            fw.op("act", lambda e, bkq=bkq, eLb=eLb: e.activation(out=eLb[:], in_=bkq[:], func=ACT.Exp), reads=[r_bkq], writes=[r_eLb])
                            fw.op("dve", lambda e, eLb=eLb, PTb=PTb, sb=sb: e.tensor_tensor(out=PTb[:].rearrange("p (j t) -> p j t", j=4), in0=eLb[:].rearrange("p (j t) -> p j t", j=4),
                                                                                          in1=maskT[:, sb, :].unsqueeze(1).to_broadcast([128, 4, 128]), op=ALU.mult),
                                  reads=[r_eLb, r_maskT], writes=[r_PTb])
                            for j in range(4):
                                h = hq * 4 + j
                                oap, r_o = oslot(h)
                                fw.op("pe", lambda e, oap=oap, PTb=PTb, j=j, sb=sb, hq=hq: e.matmul(oap, lhsT=PTb[:, j * 128:(j + 1) * 128], rhs=Vx[:, sb, hq, :],
                                                                                                start=(sb == 0), stop=(sb == nblk - 1)),
                                      reads=[r_PTb, r_Vx], writes=[r_o], skip_self=True)
                    for h in range(8):
                        oap, r_o = oslot(h)
                        fw.op("dve", lambda e, oap=oap, h=h: e.reciprocal(out=rden[:, h:h + 1], in_=oap[:, 128:129]), reads=[r_o], writes=[r_rden])
                        fw.op("dve", lambda e, oap=oap, h=h: e.tensor_scalar(out=at[:, h * 128:(h + 1) * 128], in0=oap[:, 0:128], scalar1=rden[:, h:h + 1], scalar2=None, op0=ALU.mult),
                              reads=[r_o, r_rden], writes=[r_at])
                    t0 = it * 128
                    fw.dma("sp", attn_d[t0:t0 + 128, :], at[:], reads=[r_at])
                    if dbg:
                        fw.dma("sp", dbg_attn[t0:t0 + 128, :], at[:], reads=[r_at])
                fw.barrier()

        if 3 in phases:
            h2_d = nc.dram_tensor("h2_d", [TOWN, D], F32, kind="Internal").ap()
            with ExitStack() as es3:
                P = Pool(nc, es3)
                wstg = P.sb([128, 4096], F32, "wstg")
                wgate, r_wgate = P.sb([128, 8, 2048], BF16, "wgate")
                wa, r_wa = P.sb([128, 8, 1024], BF16, "wa")
                wo, r_wo = P.sb([128, 8, 1024], BF16, "wo")
                wcq, r_wcq = P.sb([128, 8, 1024], BF16, "wcq")
                wco, r_wco = P.sb([128, 8, 1024], BF16, "wco")
                gc_, r_gc = P.sb([128, 8], F32, "gc")
                gmm, r_gmm = P.sb([128, 8], F32, "gmm")
                fw.dma("sp", gc_[:], g_cross, writes=[r_gc])
                fw.dma("sp", gmm[:], g_mem, writes=[r_gmm])
                load_weight(wstg, wgate, r_wgate, w_in_r, OGATE, 2048, gm, r_gm)
                load_weight(wstg, wa, r_wa, w_rows(w_attn_branch), 0, 1024, None, None)
                load_weight(wstg, wo, r_wo, w_rows(w_out), 0, 1024, None, None)
                load_weight(wstg, wcq, r_wcq, w_rows(w_cross_q), 0, 1024, gc_, r_gc)
                load_weight(wstg, wco, r_wco, w_rows(w_cross_out), 0, 1024, None, None)
                xT, r_xT = P.sb([128, 8, 256], BF16, "xT3")
                xt_bufs = []
                xt, r_xt = P.sb([128, D], F32, "xt")
                xb, r_xb = P.sb([128, D], BF16, "xb")
                ss, r_ss = P.sb([128, 4], F32, "ss")
                xt_bufs = [(xt, r_xt, xb, r_xb, ss, r_ss)] * 2
                KcT, r_KcT = P.sb([128, 8, 256], BF16, "KcT")
                Vc, r_Vc = P.sb([128, 2, 1024], BF16, "Vc")
                with ExitStack() as es3a:
                    Pa = Pool(nc, es3a)
                    wckv, r_wckv = Pa.sb([128, 8, 2048], BF16, "wckv")
                    load_weight(wstg, wckv, r_wckv, w_rows(w_cross_kv), 0, 2048, gmm, r_gmm)
                    build_xT(P, 0, xT, r_xT, xt_bufs, ntile=2, src=lambda ti: mem[ti * 128:(ti + 1) * 128, :])
                    for blk in range(8):
                        bk, r_bk = banks[blk % 2]
                        for ch in range(8):
                            fw.op("pe", lambda e, ch=ch, blk=blk, bk=bk: e.matmul(bk[:, 0:256], lhsT=wckv[:, ch, blk * 128:(blk + 1) * 128], rhs=xT[:, ch, :], start=(ch == 0), stop=(ch == 7)),
                                  reads=[r_wckv, r_xT], writes=[r_bk], skip_self=True)
                        fw.op("act", lambda e, blk=blk, bk=bk: e.activation(out=KcT[:, blk, :], in_=bk[:, 0:256], func=ACT.Copy), reads=[r_bk], writes=[r_KcT])
                    for mc in range(2):
                        for cg in range(2):
                            bk, r_bk = banks[2 + cg]
                            for ch in range(8):
                                fw.op("pe", lambda e, ch=ch, mc=mc, cg=cg, bk=bk: e.matmul(bk[:], lhsT=xT[:, ch, mc * 128:(mc + 1) * 128], rhs=wckv[:, ch, 1024 + cg * 512:1024 + (cg + 1) * 512], start=(ch == 0), stop=(ch == 7)),
                                      reads=[r_wckv, r_xT], writes=[r_bk], skip_self=True)
                            fw.op("act", lambda e, mc=mc, cg=cg, bk=bk: e.activation(out=Vc[:, mc, cg * 512:(cg + 1) * 512], in_=bk[:], func=ACT.Copy), reads=[r_bk], writes=[r_Vc])
                    fw.barrier()
                gts, r_gts = P.sb([128, 2048], F32, "gts")
                atl, r_atl = P.sb([128, D], F32, "atl")
                spl, r_spl = P.sb([128, D], F32, "spl")
                bfb, r_bfb = P.sb([128, D], BF16, "bfb")
                tT, r_tT = P.sb([128, 8, 128], BF16, "tT")
                mg, r_mg = P.sb([128, D], F32, "mg")
                h1, r_h1 = P.sb([128, D], F32, "h1")
                sq, r_sq = P.sb([128, 8], F32, "sq")
                qcT, r_qcT = P.sb([128, 8, 128], BF16, "qcT")
                pc, r_pc = P.sb([128, 4, 256], F32, "pc")
                pcb, r_pcb = P.sb([128, 4, 256], BF16, "pcb")
                pT, r_pT = P.sb([128, 8, 128], BF16, "pT")
                oT, r_oT = P.sb([128, 8, 128], BF16, "oT")
                sm, r_sm = P.sb([128, 16], F32, "sm")
                junk, r_junk = P.sb([128, D], F32, "junk3")

                def to_T(src_bf, r_src, dstT, r_dst, bank_i):
                    bkt, r_bkt = banks[bank_i]
                    bktb = bkt[:].bitcast(BF16)
                    for ch in range(8):
                        fw.op("pe", lambda e, ch=ch: e.transpose(out=bktb[:, ch * 128:(ch + 1) * 128], in_=src_bf[:, ch * 128:(ch + 1) * 128], identity=ident_b),
                              reads=[r_src, r_cstb], writes=[r_bkt], skip_self=True)
                    fw.op("act", lambda e: e.activation(out=dstT[:], in_=bktb.rearrange("p (c t) -> p c t", c=8), func=ACT.Copy), reads=[r_bkt], writes=[r_dst])

                def rstd_of(src, r_src, col, n=D):
                    fw.op("act", lambda e: e.activation(out=junk[:], in_=src[:], func=ACT.Square, accum_out=sq[:, col:col + 1]), reads=[r_src], writes=[r_junk, r_sq])
                    fw.op("dve", lambda e: e.tensor_scalar(out=sq[:, col + 1:col + 2], in0=sq[:, col:col + 1], scalar1=1.0 / n, scalar2=EPS, op0=ALU.mult, op1=ALU.add), reads=[r_sq], writes=[r_sq])
                    fw.op("act", lambda e: e.activation(out=sq[:, col + 2:col + 3], in_=sq[:, col + 1:col + 2], func=ACT.Ln), reads=[r_sq], writes=[r_sq])
                    fw.op("act", lambda e: e.activation(out=sq[:, col + 3:col + 4], in_=sq[:, col + 2:col + 3], func=ACT.Exp, scale=-0.5), reads=[r_sq], writes=[r_sq])
                    return sq[:, col + 3:col + 4]

                for it in range(16):
                    seg, ti = 6 + it // 8, it % 8
                    t0 = it * 128
                    build_xT(P, seg, xT[:, :, 0:128], r_xT, xt_bufs, ntile=1, src=lambda _t, seg=seg, ti=ti: x_seg[seg, ti * 128:(ti + 1) * 128, :])
                    for cg in range(4):
                        bk, r_bk = banks[2 + (cg % 2)]
                        for ch in range(8):
                            fw.op("pe", lambda e, ch=ch, cg=cg, bk=bk: e.matmul(bk[:], lhsT=xT[:, ch, 0:128], rhs=wgate[:, ch, cg * 512:(cg + 1) * 512], start=(ch == 0), stop=(ch == 7)),
                                  reads=[r_xT, r_wgate], writes=[r_bk], skip_self=True)
                        fw.op("act", lambda e, cg=cg, bk=bk: e.activation(out=gts[:, cg * 512:(cg + 1) * 512], in_=bk[:], func=ACT.Sigmoid), reads=[r_bk], writes=[r_gts])
                    fw.dma("sp", atl[:], attn_d[t0:t0 + 128, :], writes=[r_atl])
                    fw.dma("sp", spl[:], sproj_d[t0:t0 + 128, :], writes=[r_spl])
                    fw.op("dve", lambda e: e.tensor_copy(out=bfb[:], in_=atl[:]), reads=[r_atl], writes=[r_bfb])
                    to_T(bfb, r_bfb, tT, r_tT, 4)
                    for cg in range(2):
                        bk, r_bk = banks[2 + cg]
                        for ch in range(8):
                            fw.op("pe", lambda e, ch=ch, cg=cg, bk=bk: e.matmul(bk[:], lhsT=tT[:, ch, :], rhs=wa[:, ch, cg * 512:(cg + 1) * 512], start=(ch == 0), stop=(ch == 7)),
                                  reads=[r_tT, r_wa], writes=[r_bk], skip_self=True)
                        fw.op("dve", lambda e, cg=cg, bk=bk: e.tensor_tensor(out=mg[:, cg * 512:(cg + 1) * 512], in0=bk[:], in1=gts[:, cg * 512:(cg + 1) * 512], op=ALU.mult),
                              reads=[r_bk, r_gts], writes=[r_mg])
                    fw.op("dve", lambda e: e.tensor_tensor(out=spl[:], in0=spl[:], in1=gts[:, 1024:2048], op=ALU.mult), reads=[r_spl, r_gts], writes=[r_spl])
                    fw.op("dve", lambda e: e.tensor_tensor(out=mg[:], in0=mg[:], in1=spl[:], op=ALU.add), reads=[r_mg, r_spl], writes=[r_mg])
                    fw.op("dve", lambda e: e.tensor_copy(out=bfb[:], in_=mg[:]), reads=[r_mg], writes=[r_bfb])
                    to_T(bfb, r_bfb, tT, r_tT, 5)
                    xt_cur, r_xt_cur = xt_bufs[0][0], xt_bufs[0][1]
                    for cg in range(2):
                        bk, r_bk = banks[2 + cg]
                        for ch in range(8):
                            fw.op("pe", lambda e, ch=ch, cg=cg, bk=bk: e.matmul(bk[:], lhsT=tT[:, ch, :], rhs=wo[:, ch, cg * 512:(cg + 1) * 512], start=(ch == 0), stop=(ch == 7)),
                                  reads=[r_tT, r_wo], writes=[r_bk], skip_self=True)
                        fw.op("dve", lambda e, cg=cg, bk=bk: e.tensor_tensor(out=h1[:, cg * 512:(cg + 1) * 512], in0=bk[:], in1=xt_cur[:, cg * 512:(cg + 1) * 512], op=ALU.add),
                              reads=[r_bk, r_xt_cur], writes=[r_h1])
                    rs = rstd_of(h1, r_h1, 0)
                    fw.op("dve", lambda e: e.tensor_scalar(out=bfb[:], in0=h1[:], scalar1=rs, scalar2=None, op0=ALU.mult), reads=[r_h1, r_sq], writes=[r_bfb])
                    to_T(bfb, r_bfb, tT, r_tT, 4)
                    for half in range(2):
                        bk, r_bk = banks[2 + half]
                        for b4 in range(4):
                            blk = half * 4 + b4
                            for ch in range(8):
                                fw.op("pe", lambda e, ch=ch, blk=blk, b4=b4, bk=bk: e.matmul(bk[:, b4 * 128:(b4 + 1) * 128], lhsT=wcq[:, ch, blk * 128:(blk + 1) * 128], rhs=tT[:, ch, :], start=(ch == 0), stop=(ch == 7)),
                                      reads=[r_wcq, r_tT], writes=[r_bk], skip_self=True)
                        fw.op("act", lambda e, half=half, bk=bk: e.activation(out=qcT[:, half * 4:(half + 1) * 4, :], in_=bk[:].rearrange("p (b t) -> p b t", b=4), func=ACT.Copy, scale=1.0 / 16.0),
                              reads=[r_bk], writes=[r_qcT])
                    for hp in range(2):
                        bk, r_bk = banks[2 + hp]
                        for j in range(2):
                            h = hp * 2 + j
                            for c in range(2):
                                fw.op("pe", lambda e, h=h, j=j, c=c, bk=bk: e.matmul(bk[:, j * 256:(j + 1) * 256], lhsT=qcT[:, 2 * h + c, :], rhs=KcT[:, 2 * h + c, :], start=(c == 0), stop=(c == 1)),
                                      reads=[r_qcT, r_KcT], writes=[r_bk], skip_self=True)
                        fw.op("act", lambda e, hp=hp, bk=bk: e.activation(out=pc[:, hp * 2:(hp + 1) * 2, :], in_=bk[:].rearrange("p (j m) -> p j m", j=2), func=ACT.Copy), reads=[r_bk], writes=[r_pc])
                    fw.op("dve", lambda e: e.tensor_reduce(out=sm[:, 0:4], in_=pc[:], axis=AX.X, op=ALU.max), reads=[r_pc], writes=[r_sm])
                    fw.op("dve", lambda e: e.tensor_scalar(out=sm[:, 4:8], in0=sm[:, 0:4], scalar1=-1.0, scalar2=None, op0=ALU.mult), reads=[r_sm], writes=[r_sm])
                    for h in range(4):
                        fw.op("act", lambda e, h=h: e.activation(out=pc[:, h, :], in_=pc[:, h, :], func=ACT.Exp, bias=sm[:, 4 + h:5 + h], accum_out=sm[:, 8 + h:9 + h]), reads=[r_pc, r_sm], writes=[r_pc, r_sm])
                    fw.op("dve", lambda e: e.reciprocal(out=sm[:, 12:16], in_=sm[:, 8:12]), reads=[r_sm], writes=[r_sm])
                    fw.op("dve", lambda e: e.tensor_tensor(out=pcb[:], in0=pc[:], in1=sm[:, 12:16].unsqueeze(2).to_broadcast([128, 4, 256]), op=ALU.mult), reads=[r_pc, r_sm], writes=[r_pcb])
                    bkt, r_bkt = banks[4]
                    bktb = bkt[:].bitcast(BF16)
                    for h in range(4):
                        for mc in range(2):
                            fw.op("pe", lambda e, h=h, mc=mc: e.transpose(out=bktb[:, (2 * h + mc) * 128:(2 * h + mc + 1) * 128], in_=pcb[:, h, mc * 128:(mc + 1) * 128], identity=ident_b),
                                  reads=[r_pcb, r_cstb], writes=[r_bkt], skip_self=True)
                    fw.op("act", lambda e: e.activation(out=pT[:], in_=bktb.rearrange("p (c t) -> p c t", c=8), func=ACT.Copy), reads=[r_bkt], writes=[r_pT])
                    for half in range(2):
                        bk, r_bk = banks[2 + half]
                        for b4 in range(4):
                            blk = half * 4 + b4
                            h, dc = blk // 2, blk % 2
                            for mc in range(2):
                                fw.op("pe", lambda e, h=h, dc=dc, mc=mc, b4=b4, bk=bk: e.matmul(bk[:, b4 * 128:(b4 + 1) * 128], lhsT=Vc[:, mc, h * 256 + dc * 128:h * 256 + (dc + 1) * 128], rhs=pT[:, 2 * h + mc, :],
                                                                                        start=(mc == 0), stop=(mc == 1)),
                                      reads=[r_Vc, r_pT], writes=[r_bk], skip_self=True)
                        fw.op("act", lambda e, half=half, bk=bk: e.activation(out=oT[:, half * 4:(half + 1) * 4, :], in_=bk[:].rearrange("p (b t) -> p b t", b=4), func=ACT.Copy), reads=[r_bk], writes=[r_oT])
                    for cg in range(2):
                        bk, r_bk = banks[2 + cg]
                        for ch in range(8):
                            fw.op("pe", lambda e, ch=ch, cg=cg, bk=bk: e.matmul(bk[:], lhsT=oT[:, ch, :], rhs=wco[:, ch, cg * 512:(cg + 1) * 512], start=(ch == 0), stop=(ch == 7)),
                                  reads=[r_oT, r_wco], writes=[r_bk], skip_self=True)
                        fw.op("dve", lambda e, cg=cg, bk=bk: e.tensor_tensor(out=mg[:, cg * 512:(cg + 1) * 512], in0=bk[:], in1=h1[:, cg * 512:(cg + 1) * 512], op=ALU.add),
                              reads=[r_bk, r_h1], writes=[r_mg])
                    fw.dma("sp", h2_d[t0:t0 + 128, :], mg[:], reads=[r_mg])
                fw.barrier()

            with ExitStack() as es4:
                P = Pool(nc, es4)
                wstg = P.sb([128, 4096], F32, "wstg")
                wpq, r_wpq = P.sb([128, 8, 2048], BF16, "wpq")
                gfc, r_gfc = P.sb([128, 8], F32, "gfc")
                fw.dma("sp", gfc[:], g_ffn_c, writes=[r_gfc])
                load_weight(wstg, wpq, r_wpq, w_rows(w_peer_q), 0, 2048, gfc, r_gfc)
                gff, r_gff = P.sb([128, D], F32, "gff")
                gfin, r_gfin = P.sb([128, D], F32, "gfin")
                fw.dma("sp", gff[:], g_ffn, writes=[r_gff])
                fw.dma("sp", gfin[:], g_final, writes=[r_gfin])
                skT, r_skT = P.sb([128, 16, 128], BF16, "skT")
                skf, r_skf = P.sb([128, 16, 128], F32, "skf")
                skb, r_skb = P.sb([128, 16, 128], BF16, "skb")
                fw.dma("sp", skf[:], sub_keys.rearrange("b k d -> k b d"), writes=[r_skf])
                fw.op("dve", lambda e: e.tensor_copy(out=skb[:], in_=skf[:]), reads=[r_skf], writes=[r_skb])
                for half in range(2):
                    bkt, r_bkt = banks[half]
                    bktb = bkt[:].bitcast(BF16)
                    for i in range(8):
                        fw.op("pe", lambda e, i=i, half=half, bktb=bktb: e.transpose(out=bktb[:, i * 128:(i + 1) * 128], in_=skb[:, half * 8 + i, :], identity=ident_b),
                              reads=[r_skb, r_cstb], writes=[r_bkt], skip_self=True)
                    fw.op("act", lambda e, half=half, bktb=bktb: e.activation(out=skT[:, half * 8:(half + 1) * 8, :], in_=bktb.rearrange("p (c t) -> p c t", c=8), func=ACT.Copy), reads=[r_bkt], writes=[r_skT])
                iota16, r_iota = P.sb([128, 16], F32, "iota16")
                for i in range(16):
                    fw.op("pool", lambda e, i=i: e.memset(iota16[:, i:i + 1], float(i)), writes=[r_iota])
                h2, r_h2 = P.sb([128, D], F32, "h2")
                a3, r_a3 = P.sb([128, D], F32, "a3")
                bfb, r_bfb = P.sb([128, D], BF16, "bfb4")
                tT, r_tT = P.sb([128, 8, 128], BF16, "tT4")
                qpT, r_qpT = P.sb([128, 16, 128], BF16, "qpT")
                sc, r_sc = P.sb([128, 16, 128], F32, "sc")
                scw, r_scw = P.sb([128, 16, 128], F32, "scw")
                tops, r_tops = P.sb([128, 16, 16], F32, "tops")
                topi, r_topi = P.sb([128, 16, 16], U32, "topi")
                topf, r_topf = P.sb([128, 16, 16], F32, "topf")
                cand, r_cand = P.sb([128, 8, 256], F32, "cand")
                candw, r_candw = P.sb([128, 8, 256], F32, "candw")
                selv, r_selv = P.sb([128, 8, 16], F32, "selv")
                seli, r_seli = P.sb([128, 8, 16], U32, "seli")
                selt, r_selt = P.sb([128, 8, 16], U32, "selt")
                sif, r_sif = P.sb([128, 8, 16], F32, "sif")
                sjf, r_sjf = P.sb([128, 8, 16], F32, "sjf")
                oh, r_oh = P.sb([128, 8, 16, 16], F32, "oh")
                eaf, r_eaf = P.sb([128, 8, 16], F32, "eaf")
                ebf, r_ebf = P.sb([128, 8, 16], F32, "ebf")
                eidx, r_eidx = P.sb([128, 128], I32, "eidx")
                gw, r_gw = P.sb([128, 8, 16], F32, "gw")
                g8, r_g8 = P.sb([128, 16], F32, "g8")
                dots, r_dots = P.sb([128, 128], F32, "dots")
                wts, r_wts = P.sb([128, 128], F32, "wts")
                Ub = [P.sb([128, D], F32, "Ub") for _ in range(3)]
                prod, r_prod = P.sb([128, D], F32, "prod")
                acc, r_acc = P.sb([128, D], F32, "acc")
                sq, r_sq = P.sb([128, 8], F32, "sq4")
                junk, r_junk = P.sb([128, D], F32, "junk4")
                for it in range(16):
                    t0 = it * 128
                    fw.dma("sp", h2[:], h2_d[t0:t0 + 128, :], writes=[r_h2])
                    fw.op("act", lambda e: e.activation(out=junk[:], in_=h2[:], func=ACT.Square, accum_out=sq[:, 0:1]), reads=[r_h2], writes=[r_junk, r_sq])
                    fw.op("dve", lambda e: e.tensor_scalar(out=sq[:, 1:2], in0=sq[:, 0:1], scalar1=1.0 / D, scalar2=EPS, op0=ALU.mult, op1=ALU.add), reads=[r_sq], writes=[r_sq])
                    fw.op("act", lambda e: e.activation(out=sq[:, 2:3], in_=sq[:, 1:2], func=ACT.Ln), reads=[r_sq], writes=[r_sq])
                    fw.op("act", lambda e: e.activation(out=sq[:, 3:4], in_=sq[:, 2:3], func=ACT.Exp, scale=-0.5), reads=[r_sq], writes=[r_sq])
                    fw.op("dve", lambda e: e.tensor_scalar(out=bfb[:], in0=h2[:], scalar1=sq[:, 3:4], scalar2=None, op0=ALU.mult), reads=[r_h2, r_sq], writes=[r_bfb])
                    fw.op("dve", lambda e: e.scalar_tensor_tensor(out=a3[:], in0=h2[:], scalar=sq[:, 3:4], in1=gff[:], op0=ALU.mult, op1=ALU.mult), reads=[r_h2, r_sq, r_gff], writes=[r_a3])
                    bkt, r_bkt = banks[0]
                    bktb = bkt[:].bitcast(BF16)
                    for ch in range(8):
                        fw.op("pe", lambda e, ch=ch: e.transpose(out=bktb[:, ch * 128:(ch + 1) * 128], in_=bfb[:, ch * 128:(ch + 1) * 128], identity=ident_b),
                              reads=[r_bfb, r_cstb], writes=[r_bkt], skip_self=True)
                    fw.op("act", lambda e: e.activation(out=tT[:], in_=bktb.rearrange("p (c t) -> p c t", c=8), func=ACT.Copy), reads=[r_bkt], writes=[r_tT])
                    for q4 in range(4):
                        bk, r_bk = banks[1 + (q4 % 2)]
                        for b4 in range(4):
                            blk = q4 * 4 + b4
                            for ch in range(8):
                                fw.op("pe", lambda e, ch=ch, blk=blk, b4=b4, bk=bk: e.matmul(bk[:, b4 * 128:(b4 + 1) * 128], lhsT=wpq[:, ch, blk * 128:(blk + 1) * 128], rhs=tT[:, ch, :], start=(ch == 0), stop=(ch == 7)),
                                      reads=[r_wpq, r_tT], writes=[r_bk], skip_self=True)
                        fw.op("act", lambda e, q4=q4, bk=bk: e.activation(out=qpT[:, q4 * 4:(q4 + 1) * 4, :], in_=bk[:].rearrange("p (b t) -> p b t", b=4), func=ACT.Copy), reads=[r_bk], writes=[r_qpT])
                    for q4 in range(4):
                        bk, r_bk = banks[3 + (q4 % 2)]
                        for b4 in range(4):
                            blk = q4 * 4 + b4
                            fw.op("pe", lambda e, blk=blk, b4=b4, bk=bk: e.matmul(bk[:, b4 * 128:(b4 + 1) * 128], lhsT=qpT[:, blk, :], rhs=skT[:, blk, :], start=True, stop=True),
                                  reads=[r_qpT, r_skT], writes=[r_bk], skip_self=True)
                        fw.op("act", lambda e, q4=q4, bk=bk: e.activation(out=sc[:, q4 * 4:(q4 + 1) * 4, :], in_=bk[:].rearrange("p (b t) -> p b t", b=4), func=ACT.Copy), reads=[r_bk], writes=[r_sc])
                    for blk in range(16):
                        fw.op("dve", lambda e, blk=blk: e.max(out=tops[:, blk, 0:8], in_=sc[:, blk, :]), reads=[r_sc], writes=[r_tops])
                        fw.op("dve", lambda e, blk=blk: e.max_index(out=topi[:, blk, 0:8], in_max=tops[:, blk, 0:8], in_values=sc[:, blk, :]), reads=[r_sc, r_tops], writes=[r_topi])
                        fw.op("dve", lambda e, blk=blk: e.match_replace(out=scw[:, blk, :], in_to_replace=tops[:, blk, 0:8], in_values=sc[:, blk, :], imm_value=NEG), reads=[r_sc, r_tops], writes=[r_scw])
                        fw.op("dve", lambda e, blk=blk: e.max(out=tops[:, blk, 8:16], in_=scw[:, blk, :]), reads=[r_scw], writes=[r_tops])
                        fw.op("dve", lambda e, blk=blk: e.max_index(out=topi[:, blk, 8:16], in_max=tops[:, blk, 8:16], in_values=scw[:, blk, :]), reads=[r_scw, r_tops], writes=[r_topi])
                    fw.op("dve", lambda e: e.tensor_copy(out=topf[:], in_=topi[:]), reads=[r_topi], writes=[r_topf])
                    tv = tops[:].rearrange("p (h q) k -> p h q k", q=2)
                    fw.op("dve", lambda e: e.tensor_tensor(out=cand[:].rearrange("p h (i j) -> p h i j", j=16), in0=tv[:, :, 0, :].unsqueeze(3).to_broadcast([128, 8, 16, 16]),
                                                           in1=tv[:, :, 1, :].unsqueeze(2).to_broadcast([128, 8, 16, 16]), op=ALU.add), reads=[r_tops], writes=[r_cand])
                    for h in range(8):
                        fw.op("dve", lambda e, h=h: e.max(out=selv[:, h, 0:8], in_=cand[:, h, :]), reads=[r_cand], writes=[r_selv])
                        fw.op("dve", lambda e, h=h: e.max_index(out=seli[:, h, 0:8], in_max=selv[:, h, 0:8], in_values=cand[:, h, :]), reads=[r_cand, r_selv], writes=[r_seli])
                        fw.op("dve", lambda e, h=h: e.match_replace(out=candw[:, h, :], in_to_replace=selv[:, h, 0:8], in_values=cand[:, h, :], imm_value=NEG), reads=[r_cand, r_selv], writes=[r_candw])
                        fw.op("dve", lambda e, h=h: e.max(out=selv[:, h, 8:16], in_=candw[:, h, :]), reads=[r_candw], writes=[r_selv])
                        fw.op("dve", lambda e, h=h: e.max_index(out=seli[:, h, 8:16], in_max=selv[:, h, 8:16], in_values=candw[:, h, :]), reads=[r_candw, r_selv], writes=[r_seli])
                    fw.op("dve", lambda e: e.tensor_single_scalar(out=selt[:], in_=seli[:], scalar=4, op=ALU.logical_shift_right), reads=[r_seli], writes=[r_selt])
                    fw.op("dve", lambda e: e.tensor_copy(out=sif[:], in_=selt[:]), reads=[r_selt], writes=[r_sif])
                    fw.op("dve", lambda e: e.tensor_single_scalar(out=selt[:], in_=seli[:], scalar=15, op=ALU.bitwise_and), reads=[r_seli], writes=[r_selt])
                    fw.op("dve", lambda e: e.tensor_copy(out=sjf[:], in_=selt[:]), reads=[r_selt], writes=[r_sjf])
                    tf = topf[:].rearrange("p (h q) k -> p h q k", q=2)
                    for (srcf, r_srcf, q, dst, r_dst) in ((sif, r_sif, 0, eaf, r_eaf), (sjf, r_sjf, 1, ebf, r_ebf)):
                        fw.op("dve", lambda e, srcf=srcf: e.tensor_tensor(out=oh[:], in0=srcf[:].unsqueeze(3).to_broadcast([128, 8, 16, 16]),
                                                                         in1=iota16[:].unsqueeze(1).unsqueeze(2).to_broadcast([128, 8, 16, 16]), op=ALU.is_equal),
                              reads=[r_srcf, r_iota], writes=[r_oh])
                        fw.op("dve", lambda e, q=q: e.tensor_tensor(out=oh[:], in0=oh[:], in1=tf[:, :, q, :].unsqueeze(2).to_broadcast([128, 8, 16, 16]), op=ALU.mult),
                              reads=[r_oh, r_topf], writes=[r_oh])
                        fw.op("dve", lambda e, dst=dst: e.tensor_reduce(out=dst[:], in_=oh[:], axis=AX.X, op=ALU.add), reads=[r_oh], writes=[r_dst])
                    fw.op("dve", lambda e: e.scalar_tensor_tensor(out=eaf[:], in0=eaf[:], scalar=128.0, in1=ebf[:], op0=ALU.mult, op1=ALU.add), reads=[r_eaf, r_ebf], writes=[r_eaf])
                    fw.op("dve", lambda e: e.tensor_copy(out=eidx[:], in_=eaf[:].rearrange("p h k -> p (h k)")), reads=[r_eaf], writes=[r_eidx])
                    fw.op("dve", lambda e: e.tensor_tensor(out=gw[:], in0=selv[:], in1=selv[:, :, 0:1].to_broadcast([128, 8, 16]), op=ALU.subtract), reads=[r_selv], writes=[r_gw])
                    fw.op("act", lambda e: e.activation(out=gw[:], in_=gw[:], func=ACT.Exp), reads=[r_gw], writes=[r_gw])
                    fw.op("dve", lambda e: e.tensor_reduce(out=g8[:, 0:8], in_=gw[:], axis=AX.X, op=ALU.add), reads=[r_gw], writes=[r_g8])
                    fw.op("dve", lambda e: e.reciprocal(out=g8[:, 8:16], in_=g8[:, 0:8]), reads=[r_g8], writes=[r_g8])
                    fw.op("dve", lambda e: e.tensor_tensor(out=gw[:], in0=gw[:], in1=g8[:, 8:16].unsqueeze(2).to_broadcast([128, 8, 16]), op=ALU.mult), reads=[r_gw, r_g8], writes=[r_gw])
                    for sl in range(128):
                        ub, r_ub = Ub[sl % 3]
                        fw.dma("pool", ub[:], peer_u, reads=[r_eidx], writes=[r_ub], indirect=bass.IndirectOffsetOnAxis(ap=eidx[:, sl:sl + 1], axis=0))
                        fw.op("dve", lambda e, ub=ub: e.tensor_tensor(out=prod[:], in0=ub[:], in1=a3[:], op=ALU.mult), reads=[r_ub, r_a3], writes=[r_prod])
                        fw.op("act", lambda e, sl=sl: e.activation(out=junk[:], in_=prod[:], func=ACT.Copy, accum_out=dots[:, sl:sl + 1]), reads=[r_prod], writes=[r_junk, r_dots])
                    fw.op("act", lambda e: e.activation(out=wts[:], in_=dots[:], func=ACT.Gelu), reads=[r_dots], writes=[r_wts])
                    fw.op("dve", lambda e: e.tensor_tensor(out=wts[:], in0=wts[:], in1=gw[:].rearrange("p h k -> p (h k)"), op=ALU.mult), reads=[r_wts, r_gw], writes=[r_wts])
                    for sl in range(128):
                        ub, r_ub = Ub[sl % 3]
                        fw.dma("pool", ub[:], peer_v, reads=[r_eidx], writes=[r_ub], indirect=bass.IndirectOffsetOnAxis(ap=eidx[:, sl:sl + 1], axis=0))
                        if sl == 0:
                            fw.op("dve", lambda e, ub=ub: e.tensor_scalar(out=acc[:], in0=ub[:], scalar1=wts[:, 0:1], scalar2=None, op0=ALU.mult), reads=[r_ub, r_wts], writes=[r_acc])
                        else:
                            fw.op("dve", lambda e, ub=ub, sl=sl: e.scalar_tensor_tensor(out=acc[:], in0=ub[:], scalar=wts[:, sl:sl + 1], in1=acc[:], op0=ALU.mult, op1=ALU.add),
                                  reads=[r_ub, r_wts, r_acc], writes=[r_acc])
                    fw.op("dve", lambda e: e.tensor_tensor(out=acc[:], in0=acc[:], in1=h2[:], op=ALU.add), reads=[r_acc, r_h2], writes=[r_acc])
                    fw.op("act", lambda e: e.activation(out=junk[:], in_=acc[:], func=ACT.Square, accum_out=sq[:, 4:5]), reads=[r_acc], writes=[r_junk, r_sq])
                    fw.op("dve", lambda e: e.tensor_scalar(out=sq[:, 5:6], in0=sq[:, 4:5], scalar1=1.0 / D, scalar2=EPS, op0=ALU.mult, op1=ALU.add), reads=[r_sq], writes=[r_sq])
                    fw.op("act", lambda e: e.activation(out=sq[:, 6:7], in_=sq[:, 5:6], func=ACT.Ln), reads=[r_sq], writes=[r_sq])
                    fw.op("act", lambda e: e.activation(out=sq[:, 7:8], in_=sq[:, 6:7], func=ACT.Exp, scale=-0.5), reads=[r_sq], writes=[r_sq])
                    fw.op("dve", lambda e: e.scalar_tensor_tensor(out=prod[:], in0=acc[:], scalar=sq[:, 7:8], in1=gfin[:], op0=ALU.mult, op1=ALU.mult), reads=[r_acc, r_sq, r_gfin], writes=[r_prod])
                    fw.dma("sp", y_out[t0:t0 + 128, :], prod[:], reads=[r_prod])
                fw.barrier()

        if 3 not in phases:
            zt, r_zt = P0.sb([128, D], F32, "zt")
            fw.op("pool", lambda e: e.memset(zt[:], 0.0), writes=[r_zt])
            for ti in range(16):
                fw.dma("sp", y_out[ti * 128:(ti + 1) * 128, :], zt[:], reads=[r_zt])
        fw.barrier()
    return nc


def _prep_inputs(inputs):
    f32 = np.float32
    x = np.asarray(inputs["x"], f32)
    pos = np.asarray(inputs["positions"])
    ident = np.eye(128, dtype=f32)
    k_ = np.arange(128)
    tri = (k_[:, None] <= k_[None, :]).astype(f32)
    ones = np.ones((128, 128), f32)
    tri_pen = np.where(k_[None, :] <= k_[:, None], 0.0, NEG).astype(f32)
    consts = np.concatenate([ident, tri, ones, tri_pen, np.zeros((128, 128), f32)], axis=1)
    inv16 = (500000.0 ** (-2.0 * np.arange(16, dtype=f32) / 32)).astype(f32)
    inv8 = (500000.0 ** (-2.0 * np.arange(8, dtype=f32) / 16)).astype(f32)
    invf = np.tile(np.concatenate([inv16, inv8])[None, :], (128, 1)).astype(f32)

    def pc(v, n):
        return np.ascontiguousarray(np.asarray(v, f32).reshape(n, 128).T)

    def bc(v):
        return np.ascontiguousarray(np.tile(np.asarray(v, f32).reshape(1, -1), (128, 1)))

    cw = np.asarray(inputs["conv_w"], f32)[0, :, 0, :]
    convw = np.ascontiguousarray(cw.reshape(4, 24, 128).transpose(2, 1, 0).reshape(128, 96))
    shared = {
        "consts": consts, "invf": invf,
        "w_in": np.asarray(inputs["w_in"], f32)[0],
        "g_mix": pc(inputs["norm_mix_g"][0], 8),
        "convw": convw, "convb": pc(inputs["conv_b"][0], 24),
        "hp3": np.concatenate([bc(inputs["dt_bias"][0]), bc(inputs["a_log"][0]), bc(inputs["d_skip"][0])], axis=1),
        "g_ssd": pc(inputs["ssd_norm_g"][0], 16),
        "w_attn_branch": np.asarray(inputs["w_attn_branch"], f32)[0],
        "w_ssd_branch": np.asarray(inputs["w_ssd_branch"], f32)[0],
        "w_out": np.asarray(inputs["w_out"], f32)[0],
        "g_cross": pc(inputs["norm_cross_g"][0], 8), "g_mem": pc(inputs["norm_mem_g"][0], 8),
        "w_cross_q": np.asarray(inputs["w_cross_q"], f32)[0],
        "w_cross_kv": np.asarray(inputs["w_cross_kv"], f32)[0],
        "w_cross_out": np.asarray(inputs["w_cross_out"], f32)[0],
        "g_ffn": bc(inputs["norm_ffn_g"][0]), "g_ffn_c": pc(inputs["norm_ffn_g"][0], 8),
        "w_peer_q": np.asarray(inputs["w_peer_q"], f32)[0],
        "sub_keys": np.ascontiguousarray(np.asarray(inputs["peer_sub_keys"], f32)[0].reshape(16, 128, 128)),
        "peer_u": np.asarray(inputs["peer_u"], f32)[0],
        "peer_v": np.asarray(inputs["peer_v"], f32)[0],
        "g_final": bc(inputs["norm_final_g"]),
    }
    maps = []
    for c in range(NCORE):
        b, j = c // 4, c % 4
        xs = np.zeros((NSEG, SEGT, D), f32)
        ps = np.zeros((NSEG, SEGT), f32)
        vs = np.zeros((NSEG, SEGT), f32)
        pen = np.full((6,), NEG, f32)
        for s in range(NSEG):
            t0 = 2048 * j - NCTX + s * SEGT
            if t0 >= 0:
                xs[s] = x[b, t0:t0 + SEGT]
                ps[s] = pos[b, t0:t0 + SEGT].astype(f32)
                vs[s] = 1.0
                if s < 6:
                    pen[s] = 0.0
        m = dict(shared)
        m["x_seg"] = xs
        m["pos_seg"] = np.ascontiguousarray(ps.reshape(NSEG * 8, 128).T)
        m["valid_seg"] = np.ascontiguousarray(vs.reshape(NSEG * 8, 128).T)
        m["pen_ctx"] = np.ascontiguousarray(np.tile(pen[None, :], (128, 1)))
        m["mem"] = np.asarray(inputs["mem"], f32)[b]
        maps.append(m)
    return maps


def kernel(**inputs):
    nc = build()
    maps = _prep_inputs(inputs)
    res = run_bass_kernel_spmd(nc, maps, core_ids=list(range(NCORE)))
    out = np.zeros((2, 8192, D), np.float32)
    for c in range(NCORE):
        b, j = c // 4, c % 4
        out[b, 2048 * j:2048 * (j + 1)] = res.results[c]["y_out"]
    return out
```

```python
import math
import numpy as np
import ml_dtypes
from contextlib import ExitStack
import concourse.bass as bass
import concourse.mybir as mybir
from concourse.bass_utils import run_bass_kernel_spmd

F32 = mybir.dt.float32
BF16 = mybir.dt.bfloat16
I32 = mybir.dt.int32
U32 = mybir.dt.uint32
ALU = mybir.AluOpType
ACT = mybir.ActivationFunctionType
AX = mybir.AxisListType

D = 1024
NCORE = 8
TOWN = 2048
SEGT = 1024
NSEG = 8
NCTX = 6144
EPS = 1e-6
NEG = -1.0e30
OQ, OK_, OV, OIQ, OIK, OIW, OZ, OXBC, ODT, OGATE = 0, 1024, 1280, 1536, 1792, 1856, 1860, 3908, 6980, 7012
TWO_PI = 2.0 * math.pi


class Res:
    __slots__ = ("name", "w", "r")

    def __init__(self, name=""):
        self.name = name
        self.w = None
        self.r = []


class FW:
    NDMA = 24

    def __init__(self, nc, es):
        self.nc = nc
        self.eng = {"pe": nc.tensor, "dve": nc.vector, "act": nc.scalar, "pool": nc.gpsimd, "sp": nc.sync}
        self.sem = {k: es.enter_context(nc.semaphore("s_" + k)) for k in self.eng}
        self.cnt = {k: 0 for k in self.eng}
        self.dsem = [es.enter_context(nc.semaphore(f"d{i}")) for i in range(self.NDMA)]
        self.dcnt = [0] * self.NDMA
        self.dnext = 0
        self.seen = {k: {} for k in self.eng}

    def _wait(self, e, tok):
        sem, val = tok
        if self.seen[e].get(sem.name, 0) >= val:
            return
        self.eng[e].wait_ge(sem, val)
        self.seen[e][sem.name] = val

    def _deps(self, e, reads, writes, skip_self=False):
        toks = []
        for r in reads:
            if r.w is not None:
                toks.append(r.w)
        for w in writes:
            if w.w is not None:
                toks.append(w.w)
            toks.extend(w.r)
        best = {}
        for sem, val in toks:
            if skip_self and sem is self.sem[e]:
                continue
            if sem.name not in best or best[sem.name][1] < val:
                best[sem.name] = (sem, val)
        for tok in best.values():
            self._wait(e, tok)

    def _commit(self, tok, reads, writes):
        for r in reads:
            r.r.append(tok)
            if len(r.r) > 32:
                best = {}
                for s, v in r.r:
                    if s.name not in best or best[s.name][1] < v:
                        best[s.name] = (s, v)
                r.r = list(best.values())
        for w in writes:
            w.w = tok
            w.r = []

    def op(self, e, fn, reads=(), writes=(), skip_self=False):
        self._deps(e, reads, writes, skip_self)
        ins = fn(self.eng[e])
        self.cnt[e] += 1
        ins.then_inc(self.sem[e], 1)
        tok = (self.sem[e], self.cnt[e])
        self._commit(tok, reads, writes)
        return tok

    def dma(self, e, out, in_, reads=(), writes=(), indirect=None, **kw):
        i = self.dnext
        self.dnext = (self.dnext + 1) % self.NDMA
        sem = self.dsem[i]
        if self.dcnt[i] > 0:
            self._wait(e, (sem, self.dcnt[i]))
        self._deps(e, reads, writes)
        if indirect is not None:
            ins = self.eng[e].indirect_dma_start(out=out, out_offset=None, in_=in_, in_offset=indirect)
        else:
            ins = self.eng[e].dma_start(out=out, in_=in_, **kw)
        self.dcnt[i] += 16
        ins.then_inc(sem, 16)
        tok = (sem, self.dcnt[i])
        self._commit(tok, reads, writes)
        return tok

    def barrier(self):
        toks = [(self.sem[k], self.cnt[k]) for k in self.eng if self.cnt[k] > 0]
        toks += [(self.dsem[i], self.dcnt[i]) for i in range(self.NDMA) if self.dcnt[i] > 0]
        for e in self.eng:
            for t in toks:
                if t[0] is self.sem[e]:
                    continue
                self._wait(e, t)


class Pool:
    N = [0]

    def __init__(self, nc, es):
        self.nc, self.es = nc, es

    def sb(self, shape, dt, name=None):
        Pool.N[0] += 1
        t = self.es.enter_context(self.nc.sbuf_tensor(f"{name or 't'}_{Pool.N[0]}", list(shape), dt))
        return t, Res(name or "t")


def build(dbg=False, phases=(1, 2, 3)):
    nc = bass.Bass("TRN2", target_bir_lowering=False)

    def din(name, shape, dt=F32):
        return nc.dram_tensor(name, list(shape), dt, kind="ExternalInput").ap()

    x_seg = din("x_seg", [NSEG, SEGT, D])
    pos_seg = din("pos_seg", [128, NSEG * 8])
    valid_seg = din("valid_seg", [128, NSEG * 8])
    pen_ctx = din("pen_ctx", [128, 6])
    consts = din("consts", [128, 5 * 128])
    invf = din("invf", [128, 24])
    mem = din("mem", [256, D])
    w_in = din("w_in", [D, 9060])
    g_mix = din("g_mix", [128, 8])
    convw = din("convw", [128, 24 * 4])
    convb = din("convb", [128, 24])
    hp3 = din("hp3", [128, 96])
    g_ssd = din("g_ssd", [128, 16])
    w_attn_branch = din("w_attn_branch", [D, D])
    w_ssd_branch = din("w_ssd_branch", [2048, D])
    w_out = din("w_out", [D, D])
    g_cross = din("g_cross", [128, 8])
    g_mem = din("g_mem", [128, 8])
    w_cross_q = din("w_cross_q", [D, D])
    w_cross_kv = din("w_cross_kv", [D, 2048])
    w_cross_out = din("w_cross_out", [D, D])
    g_ffn = din("g_ffn", [128, D])
    g_ffn_c = din("g_ffn_c", [128, 8])
    w_peer_q = din("w_peer_q", [D, 2048])
    sub_keys = din("sub_keys", [16, 128, 128])
    peer_u = din("peer_u", [16384, D])
    peer_v = din("peer_v", [16384, D])
    g_final = din("g_final", [128, D])
    y_out = nc.dram_tensor("y_out", [TOWN, D], F32, kind="ExternalOutput").ap()
    sproj_d = nc.dram_tensor("sproj_d", [TOWN, D], F32, kind="Internal" if 1 in phases else "ExternalInput").ap()
    attn_d = nc.dram_tensor("attn_d", [TOWN, D], F32, kind="Internal" if 2 in phases else "ExternalInput").ap()
    if dbg:
        dbg_sproj = nc.dram_tensor("dbg_sproj", [TOWN, D], F32, kind="ExternalOutput").ap()
        dbg_attn = nc.dram_tensor("dbg_attn", [TOWN, D], F32, kind="ExternalOutput").ap()

    with ExitStack() as es0:
        fw = FW(nc, es0)
        P0 = Pool(nc, es0)
        banks = []
        for i in range(8):
            t = es0.enter_context(nc.psum_tensor(f"bank{i}", [128, 512], F32))
            banks.append((t, Res(f"bank{i}")))
        cst, r_cst = P0.sb([128, 5 * 128], F32, "cst")
        cstb, r_cstb = P0.sb([128, 5 * 128], BF16, "cstb")
        fw.dma("sp", cst[:], consts, writes=[r_cst])
        fw.op("dve", lambda e: e.tensor_copy(out=cstb[:], in_=cst[:]), reads=[r_cst], writes=[r_cstb])
        ident_b = cstb[:, 0:128]
        tri_f = cst[:, 128:256]
        ones_f = cst[:, 256:384]
        tri_pen = cst[:, 384:512]
        ones_b = cstb[:, 256:384]
        gm, r_gm = P0.sb([128, 8], F32, "gm")
        fw.dma("sp", gm[:], g_mix, writes=[r_gm])
        posv, r_posv = P0.sb([128, NSEG * 8], F32, "posv")
        fw.dma("sp", posv[:], pos_seg, writes=[r_posv])
        validv, r_validv = P0.sb([128, NSEG * 8], F32, "validv")
        fw.dma("sp", validv[:], valid_seg, writes=[r_validv])
        invt, r_invt = P0.sb([128, 24], F32, "invt")
        fw.dma("sp", invt[:], invf, writes=[r_invt])

        def w_rows(ap2d):
            return ap2d.rearrange("(c p) n -> p c n", p=128)

        w_in_r = w_rows(w_in)

        def load_weight(pool, dst, r_dst, src_r, c0, ncols, gt, r_gt, nch=8, eng="pool", stg_cols=512):
            stgf, r_stg = pool
            stg_cols = 4096 // nch
            stg = stgf[:, 0:nch * stg_cols].rearrange("p (c n) -> p c n", c=nch)
            done = 0
            while done < ncols:
                n = min(stg_cols, ncols - done)
                fw.dma("sp", stg[:, 0:nch, 0:n], src_r[:, 0:nch, c0 + done:c0 + done + n], writes=[r_stg])
                if gt is None:
                    fw.op(eng, lambda e, n=n, done=done: e.tensor_copy(out=dst[:, 0:nch, done:done + n], in_=stg[:, 0:nch, 0:n]),
                          reads=[r_stg], writes=[r_dst])
                else:
                    fw.op(eng, lambda e, n=n, done=done: e.tensor_tensor(
                        out=dst[:, 0:nch, done:done + n], in0=stg[:, 0:nch, 0:n],
                        in1=gt[:, 0:nch].unsqueeze(2).to_broadcast([128, nch, n]), op=ALU.mult),
                        reads=[r_stg, r_gt], writes=[r_dst])
                done += n

        def build_xT(pool, seg, xT, r_xT, xt_bufs, ntile=8, src=None, gdiv=D):
            for ti in range(ntile):
                xt, r_xt, xb, r_xb, ss, r_ss = xt_bufs[ti % 2]
                srcap = x_seg[seg, ti * 128:(ti + 1) * 128, :] if src is None else src(ti)
                fw.dma("sp", xt[:], srcap, writes=[r_xt])
                fw.op("act", lambda e: e.activation(out=xb[:], in_=xt[:], func=ACT.Square, accum_out=ss[:, 0:1]),
                      reads=[r_xt], writes=[r_xb, r_ss])
                fw.op("dve", lambda e: e.tensor_scalar(out=ss[:, 1:2], in0=ss[:, 0:1], scalar1=1.0 / gdiv, scalar2=EPS,
                                                       op0=ALU.mult, op1=ALU.add), reads=[r_ss], writes=[r_ss])
                fw.op("act", lambda e: e.activation(out=ss[:, 3:4], in_=ss[:, 1:2], func=ACT.Ln), reads=[r_ss], writes=[r_ss])
                fw.op("act", lambda e: e.activation(out=ss[:, 2:3], in_=ss[:, 3:4], func=ACT.Exp, scale=-0.5), reads=[r_ss], writes=[r_ss])
                fw.op("dve", lambda e: e.tensor_scalar(out=xb[:], in0=xt[:], scalar1=ss[:, 2:3], scalar2=None, op0=ALU.mult),
                      reads=[r_xt, r_ss], writes=[r_xb])
                bk, r_bk = banks[ti % 2]
                bkb = bk[:].bitcast(BF16)
                for ch in range(8):
                    fw.op("pe", lambda e, ch=ch: e.transpose(out=bkb[:, ch * 128:(ch + 1) * 128], in_=xb[:, ch * 128:(ch + 1) * 128],
                                                            identity=ident_b),
                          reads=[r_xb, r_cstb], writes=[r_bk], skip_self=True)
                fw.op("act", lambda e, ti=ti: e.activation(out=xT[:, :, ti * 128:(ti + 1) * 128],
                                                          in_=bkb.rearrange("p (c t) -> p c t", c=8), func=ACT.Copy),
                      reads=[r_bk], writes=[r_xT])

        if 1 in phases:
            with ExitStack() as es1:
                P = Pool(nc, es1)
                xT, r_xT = P.sb([128, 8, SEGT], BF16, "xT")
                xt_bufs = []
                for i in range(2):
                    xt, r_xt = P.sb([128, D], F32, "xt")
                    xb, r_xb = P.sb([128, D], BF16, "xb")
                    ss, r_ss = P.sb([128, 4], F32, "ss")
                    xt_bufs.append((xt, r_xt, xb, r_xb, ss, r_ss))
                wstg = P.sb([128, 4096], F32, "wstg")
                cw, r_cw = P.sb([128, 24, 4], F32, "cw")
                cb, r_cb = P.sb([128, 24], F32, "cb")
                fw.dma("sp", cw[:], convw.rearrange("p (r k) -> p r k", k=4), writes=[r_cw])
                fw.dma("sp", cb[:], convb, writes=[r_cb])
                hp, r_hp = P.sb([128, 96], F32, "hp")
                fw.dma("sp", hp[:], hp3, writes=[r_hp])
                gs, r_gs = P.sb([128, 16], F32, "gs")
                fw.dma("sp", gs[:], g_ssd, writes=[r_gs])
                a_bc, r_abc = P.sb([128, 32], F32, "a_bc")
                fw.op("act", lambda e: e.activation(out=a_bc[:], in_=hp[:, 32:64], func=ACT.Exp), reads=[r_hp], writes=[r_abc])
                fw.op("dve", lambda e: e.tensor_scalar(out=a_bc[:], in0=a_bc[:], scalar1=-1.0, scalar2=None, op0=ALU.mult),
                      reads=[r_abc], writes=[r_abc])
                wdt, r_wdt = P.sb([128, 8, 32], BF16, "wdt")
                load_weight(wstg, wdt, r_wdt, w_in_r, ODT, 32, gm, r_gm)
                halo, r_halo = P.sb([128, 24, 3], F32, "halo")
                fw.op("pool", lambda e: e.memset(halo[:], 0.0), writes=[r_halo])
                state, r_state = P.sb([128, 4, 512], F32, "state")
                fw.op("pool", lambda e: e.memset(state[:], 0.0), writes=[r_state])
                state_bf, r_statebf = P.sb([128, 512], BF16, "state_bf")
                NT = SEGT // 128
                dtr, r_dtr = P.sb([128, NT, 32], F32, "dtr")
                tmpa, r_tmpa = P.sb([128, NT, 32], F32, "tmpa")
                tmpb, r_tmpb = P.sb([128, NT, 32], F32, "tmpb")
                dt_all, r_dt = P.sb([128, NT, 32], F32, "dt_all")
                adt, r_adt = P.sb([128, NT, 32], F32, "adt")
                acs, r_acs = P.sb([128, NT, 32], F32, "acs")
                tot, r_tot = P.sb([128, NT, 32], F32, "tot")
                dtdte, r_dtdte = P.sb([128, NT, 32], F32, "dtdte")
                cdec, r_cdec = P.sb([128, NT, 32], F32, "cdec")
                eacs, r_eacs = P.sb([128, NT, 32], F32, "eacs")
                wrow, r_wrow = P.sb([128, 8, 128], BF16, "wrow")
                rowbufs = [P.sb([128, 3 + SEGT], F32, "rowbuf") for _ in range(2)]
                convacc = [P.sb([128, SEGT], F32, "convacc") for _ in range(2)]
                rowact = [P.sb([128, SEGT], BF16, "rowact") for _ in range(2)]
                xs_tok, r_xstok = P.sb([128, NT, 512], BF16, "xs_tok")
                B_tok, r_Btok = P.sb([128, NT, 128], BF16, "B_tok")
                BT, r_BT = P.sb([128, SEGT], BF16, "BT")
                CT, r_CT = P.sb([128, SEGT], BF16, "CT")
                xdt, r_xdt = P.sb([128, 512], BF16, "xdt")
                xdd, r_xdd = P.sb([128, 512], BF16, "xdd")
                y_store, r_ys = P.sb([128, NT, 2048], BF16, "y_store")
                cbm, r_cbm = P.sb([128, 128], F32, "cbm")
                triadt, r_triadt = P.sb([128, 8, 128], F32, "triadt")
                segb, r_segb = P.sb([128, 8, 128], F32, "segb")
                mT, r_mT = P.sb([128, 8, 128], BF16, "mT")
                t1, r_t1 = P.sb([128, 512], F32, "t1")
                t2, r_t2 = P.sb([128, 512], F32, "t2")
                wz, r_wz = P.sb([128, 8, 512], BF16, "wz")
                ws, r_ws = P.sb([128, 16, 1024], BF16, "ws")
                zact, r_zact = P.sb([128, 512], F32, "zact")
                ssq, r_ssq = P.sb([128, NT, 8], F32, "ssq")
                ygT, r_ygT = P.sb([128, 16, 128], BF16, "ygT")
                spt, r_spt = P.sb([128, D], F32, "spt")
                junk, r_junk = P.sb([128, 512], F32, "junk")

                for seg in range(NSEG):
                    own = seg >= 6
                    build_xT(P, seg, xT, r_xT, xt_bufs)
                    bk, r_bk = banks[2]
                    for ti in range(NT):
                        for ch in range(8):
                            fw.op("pe", lambda e, ti=ti, ch=ch: e.matmul(bk[:, ti * 32:(ti + 1) * 32], lhsT=xT[:, ch, ti * 128:(ti + 1) * 128],
                                                                          rhs=wdt[:, ch, :], start=(ch == 0), stop=(ch == 7)),
                                  reads=[r_xT, r_wdt], writes=[r_bk], skip_self=True)
                    bkv = bk[:, 0:NT * 32].rearrange("p (t h) -> p t h", h=32)
                    fw.op("dve", lambda e: e.tensor_tensor(out=dtr[:], in0=bkv, in1=hp[:, 0:32].unsqueeze(1).to_broadcast([128, NT, 32]), op=ALU.add),
                          reads=[r_bk, r_hp], writes=[r_dtr])
                    fw.op("dve", lambda e: e.tensor_scalar(out=tmpa[:], in0=dtr[:], scalar1=-1.0, scalar2=None, op0=ALU.mult), reads=[r_dtr], writes=[r_tmpa])
                    fw.op("dve", lambda e: e.tensor_tensor(out=tmpa[:], in0=tmpa[:], in1=dtr[:], op=ALU.max), reads=[r_dtr, r_tmpa], writes=[r_tmpa])
                    fw.op("act", lambda e: e.activation(out=tmpa[:], in_=tmpa[:], func=ACT.Exp, scale=-1.0), reads=[r_tmpa], writes=[r_tmpa])
                    fw.op("act", lambda e: e.activation(out=tmpa[:], in_=tmpa[:], func=ACT.Ln, bias=1.0), reads=[r_tmpa], writes=[r_tmpa])
                    fw.op("dve", lambda e: e.tensor_single_scalar(out=tmpb[:], in_=dtr[:], scalar=0.0, op=ALU.max), reads=[r_dtr], writes=[r_tmpb])
                    fw.op("dve", lambda e: e.tensor_tensor(out=tmpb[:], in0=tmpb[:], in1=tmpa[:], op=ALU.add), reads=[r_tmpa, r_tmpb], writes=[r_tmpb])
                    fw.op("dve", lambda e, seg=seg: e.tensor_tensor(out=dt_all[:], in0=tmpb[:],
                                                                  in1=validv[:, seg * NT:(seg + 1) * NT].unsqueeze(2).to_broadcast([128, NT, 32]), op=ALU.mult),
                          reads=[r_tmpb, r_validv], writes=[r_dt])
                    fw.op("dve", lambda e: e.tensor_tensor(out=adt[:], in0=dt_all[:], in1=a_bc[:].unsqueeze(1).to_broadcast([128, NT, 32]), op=ALU.mult),
                          reads=[r_dt, r_abc], writes=[r_adt])
                    bk3, r_bk3 = banks[3]
                    for ti in range(NT):
                        fw.op("pe", lambda e, ti=ti: e.matmul(bk[:, ti * 32:(ti + 1) * 32], lhsT=tri_f, rhs=adt[:, ti, :], start=True, stop=True),
                              reads=[r_cst, r_adt], writes=[r_bk], skip_self=True)
                        fw.op("pe", lambda e, ti=ti: e.matmul(bk3[:, ti * 32:(ti + 1) * 32], lhsT=ones_f, rhs=adt[:, ti, :], start=True, stop=True),
                              reads=[r_cst, r_adt], writes=[r_bk3], skip_self=True)
                    fw.op("act", lambda e: e.activation(out=acs[:], in_=bkv, func=ACT.Copy), reads=[r_bk], writes=[r_acs])
                    bk3v = bk3[:, 0:NT * 32].rearrange("p (t h) -> p t h", h=32)
                    fw.op("act", lambda e: e.activation(out=tot[:], in_=bk3v, func=ACT.Copy), reads=[r_bk3], writes=[r_tot])
                    fw.op("dve", lambda e: e.tensor_tensor(out=tmpa[:], in0=tot[:], in1=acs[:], op=ALU.subtract), reads=[r_tot, r_acs], writes=[r_tmpa])
                    fw.op("act", lambda e: e.activation(out=tmpa[:], in_=tmpa[:], func=ACT.Exp), reads=[r_tmpa], writes=[r_tmpa])
                    fw.op("dve", lambda e: e.tensor_tensor(out=dtdte[:], in0=tmpa[:], in1=dt_all[:], op=ALU.mult), reads=[r_tmpa, r_dt], writes=[r_dtdte])
                    fw.op("act", lambda e: e.activation(out=cdec[:], in_=tot[:], func=ACT.Exp), reads=[r_tot], writes=[r_cdec])
                    if own:
                        fw.op("act", lambda e: e.activation(out=eacs[:], in_=acs[:], func=ACT.Exp), reads=[r_acs], writes=[r_eacs])
                        fw.op("pool", lambda e: e.memset(ssq[:], 0.0), writes=[r_ssq])

                    for g in range(4):
                        rows = [(4 * g + i, "xs", i) for i in range(4)] + [(16 + g, "B", 0)] + ([(20 + g, "C", 0)] if seg >= 5 else [])
                        for ri, (r, kind, sub) in enumerate(rows):
                            rb, r_rb = rowbufs[ri % 2]
                            ca, r_ca = convacc[ri % 2]
                            ra, r_ra = rowact[ri % 2]
                            load_weight(wstg, wrow, r_wrow, w_in_r, OXBC + r * 128, 128, gm, r_gm)
                            for tg in range(SEGT // 512):
                                bkp, r_bkp = banks[4 + (tg % 2)]
                                for ch in range(8):
                                    fw.op("pe", lambda e, ch=ch, tg=tg, bkp=bkp: e.matmul(bkp[:], lhsT=wrow[:, ch, :], rhs=xT[:, ch, tg * 512:(tg + 1) * 512],
                                                                                          start=(ch == 0), stop=(ch == 7)),
                                          reads=[r_wrow, r_xT], writes=[r_bkp], skip_self=True)
                                fw.op("act", lambda e, tg=tg, bkp=bkp, rb=rb: e.activation(out=rb[:, 3 + tg * 512:3 + (tg + 1) * 512], in_=bkp[:], func=ACT.Copy),
                                      reads=[r_bkp], writes=[r_rb])
                            fw.op("pool", lambda e, rb=rb, r=r: e.tensor_copy(out=rb[:, 0:3], in_=halo[:, r, :]), reads=[r_halo], writes=[r_rb])
                            fw.op("pool", lambda e, rb=rb, r=r: e.tensor_copy(out=halo[:, r, :], in_=rb[:, SEGT:SEGT + 3]), reads=[r_rb], writes=[r_halo])
                            if kind == "C" and not own:
                                continue
                            fw.op("dve", lambda e, rb=rb, ca=ca, r=r: e.tensor_scalar(out=ca[:], in0=rb[:, 0:SEGT], scalar1=cw[:, r, 0:1], scalar2=None, op0=ALU.mult),
                                  reads=[r_rb, r_cw], writes=[r_ca])
                            for k in range(1, 4):
                                fw.op("dve", lambda e, rb=rb, ca=ca, r=r, k=k: e.scalar_tensor_tensor(out=ca[:], in0=rb[:, k:k + SEGT], scalar=cw[:, r, k:k + 1], in1=ca[:],
                                                                                                     op0=ALU.mult, op1=ALU.add),
                                      reads=[r_rb, r_cw, r_ca], writes=[r_ca])
                            fw.op("act", lambda e, ca=ca, ra=ra, r=r: e.activation(out=ra[:], in_=ca[:], func=ACT.Silu, bias=cb[:, r:r + 1]),
                                  reads=[r_ca, r_cb], writes=[r_ra])
                            if kind == "C":
                                fw.op("pool", lambda e, ra=ra: e.tensor_copy(out=CT[:], in_=ra[:]), reads=[r_ra], writes=[r_CT])
                                continue
                            if kind == "B" and own:
                                fw.op("pool", lambda e, ra=ra: e.tensor_copy(out=BT[:], in_=ra[:]), reads=[r_ra], writes=[r_BT])
                            bkt, r_bkt = banks[6 + (ri % 2)]
                            bktb = bkt[:].bitcast(BF16)
                            for ti in range(NT):
                                fw.op("pe", lambda e, ti=ti, ra=ra, bktb=bktb: e.transpose(out=bktb[:, ti * 128:(ti + 1) * 128], in_=ra[:, ti * 128:(ti + 1) * 128], identity=ident_b),
                                      reads=[r_ra, r_cstb], writes=[r_bkt], skip_self=True)
                            src = bktb[:, 0:NT * 128].rearrange("p (t c) -> p t c", c=128)
                            if kind == "xs":
                                fw.op("act", lambda e, src=src, sub=sub: e.activation(out=xs_tok[:, :, sub * 128:(sub + 1) * 128], in_=src, func=ACT.Copy),
                                      reads=[r_bkt], writes=[r_xstok])
                            else:
                                fw.op("act", lambda e, src=src: e.activation(out=B_tok[:], in_=src, func=ACT.Copy), reads=[r_bkt], writes=[r_Btok])
                        for c in range(NT):
                            hs = slice(8 * g, 8 * g + 8)
                            xsv = xs_tok[:, c, :].rearrange("p (h q) -> p h q", q=64)
                            fw.op("dve", lambda e, c=c, xsv=xsv, hs=hs: e.tensor_tensor(out=xdd[:].rearrange("p (h q) -> p h q", q=64), in0=xsv,
                                                                                        in1=dtdte[:, c, hs].unsqueeze(2).to_broadcast([128, 8, 64]), op=ALU.mult),
                                  reads=[r_xstok, r_dtdte], writes=[r_xdd])
                            bks, r_bks = banks[2]
                            fw.op("pe", lambda e, c=c, bks=bks: e.matmul(bks[:], lhsT=B_tok[:, c, :], rhs=xdd[:], start=True, stop=True),
                                  reads=[r_Btok, r_xdd], writes=[r_bks], skip_self=True)
                            if own:
                                fw.op("act", lambda e, g=g: e.activation(out=state_bf[:], in_=state[:, g, :], func=ACT.Copy), reads=[r_state], writes=[r_statebf])
                                fw.op("dve", lambda e, c=c, xsv=xsv, hs=hs: e.tensor_tensor(out=xdt[:].rearrange("p (h q) -> p h q", q=64), in0=xsv,
                                                                                            in1=dt_all[:, c, hs].unsqueeze(2).to_broadcast([128, 8, 64]), op=ALU.mult),
                                      reads=[r_xstok, r_dt], writes=[r_xdt])
                                cs = slice(c * 128, (c + 1) * 128)
                                bkc, r_bkc = banks[3]
                                fw.op("pe", lambda e, cs=cs, bkc=bkc: e.matmul(bkc[:, 0:128], lhsT=BT[:, cs], rhs=CT[:, cs], start=True, stop=True),
                                      reads=[r_BT, r_CT], writes=[r_bkc], skip_self=True)
                                fw.op("dve", lambda e, bkc=bkc: e.tensor_tensor(out=cbm[:], in0=bkc[:, 0:128], in1=tri_f, op=ALU.mult), reads=[r_bkc, r_cst], writes=[r_cbm])
                                fw.op("pool", lambda e, c=c, hs=hs: e.tensor_tensor(out=triadt[:], in0=tri_f.unsqueeze(1).to_broadcast([128, 8, 128]),
                                                                                   in1=adt[:, c, hs].unsqueeze(2).to_broadcast([128, 8, 128]), op=ALU.mult),
                                      reads=[r_cst, r_adt], writes=[r_triadt])
                                bka, r_bka = banks[4]
                                bkb_, r_bkb_ = banks[5]
                                fw.op("pe", lambda e, bka=bka: e.matmul(bka[:], lhsT=ones_f, rhs=triadt[:, 0:4, :].rearrange("p h l -> p (h l)"), start=True, stop=True),
                                      reads=[r_cst, r_triadt], writes=[r_bka], skip_self=True)
                                fw.op("pe", lambda e, bkb_=bkb_: e.matmul(bkb_[:], lhsT=ones_f, rhs=triadt[:, 4:8, :].rearrange("p h l -> p (h l)"), start=True, stop=True),
                                      reads=[r_cst, r_triadt], writes=[r_bkb_], skip_self=True)
                                for h in range(8):
                                    bsrc, r_bsrc = (bka, r_bka) if h < 4 else (bkb_, r_bkb_)
                                    hh = h % 4
                                    fw.op("dve", lambda e, h=h, hh=hh, bsrc=bsrc, c=c, g=g: e.scalar_tensor_tensor(
                                        out=segb[:, h, :], in0=bsrc[:, hh * 128:(hh + 1) * 128], scalar=acs[:, c, 8 * g + h:8 * g + h + 1], in1=tri_f,
                                        op0=ALU.subtract, op1=ALU.mult), reads=[r_bsrc, r_acs, r_cst], writes=[r_segb])
                                fw.op("act", lambda e: e.activation(out=segb[:], in_=segb[:], func=ACT.Exp), reads=[r_segb], writes=[r_segb])
                                fw.op("dve", lambda e: e.tensor_tensor(out=mT[:], in0=segb[:], in1=cbm[:].unsqueeze(1).to_broadcast([128, 8, 128]), op=ALU.mult),
                                      reads=[r_segb, r_cbm], writes=[r_mT])
                                bky, r_bky = banks[6]
                                for h in range(8):
                                    fw.op("pe", lambda e, h=h, bky=bky: e.matmul(bky[:, h * 64:(h + 1) * 64], lhsT=mT[:, h, :], rhs=xdt[:, h * 64:(h + 1) * 64], start=True, stop=True),
                                          reads=[r_mT, r_xdt], writes=[r_bky], skip_self=True)
                                bko, r_bko = banks[7]
                                fw.op("pe", lambda e, cs=cs, bko=bko: e.matmul(bko[:], lhsT=CT[:, cs], rhs=state_bf[:], start=True, stop=True),
                                      reads=[r_CT, r_statebf], writes=[r_bko], skip_self=True)
                                fw.op("dve", lambda e, c=c, hs=hs, bko=bko: e.tensor_tensor(out=t1[:].rearrange("p (h q) -> p h q", q=64),
                                                                                          in0=bko[:].rearrange("p (h q) -> p h q", q=64),
                                                                                          in1=eacs[:, c, hs].unsqueeze(2).to_broadcast([128, 8, 64]), op=ALU.mult),
                                      reads=[r_bko, r_eacs], writes=[r_t1])
                                fw.op("pool", lambda e, xsv=xsv, hs=hs: e.tensor_tensor(out=t2[:].rearrange("p (h q) -> p h q", q=64), in0=xsv,
                                                                                       in1=hp[:, 64 + hs.start:64 + hs.stop].unsqueeze(2).to_broadcast([128, 8, 64]), op=ALU.mult),
                                      reads=[r_xstok, r_hp], writes=[r_t2])
                                fw.op("dve", lambda e: e.tensor_tensor(out=t1[:], in0=t1[:], in1=t2[:], op=ALU.add), reads=[r_t1, r_t2], writes=[r_t1])
                                fw.op("dve", lambda e, c=c, g=g, bky=bky: e.tensor_tensor(out=y_store[:, c, g * 512:(g + 1) * 512], in0=bky[:], in1=t1[:], op=ALU.add),
                                      reads=[r_bky, r_t1], writes=[r_ys])
                            stv = state[:, g, :].rearrange("p (h q) -> p h q", q=64)
                            fw.op("dve", lambda e, c=c, hs=hs, stv=stv: e.tensor_tensor(out=stv, in0=stv, in1=cdec[:, c, hs].unsqueeze(2).to_broadcast([128, 8, 64]), op=ALU.mult),
                                  reads=[r_state, r_cdec], writes=[r_state])
                            fw.op("dve", lambda e, g=g, bks=bks: e.tensor_tensor(out=state[:, g, :], in0=state[:, g, :], in1=bks[:], op=ALU.add),
                                  reads=[r_state, r_bks], writes=[r_state])
                        if own:
                            load_weight(wstg, wz, r_wz, w_in_r, OZ + g * 512, 512, gm, r_gm)
                            for c in range(NT):
                                bkz, r_bkz = banks[2 + (c % 2)]
                                for ch in range(8):
                                    fw.op("pe", lambda e, c=c, ch=ch, bkz=bkz: e.matmul(bkz[:], lhsT=xT[:, ch, c * 128:(c + 1) * 128], rhs=wz[:, ch, :], start=(ch == 0), stop=(ch == 7)),
                                          reads=[r_xT, r_wz], writes=[r_bkz], skip_self=True)
                                fw.op("act", lambda e, bkz=bkz: e.activation(out=zact[:], in_=bkz[:], func=ACT.Silu), reads=[r_bkz], writes=[r_zact])
                                ysl = y_store[:, c, g * 512:(g + 1) * 512]
                                fw.op("dve", lambda e, ysl=ysl: e.tensor_tensor(out=zact[:], in0=zact[:], in1=ysl, op=ALU.mult), reads=[r_zact, r_ys], writes=[r_zact])
                                fw.op("act", lambda e, c=c, g=g: e.activation(out=junk[:], in_=zact[:], func=ACT.Square, accum_out=ssq[:, c, g:g + 1]),
                                      reads=[r_zact], writes=[r_junk, r_ssq])
                                fw.op("pool", lambda e, ysl=ysl: e.tensor_copy(out=ysl, in_=zact[:]), reads=[r_zact], writes=[r_ys])
                    if own:
                        load_weight(wstg, ws, r_ws, w_rows(w_ssd_branch), 0, 1024, gs, r_gs, nch=16, stg_cols=512)
                        for c in range(NT):
                            fw.op("dve", lambda e, c=c: e.tensor_reduce(out=ssq[:, c, 4:5], in_=ssq[:, c, 0:4], axis=AX.X, op=ALU.add), reads=[r_ssq], writes=[r_ssq])
                            fw.op("dve", lambda e, c=c: e.tensor_scalar(out=ssq[:, c, 5:6], in0=ssq[:, c, 4:5], scalar1=1.0 / 2048, scalar2=EPS, op0=ALU.mult, op1=ALU.add),
                                  reads=[r_ssq], writes=[r_ssq])
                            fw.op("act", lambda e, c=c: e.activation(out=ssq[:, c, 7:8], in_=ssq[:, c, 5:6], func=ACT.Ln), reads=[r_ssq], writes=[r_ssq])
                            fw.op("act", lambda e, c=c: e.activation(out=ssq[:, c, 6:7], in_=ssq[:, c, 7:8], func=ACT.Exp, scale=-0.5), reads=[r_ssq], writes=[r_ssq])
                            for half in range(2):
                                bkt, r_bkt = banks[4 + half]
                                bktb = bkt[:].bitcast(BF16)
                                for i in range(8):
                                    ch = half * 8 + i
                                    fw.op("pe", lambda e, c=c, ch=ch, i=i, bktb=bktb: e.transpose(out=bktb[:, i * 128:(i + 1) * 128], in_=y_store[:, c, ch * 128:(ch + 1) * 128], identity=ident_b),
                                          reads=[r_ys, r_cstb], writes=[r_bkt], skip_self=True)
                                fw.op("act", lambda e, half=half, bktb=bktb: e.activation(out=ygT[:, half * 8:(half + 1) * 8, :], in_=bktb.rearrange("p (c t) -> p c t", c=8), func=ACT.Copy),
                                      reads=[r_bkt], writes=[r_ygT])
                            for cg in range(2):
                                bko, r_bko = banks[6 + cg]
                                for ch in range(16):
                                    fw.op("pe", lambda e, ch=ch, cg=cg, bko=bko: e.matmul(bko[:], lhsT=ygT[:, ch, :], rhs=ws[:, ch, cg * 512:(cg + 1) * 512], start=(ch == 0), stop=(ch == 15)),
                                          reads=[r_ygT, r_ws], writes=[r_bko], skip_self=True)
                                fw.op("dve", lambda e, c=c, cg=cg, bko=bko: e.tensor_scalar(out=spt[:, cg * 512:(cg + 1) * 512], in0=bko[:], scalar1=ssq[:, c, 6:7], scalar2=None, op0=ALU.mult),
                                      reads=[r_bko, r_ssq], writes=[r_spt])
                            t0 = (seg - 6) * SEGT + c * 128
                            fw.dma("sp", sproj_d[t0:t0 + 128, :], spt[:], reads=[r_spt])
                            if dbg:
                                fw.dma("sp", dbg_sproj[t0:t0 + 128, :], spt[:], reads=[r_spt])
                fw.barrier()

        if 2 in phases:
            with ExitStack() as es2:
                P = Pool(nc, es2)
                NT = SEGT // 128
                KT, r_KT = P.sb([128, 2, 8192], BF16, "KT")
                Vx, r_Vx = P.sb([128, 64, 2, 129], BF16, "Vx")
                ikT, r_ikT = P.sb([128, 4096], BF16, "ikT")
                fw.op("pool", lambda e: e.memset(Vx[:], 1.0), writes=[r_Vx])
                wq, r_wq = P.sb([128, 8, 1024], BF16, "wq")
                wiq, r_wiq = P.sb([128, 8, 260], BF16, "wiq")
                penx, r_penx = P.sb([128, 8], F32, "penx")
                fw.op("pool", lambda e: e.memset(penx[:], 0.0), writes=[r_penx])
                fw.dma("sp", penx[:, 0:6], pen_ctx, writes=[r_penx])
                trig, r_trig = P.sb([128, 4, 24], F32, "trig")
                trigi, r_trigi = P.sb([128, 24], I32, "trigi")
                trigk, r_trigk = P.sb([128, 24], F32, "trigk")
                rt = [P.sb([128, 8, 16], F32, "rt") for _ in range(4)]
                kb, r_kb = P.sb([128, 1024], BF16, "kb")
                xt_bufs = []
                for i in range(1):
                    xt, r_xt = P.sb([128, D], F32, "xt")
                    xb, r_xb = P.sb([128, D], BF16, "xb")
                    ss, r_ss = P.sb([128, 4], F32, "ss")
                    xt_bufs.append((xt, r_xt, xb, r_xb, ss, r_ss))
                xt_bufs = xt_bufs * 2
                es2a = ExitStack()
                Pa = Pool(nc, es2a)
                xT, r_xT = Pa.sb([128, 8, SEGT], BF16, "xT2")
                wstg = Pa.sb([128, 4096], F32, "wstg")
                wkv, r_wkv = Pa.sb([128, 8, 512], BF16, "wkv")
                wik, r_wik = Pa.sb([128, 8, 64], BF16, "wik")
                kf, r_kf = Pa.sb([128, 1024], F32, "kf")
                load_weight(wstg, wkv, r_wkv, w_in_r, OK_, 512, gm, r_gm)
                load_weight(wstg, wik, r_wik, w_in_r, OIK, 64, gm, r_gm)
                load_weight(wstg, wq, r_wq, w_in_r, OQ, 1024, gm, r_gm)
                load_weight(wstg, wiq, r_wiq, w_in_r, OIQ, 256, gm, r_gm)
                stgf, r_stg = wstg
                stg4 = stgf[:, 0:32].rearrange("p (c n) -> p c n", c=8)
                fw.dma("sp", stg4, w_in_r[:, :, OIW:OIW + 4], writes=[r_stg])
                fw.op("pool", lambda e: e.tensor_tensor(out=wiq[:, :, 256:260], in0=stg4, in1=gm[:].unsqueeze(2).to_broadcast([128, 8, 4]), op=ALU.mult),
                      reads=[r_stg, r_gm], writes=[r_wiq])

                def trig_tables(col):
                    fw.op("dve", lambda e: e.tensor_scalar(out=trig[:, 0, :], in0=invt[:], scalar1=posv[:, col:col + 1], scalar2=None, op0=ALU.mult),
                          reads=[r_invt, r_posv], writes=[r_trig])
                    for which, shift in ((2, 0.0), (3, 0.25)):
                        fw.op("dve", lambda e, shift=shift: e.tensor_scalar(out=trig[:, 1, :], in0=trig[:, 0, :], scalar1=1.0 / TWO_PI, scalar2=shift, op0=ALU.mult, op1=ALU.add),
                              reads=[r_trig], writes=[r_trig])
                        fw.op("dve", lambda e: e.tensor_copy(out=trigi[:], in_=trig[:, 1, :]), reads=[r_trig], writes=[r_trigi])
                        fw.op("dve", lambda e: e.tensor_copy(out=trigk[:], in_=trigi[:]), reads=[r_trigi], writes=[r_trigk])
                        fw.op("dve", lambda e: e.scalar_tensor_tensor(out=trig[:, 1, :], in0=trigk[:], scalar=-TWO_PI, in1=trig[:, 0, :], op0=ALU.mult, op1=ALU.add),
                              reads=[r_trigk, r_trig], writes=[r_trig])
                        fw.op("dve", lambda e, shift=shift: e.tensor_scalar(out=trig[:, 1, :], in0=trig[:, 1, :], scalar1=shift * TWO_PI, scalar2=-math.pi, op0=ALU.add, op1=ALU.max),
                              reads=[r_trig], writes=[r_trig])
                        fw.op("dve", lambda e: e.tensor_scalar(out=trig[:, 1, :], in0=trig[:, 1, :], scalar1=math.pi, scalar2=None, op0=ALU.min),
                              reads=[r_trig], writes=[r_trig])
                        fw.op("act", lambda e, which=which: e.activation(out=trig[:, which, :], in_=trig[:, 1, :], func=ACT.Sin), reads=[r_trig], writes=[r_trig])

                def rope(buf, r_buf, nh, dh, half, toff):
                    v = buf[:, 0:nh * dh].rearrange("p (h d) -> p h d", d=dh)
                    x1, x2 = v[:, :, 0:half], v[:, :, half:2 * half]
                    sn = trig[:, 2, toff:toff + half].unsqueeze(1).to_broadcast([128, nh, half])
                    cs = trig[:, 3, toff:toff + half].unsqueeze(1).to_broadcast([128, nh, half])
                    tm = [(t[0][:, 0:nh, 0:half], t[1]) for t in rt]
                    fw.op("dve", lambda e: e.tensor_tensor(out=tm[0][0], in0=x1, in1=cs, op=ALU.mult), reads=[r_buf, r_trig], writes=[tm[0][1]])
                    fw.op("dve", lambda e: e.tensor_tensor(out=tm[1][0], in0=x2, in1=sn, op=ALU.mult), reads=[r_buf, r_trig], writes=[tm[1][1]])
                    fw.op("dve", lambda e: e.tensor_tensor(out=tm[2][0], in0=x2, in1=cs, op=ALU.mult), reads=[r_buf, r_trig], writes=[tm[2][1]])
                    fw.op("dve", lambda e: e.tensor_tensor(out=tm[3][0], in0=x1, in1=sn, op=ALU.mult), reads=[r_buf, r_trig], writes=[tm[3][1]])
                    fw.op("dve", lambda e: e.tensor_tensor(out=x1, in0=tm[0][0], in1=tm[1][0], op=ALU.subtract), reads=[tm[0][1], tm[1][1]], writes=[r_buf])
                    fw.op("dve", lambda e: e.tensor_tensor(out=x2, in0=tm[2][0], in1=tm[3][0], op=ALU.add), reads=[tm[2][1], tm[3][1]], writes=[r_buf])

                for seg in range(NSEG):
                    build_xT(P, seg, xT, r_xT, xt_bufs)
                    for ti in range(NT):
                        blk = seg * NT + ti
                        trig_tables(blk)
                        bk, r_bk = banks[2 + (ti % 2)]
                        for ch in range(8):
                            fw.op("pe", lambda e, ch=ch, ti=ti, bk=bk: e.matmul(bk[:], lhsT=xT[:, ch, ti * 128:(ti + 1) * 128], rhs=wkv[:, ch, :], start=(ch == 0), stop=(ch == 7)),
                                  reads=[r_xT, r_wkv], writes=[r_bk], skip_self=True)
                        bki, r_bki = banks[4 + (ti % 2)]
                        for ch in range(8):
                            fw.op("pe", lambda e, ch=ch, ti=ti, bki=bki: e.matmul(bki[:, 0:64], lhsT=xT[:, ch, ti * 128:(ti + 1) * 128], rhs=wik[:, ch, :], start=(ch == 0), stop=(ch == 7)),
                                  reads=[r_xT, r_wik], writes=[r_bki], skip_self=True)
                        fw.op("act", lambda e, bk=bk, blk=blk: e.activation(out=Vx[:, blk, :, 0:128], in_=bk[:, 256:512].rearrange("p (k d) -> p k d", d=128), func=ACT.Copy),
                              reads=[r_bk], writes=[r_Vx])
                        fw.op("act", lambda e, bk=bk: e.activation(out=kf[:, 0:256], in_=bk[:, 0:256], func=ACT.Copy), reads=[r_bk], writes=[r_kf])
                        fw.op("act", lambda e, bki=bki: e.activation(out=kf[:, 256:320], in_=bki[:, 0:64], func=ACT.Copy), reads=[r_bki], writes=[r_kf])
                        rope(kf, r_kf, 2, 128, 16, 0)
                        rope(kf[:, 256:320], r_kf, 1, 64, 8, 16)
                        fw.op("dve", lambda e: e.tensor_copy(out=kf[:, 320:384], in_=kf[:, 256:320]), reads=[r_kf], writes=[r_kf])
                        fw.op("dve", lambda e: e.tensor_copy(out=kb[:, 0:384], in_=kf[:, 0:384]), reads=[r_kf], writes=[r_kb])
                        bkt, r_bkt = banks[6 + (ti % 2)]
                        bktb = bkt[:].bitcast(BF16)
                        for kv in range(2):
                            fw.op("pe", lambda e, kv=kv, bktb=bktb: e.transpose(out=bktb[:, kv * 128:(kv + 1) * 128], in_=kb[:, kv * 128:(kv + 1) * 128], identity=ident_b),
                                  reads=[r_kb, r_cstb], writes=[r_bkt], skip_self=True)
                        fw.op("pe", lambda e, bktb=bktb: e.transpose(out=bktb[:, 256:384], in_=kb[:, 256:384], identity=ident_b),
                              reads=[r_kb, r_cstb], writes=[r_bkt], skip_self=True)
                        fw.op("act", lambda e, bktb=bktb, blk=blk: e.activation(out=KT[:, :, blk * 128:(blk + 1) * 128], in_=bktb[:, 0:256].rearrange("p (k t) -> p k t", k=2), func=ACT.Copy),
                              reads=[r_bkt], writes=[r_KT])
                        pr = slice(0, 64) if blk < 32 else slice(64, 128)
                        fw.op("act", lambda e, bktb=bktb, blk=blk, pr=pr: e.activation(out=ikT[pr, (blk % 32) * 128:(blk % 32 + 1) * 128], in_=bktb[pr, 256:384], func=ACT.Copy),
                              reads=[r_bkt], writes=[r_ikT])

                fw.barrier()
                es2a.close()
                xT, r_xT = P.sb([128, 8, 128], BF16, "xTq")
                S, r_S = P.sb([128, 8192], F32, "S")
                mk, r_mk = P.sb([128, 8192], BF16, "mk")
                maskT, r_maskT = P.sb([128, 64, 128], BF16, "maskT")
                QT, r_QT = P.sb([128, 8, 128], BF16, "QT")
                iqT, r_iqT = P.sb([128, 4, 128], BF16, "iqT")
                qf, r_qf = P.sb([128, 1024], F32, "qf")
                wsg, r_wsg = P.sb([128, 16], F32, "wsg")
                bis, r_bis = P.sb([128, 8], F32, "bis")
                bis2, r_bis2 = P.sb([128, 2], F32, "bis2")
                r_bis2b = Res("bis2b")
                mk2, r_mk2 = mk, Res("mk2")
                rl = [P.sb([128, 512], F32, "rl") for _ in range(2)]
                eL = [P.sb([128, 512], BF16, "eL") for _ in range(4)]
                PT = [P.sb([128, 512], BF16, "PT") for _ in range(4)]
                m1c, r_m1c = P.sb([128, 1], F32, "m1c")
                fw.op("pool", lambda e: e.memset(m1c[:], -1.0), writes=[r_m1c])
                rden, r_rden = P.sb([128, 16], F32, "rden")
                at, r_at = P.sb([128, D], F32, "at")
                NITER = 24
                for it in range(16):
                    seg = 6 + it // 8
                    ti = it % 8
                    build_xT(P, seg, xT, r_xT, xt_bufs, ntile=1, src=lambda _t, seg=seg, ti=ti: x_seg[seg, ti * 128:(ti + 1) * 128, :])
                    trig_tables(seg * 8 + ti)
                    for cg in range(2):
                        bk, r_bk = banks[2 + cg]
                        for ch in range(8):
                            fw.op("pe", lambda e, ch=ch, cg=cg, bk=bk: e.matmul(bk[:], lhsT=xT[:, ch, :], rhs=wq[:, ch, cg * 512:(cg + 1) * 512], start=(ch == 0), stop=(ch == 7)),
                                  reads=[r_xT, r_wq], writes=[r_bk], skip_self=True)
                        fw.op("act", lambda e, cg=cg, bk=bk: e.activation(out=qf[:, cg * 512:(cg + 1) * 512], in_=bk[:], func=ACT.Copy), reads=[r_bk], writes=[r_qf])
                    rope(qf, r_qf, 8, 128, 16, 0)
                    fw.op("dve", lambda e: e.tensor_scalar(out=kb[:], in0=qf[:], scalar1=128.0 ** -0.5, scalar2=None, op0=ALU.mult), reads=[r_qf], writes=[r_kb])
                    bkt, r_bkt = banks[4]
                    bktb = bkt[:].bitcast(BF16)
                    for h in range(8):
                        fw.op("pe", lambda e, h=h, bktb=bktb: e.transpose(out=bktb[:, h * 128:(h + 1) * 128], in_=kb[:, h * 128:(h + 1) * 128], identity=ident_b),
                              reads=[r_kb, r_cstb], writes=[r_bkt], skip_self=True)
                    fw.op("act", lambda e, bktb=bktb: e.activation(out=QT[:], in_=bktb.rearrange("p (h t) -> p h t", h=8), func=ACT.Copy), reads=[r_bkt], writes=[r_QT])
                    bk, r_bk = banks[5]
                    for ch in range(8):
                        fw.op("pe", lambda e, ch=ch, bk=bk: e.matmul(bk[:, 0:260], lhsT=xT[:, ch, :], rhs=wiq[:, ch, :], start=(ch == 0), stop=(ch == 7)),
                              reads=[r_xT, r_wiq], writes=[r_bk], skip_self=True)
                    fw.op("act", lambda e, bk=bk: e.activation(out=qf[:, 0:260], in_=bk[:, 0:260], func=ACT.Copy), reads=[r_bk], writes=[r_qf])
                    rope(qf, r_qf, 4, 64, 8, 16)
                    fw.op("dve", lambda e: e.tensor_scalar(out=wsg[:, 0:4], in0=qf[:, 256:260], scalar1=0.0625, scalar2=None, op0=ALU.mult), reads=[r_qf], writes=[r_wsg])
                    fw.op("dve", lambda e: e.tensor_scalar(out=wsg[:, 12:16], in0=wsg[:, 0:4], scalar1=-1.0, scalar2=None, op0=ALU.mult), reads=[r_wsg], writes=[r_wsg])
                    fw.op("dve", lambda e: e.tensor_tensor(out=wsg[:, 4:8], in0=wsg[:, 0:4], in1=wsg[:, 12:16], op=ALU.max), reads=[r_wsg], writes=[r_wsg])
                    fw.op("dve", lambda e: e.tensor_scalar(out=wsg[:, 8:12], in0=wsg[:, 0:4], scalar1=0.0, scalar2=2.0, op0=ALU.is_ge, op1=ALU.mult), reads=[r_wsg], writes=[r_wsg])
                    fw.op("dve", lambda e: e.tensor_scalar(out=wsg[:, 8:12], in0=wsg[:, 8:12], scalar1=-1.0, scalar2=None, op0=ALU.add), reads=[r_wsg], writes=[r_wsg])
                    fw.op("dve", lambda e: e.tensor_tensor(out=kb[:, 0:512].rearrange("p (h r d) -> p h r d", h=4, r=2),
                                                           in0=qf[:, 0:256].rearrange("p (h d) -> p h d", d=64).unsqueeze(2).to_broadcast([128, 4, 2, 64]),
                                                           in1=wsg[:, 4:8].unsqueeze(2).unsqueeze(3).to_broadcast([128, 4, 2, 64]), op=ALU.mult), reads=[r_qf, r_wsg], writes=[r_kb])
                    bkt, r_bkt = banks[6]
                    bktb = bkt[:].bitcast(BF16)
                    for h in range(4):
                        fw.op("pe", lambda e, h=h, bktb=bktb: e.transpose(out=bktb[:, h * 128:(h + 1) * 128], in_=kb[:, h * 128:(h + 1) * 128], identity=ident_b),
                              reads=[r_kb, r_cstb], writes=[r_bkt], skip_self=True)
                    fw.op("act", lambda e, bktb=bktb: e.activation(out=iqT[:], in_=bktb[:, 0:512].rearrange("p (h t) -> p h t", h=4), func=ACT.Copy), reads=[r_bkt], writes=[r_iqT])
                    Ls = NCTX + 128 * (it + 1)
                    ngr = (Ls + 511) // 512
                    for sg in range(ngr):
                        wd = min(512, Ls - sg * 512)
                        pr = slice(0, 64) if sg < 8 else slice(64, 128)
                        c0 = (sg % 8) * 512
                        for h in range(4):
                            bk, r_bk = banks[2 + ((sg * 4 + h) % 2)]
                            rlb, r_rlb = rl[(sg * 4 + h) % 2]
                            fw.op("pe", lambda e, h=h, wd=wd, bk=bk, pr=pr, c0=c0: e.matmul(bk[:, 0:wd], lhsT=iqT[pr, h, :], rhs=ikT[pr, c0:c0 + wd], start=True, stop=True),
                                  reads=[r_iqT, r_ikT], writes=[r_bk], skip_self=True)
                            fw.op("act", lambda e, wd=wd, bk=bk, rlb=rlb: e.activation(out=rlb[:, 0:wd], in_=bk[:, 0:wd], func=ACT.Relu), reads=[r_bk], writes=[r_rlb])
                            if h == 0:
                                fw.op("dve", lambda e, sg=sg, wd=wd, rlb=rlb: e.tensor_scalar(out=S[:, sg * 512:sg * 512 + wd], in0=rlb[:, 0:wd], scalar1=wsg[:, 8:9], scalar2=None, op0=ALU.mult),
                                      reads=[r_rlb, r_wsg], writes=[r_S])
                            else:
                                fw.op("dve", lambda e, sg=sg, wd=wd, rlb=rlb, h=h: e.scalar_tensor_tensor(out=S[:, sg * 512:sg * 512 + wd], in0=rlb[:, 0:wd], scalar=wsg[:, 8 + h:9 + h],
                                                                                                         in1=S[:, sg * 512:sg * 512 + wd], op0=ALU.mult, op1=ALU.add),
                                      reads=[r_rlb, r_wsg, r_S], writes=[r_S])
                    fw.op("dve", lambda e, Ls=Ls: e.tensor_reduce(out=bis[:, 0:1], in_=S[:, 0:Ls], axis=AX.X, op=ALU.max), reads=[r_S], writes=[r_bis])
                    fw.op("dve", lambda e, Ls=Ls: e.tensor_reduce(out=bis[:, 1:2], in_=S[:, 0:Ls], axis=AX.X, op=ALU.min), reads=[r_S], writes=[r_bis])
                    for cs in range(6):
                        fw.op("pool", lambda e, cs=cs: e.tensor_scalar(out=S[:, cs * 1024:(cs + 1) * 1024], in0=S[:, cs * 1024:(cs + 1) * 1024], scalar1=penx[:, cs:cs + 1], scalar2=None, op0=ALU.add),
                              reads=[r_S, r_penx], writes=[r_S])
                    fw.op("dve", lambda e, Ls=Ls: e.tensor_tensor(out=S[:, Ls - 128:Ls], in0=S[:, Ls - 128:Ls], in1=tri_pen, op=ALU.add), reads=[r_S, r_cst], writes=[r_S])
                    fw.op("dve", lambda e: e.tensor_tensor(out=bis[:, 3:4], in0=bis[:, 0:1], in1=bis[:, 1:2], op=ALU.subtract), reads=[r_bis], writes=[r_bis])
                    fw.op("dve", lambda e: e.tensor_scalar(out=bis[:, 3:4], in0=bis[:, 3:4], scalar1=0.501, scalar2=1e-6, op0=ALU.mult, op1=ALU.add), reads=[r_bis], writes=[r_bis])
                    fw.op("dve", lambda e: e.scalar_tensor_tensor(out=bis[:, 2:3], in0=bis[:, 3:4], scalar=-0.002, in1=bis[:, 1:2], op0=ALU.mult, op1=ALU.add), reads=[r_bis], writes=[r_bis])
                    fw.op("dve", lambda e: e.tensor_scalar(out=bis[:, 2:3], in0=bis[:, 2:3], scalar1=-1e-6, scalar2=None, op0=ALU.add), reads=[r_bis], writes=[r_bis])
                    fw.op("dve", lambda e: e.tensor_scalar(out=bis[:, 3:4], in0=bis[:, 3:4], scalar1=2.0, scalar2=None, op0=ALU.mult), reads=[r_bis], writes=[r_bis])
                    La = (int(Ls * 0.45) // 128) * 128
                    nA = Ls - La
                    for itn in range(NITER):
                        fw.op("dve", lambda e: e.tensor_scalar(out=bis[:, 3:4], in0=bis[:, 3:4], scalar1=0.5, scalar2=None, op0=ALU.mult), reads=[r_bis], writes=[r_bis])
                        fw.op("dve", lambda e: e.tensor_tensor(out=bis[:, 4:5], in0=bis[:, 2:3], in1=bis[:, 3:4], op=ALU.add), reads=[r_bis], writes=[r_bis])
                        fw.op("dve", lambda e: e.tensor_scalar(out=bis2[:, 0:1], in0=bis[:, 4:5], scalar1=-1.0, scalar2=None, op0=ALU.mult), reads=[r_bis], writes=[r_bis2])
                        fw.op("act", lambda e, La=La, Ls=Ls: e.activation(out=mk2[:, La:Ls], in_=S[:, La:Ls], func=ACT.Sign, bias=bis2[:, 0:1], accum_out=bis2[:, 1:2]),
                              reads=[r_S, r_bis2], writes=[r_mk2, r_bis2b])
                        fw.op("dve", lambda e, La=La: e.tensor_scalar(out=mk[:, 0:La], in0=S[:, 0:La], scalar1=bis[:, 4:5], scalar2=None, op0=ALU.is_ge, op1=ALU.add, accum_out=bis[:, 5:6]),
                              reads=[r_S, r_bis], writes=[r_mk, r_bis])
                        fw.op("dve", lambda e: e.scalar_tensor_tensor(out=bis[:, 5:6], in0=bis2[:, 1:2], scalar=0.5, in1=bis[:, 5:6], op0=ALU.mult, op1=ALU.add), reads=[r_bis, r_bis2b], writes=[r_bis])
                        fw.op("dve", lambda e, nA=nA: e.tensor_single_scalar(out=bis[:, 6:7], in_=bis[:, 5:6], scalar=255.5 - 0.5 * nA, op=ALU.is_ge), reads=[r_bis], writes=[r_bis])
                        fw.op("dve", lambda e: e.scalar_tensor_tensor(out=bis[:, 2:3], in0=bis[:, 6:7], scalar=bis[:, 3:4], in1=bis[:, 2:3], op0=ALU.mult, op1=ALU.add), reads=[r_bis], writes=[r_bis])
                    fw.op("dve", lambda e, Ls=Ls: e.tensor_scalar(out=mk[:, 0:Ls], in0=S[:, 0:Ls], scalar1=bis[:, 2:3], scalar2=None, op0=ALU.is_ge), reads=[r_S, r_bis], writes=[r_mk, r_mk2])
                    nb = Ls // 128
                    for b0 in range(0, nb, 8):
                        nn = min(8, nb - b0)
                        bkt, r_bkt = banks[4 + ((b0 // 8) % 2)]
                        bktb = bkt[:].bitcast(BF16)
                        for bb in range(nn):
                            fw.op("pe", lambda e, bb=bb, b0=b0, bktb=bktb: e.transpose(out=bktb[:, bb * 128:(bb + 1) * 128], in_=mk[:, (b0 + bb) * 128:(b0 + bb + 1) * 128], identity=ident_b),
                                  reads=[r_mk, r_mk2, r_cstb], writes=[r_bkt], skip_self=True)
                        fw.op("act", lambda e, b0=b0, nn=nn, bktb=bktb: e.activation(out=maskT[:, b0:b0 + nn, :], in_=bktb[:, 0:nn * 128].rearrange("p (b t) -> p b t", t=128), func=ACT.Copy),
                              reads=[r_bkt], writes=[r_maskT])
                    nblk = nb
                    def oslot(h):
                        bko, r_bko = banks[h // 3]
                        return bko[:, (h % 3) * 129:(h % 3) * 129 + 129], r_bko
                    cnt = 0
                    for sb in range(nblk):
                        for hq in range(2):
                            bkq, r_bkq = banks[4 + (cnt % 4)]
                            eLb, r_eLb = eL[cnt % 4]
                            PTb, r_PTb = PT[cnt % 4]
                            cnt += 1
                            for j in range(4):
                                h = hq * 4 + j
                                fw.op("pe", lambda e, h=h, j=j, sb=sb, hq=hq, bkq=bkq: e.matmul(bkq[:, j * 128:(j + 1) * 128], lhsT=KT[:, hq, sb * 128:(sb + 1) * 128], rhs=QT[:, h, :], start=True, stop=True),
                                      reads=[r_KT, r_QT], writes=[r_bkq], skip_self=True)
                            fw.op("act", lambda e, bkq=bkq, eLb=eLb: e.activation(out=eLb[:], in_=bkq[:], func=ACT.Exp), reads=[r_bkq], writes=[r_eLb])
                            fw.op("dve", lambda e, eLb=eLb, PTb=PTb, sb=sb: e.tensor_tensor(out=PTb[:].rearrange("p (j t) -> p j t", j=4), in0=eLb[:].rearrange("p (j t) -> p j t", j=4),
                                                                                          in1=maskT[:, sb, :].unsqueeze(1).to_broadcast([128, 4, 128]), op=ALU.mult),
                                  reads=[r_eLb, r_maskT], writes=[r_PTb])
                            for j in range(4):
                                h = hq * 4 + j
                                oap, r_o = oslot(h)
                                fw.op("pe", lambda e, oap=oap, PTb=PTb, j=j, sb=sb, hq=hq: e.matmul(oap, lhsT=PTb[:, j * 128:(j + 1) * 128], rhs=Vx[:, sb, hq, :],
                                                                                                start=(sb == 0), stop=(sb == nblk - 1)),
                                      reads=[r_PTb, r_Vx], writes=[r_o], skip_self=True)
                    for h in range(8):
                        oap, r_o = oslot(h)
                        fw.op("dve", lambda e, oap=oap, h=h: e.reciprocal(out=rden[:, h:h + 1], in_=oap[:, 128:129]), reads=[r_o], writes=[r_rden])
                        fw.op("dve", lambda e, oap=oap, h=h: e.tensor_scalar(out=at[:, h * 128:(h + 1) * 128], in0=oap[:, 0:128], scalar1=rden[:, h:h + 1], scalar2=None, op0=ALU.mult),
                              reads=[r_o, r_rden], writes=[r_at])
                    t0 = it * 128
                    fw.dma("sp", attn_d[t0:t0 + 128, :], at[:], reads=[r_at])
                    if dbg:
                        fw.dma("sp", dbg_attn[t0:t0 + 128, :], at[:], reads=[r_at])
                fw.barrier()

        if 3 in phases:
            h2_d = nc.dram_tensor("h2_d", [TOWN, D], F32, kind="Internal").ap()
            with ExitStack() as es3:
                P = Pool(nc, es3)
                wstg = P.sb([128, 4096], F32, "wstg")
                wgate, r_wgate = P.sb([128, 8, 2048], BF16, "wgate")
                wa, r_wa = P.sb([128, 8, 1024], BF16, "wa")
                wo, r_wo = P.sb([128, 8, 1024], BF16, "wo")
                wcq, r_wcq = P.sb([128, 8, 1024], BF16, "wcq")
                wco, r_wco = P.sb([128, 8, 1024], BF16, "wco")
                gc_, r_gc = P.sb([128, 8], F32, "gc")
                gmm, r_gmm = P.sb([128, 8], F32, "gmm")
                fw.dma("sp", gc_[:], g_cross, writes=[r_gc])
                fw.dma("sp", gmm[:], g_mem, writes=[r_gmm])
                load_weight(wstg, wgate, r_wgate, w_in_r, OGATE, 2048, gm, r_gm)
                load_weight(wstg, wa, r_wa, w_rows(w_attn_branch), 0, 1024, None, None)
                load_weight(wstg, wo, r_wo, w_rows(w_out), 0, 1024, None, None)
                load_weight(wstg, wcq, r_wcq, w_rows(w_cross_q), 0, 1024, gc_, r_gc)
                load_weight(wstg, wco, r_wco, w_rows(w_cross_out), 0, 1024, None, None)
                xT, r_xT = P.sb([128, 8, 256], BF16, "xT3")
                xt_bufs = []
                xt, r_xt = P.sb([128, D], F32, "xt")
                xb, r_xb = P.sb([128, D], BF16, "xb")
                ss, r_ss = P.sb([128, 4], F32, "ss")
                xt_bufs = [(xt, r_xt, xb, r_xb, ss, r_ss)] * 2
                KcT, r_KcT = P.sb([128, 8, 256], BF16, "KcT")
                Vc, r_Vc = P.sb([128, 2, 1024], BF16, "Vc")
                with ExitStack() as es3a:
                    Pa = Pool(nc, es3a)
                    wckv, r_wckv = Pa.sb([128, 8, 2048], BF16, "wckv")
                    load_weight(wstg, wckv, r_wckv, w_rows(w_cross_kv), 0, 2048, gmm, r_gmm)
                    build_xT(P, 0, xT, r_xT, xt_bufs, ntile=2, src=lambda ti: mem[ti * 128:(ti + 1) * 128, :])
                    for blk in range(8):
                        bk, r_bk = banks[blk % 2]
                        for ch in range(8):
                            fw.op("pe", lambda e, ch=ch, blk=blk, bk=bk: e.matmul(bk[:, 0:256], lhsT=wckv[:, ch, blk * 128:(blk + 1) * 128], rhs=xT[:, ch, :], start=(ch == 0), stop=(ch == 7)),
                                  reads=[r_wckv, r_xT], writes=[r_bk], skip_self=True)
                        fw.op("act", lambda e, blk=blk, bk=bk: e.activation(out=KcT[:, blk, :], in_=bk[:, 0:256], func=ACT.Copy), reads=[r_bk], writes=[r_KcT])
                    for mc in range(2):
                        for cg in range(2):
                            bk, r_bk = banks[2 + cg]
                            for ch in range(8):
                                fw.op("pe", lambda e, ch=ch, mc=mc, cg=cg, bk=bk: e.matmul(bk[:], lhsT=xT[:, ch, mc * 128:(mc + 1) * 128], rhs=wckv[:, ch, 1024 + cg * 512:1024 + (cg + 1) * 512], start=(ch == 0), stop=(ch == 7)),
                                      reads=[r_wckv, r_xT], writes=[r_bk], skip_self=True)
                            fw.op("act", lambda e, mc=mc, cg=cg, bk=bk: e.activation(out=Vc[:, mc, cg * 512:(cg + 1) * 512], in_=bk[:], func=ACT.Copy), reads=[r_bk], writes=[r_Vc])
                    fw.barrier()
                gts, r_gts = P.sb([128, 2048], F32, "gts")
                atl, r_atl = P.sb([128, D], F32, "atl")
                spl, r_spl = P.sb([128, D], F32, "spl")
                bfb, r_bfb = P.sb([128, D], BF16, "bfb")
                tT, r_tT = P.sb([128, 8, 128], BF16, "tT")
                mg, r_mg = P.sb([128, D], F32, "mg")
                h1, r_h1 = P.sb([128, D], F32, "h1")
                sq, r_sq = P.sb([128, 8], F32, "sq")
                qcT, r_qcT = P.sb([128, 8, 128], BF16, "qcT")
                pc, r_pc = P.sb([128, 4, 256], F32, "pc")
                pcb, r_pcb = P.sb([128, 4, 256], BF16, "pcb")
                pT, r_pT = P.sb([128, 8, 128], BF16, "pT")
                oT, r_oT = P.sb([128, 8, 128], BF16, "oT")
                sm, r_sm = P.sb([128, 16], F32, "sm")
                junk, r_junk = P.sb([128, D], F32, "junk3")

                def to_T(src_bf, r_src, dstT, r_dst, bank_i):
                    bkt, r_bkt = banks[bank_i]
                    bktb = bkt[:].bitcast(BF16)
                    for ch in range(8):
                        fw.op("pe", lambda e, ch=ch: e.transpose(out=bktb[:, ch * 128:(ch + 1) * 128], in_=src_bf[:, ch * 128:(ch + 1) * 128], identity=ident_b),
                              reads=[r_src, r_cstb], writes=[r_bkt], skip_self=True)
                    fw.op("act", lambda e: e.activation(out=dstT[:], in_=bktb.rearrange("p (c t) -> p c t", c=8), func=ACT.Copy), reads=[r_bkt], writes=[r_dst])

                def rstd_of(src, r_src, col, n=D):
                    fw.op("act", lambda e: e.activation(out=junk[:], in_=src[:], func=ACT.Square, accum_out=sq[:, col:col + 1]), reads=[r_src], writes=[r_junk, r_sq])
                    fw.op("dve", lambda e: e.tensor_scalar(out=sq[:, col + 1:col + 2], in0=sq[:, col:col + 1], scalar1=1.0 / n, scalar2=EPS, op0=ALU.mult, op1=ALU.add), reads=[r_sq], writes=[r_sq])
                    fw.op("act", lambda e: e.activation(out=sq[:, col + 2:col + 3], in_=sq[:, col + 1:col + 2], func=ACT.Ln), reads=[r_sq], writes=[r_sq])
                    fw.op("act", lambda e: e.activation(out=sq[:, col + 3:col + 4], in_=sq[:, col + 2:col + 3], func=ACT.Exp, scale=-0.5), reads=[r_sq], writes=[r_sq])
                    return sq[:, col + 3:col + 4]

                for it in range(16):
                    seg, ti = 6 + it // 8, it % 8
                    t0 = it * 128
                    build_xT(P, seg, xT[:, :, 0:128], r_xT, xt_bufs, ntile=1, src=lambda _t, seg=seg, ti=ti: x_seg[seg, ti * 128:(ti + 1) * 128, :])
                    for cg in range(4):
                        bk, r_bk = banks[2 + (cg % 2)]
                        for ch in range(8):
                            fw.op("pe", lambda e, ch=ch, cg=cg, bk=bk: e.matmul(bk[:], lhsT=xT[:, ch, 0:128], rhs=wgate[:, ch, cg * 512:(cg + 1) * 512], start=(ch == 0), stop=(ch == 7)),
                                  reads=[r_xT, r_wgate], writes=[r_bk], skip_self=True)
                        fw.op("act", lambda e, cg=cg, bk=bk: e.activation(out=gts[:, cg * 512:(cg + 1) * 512], in_=bk[:], func=ACT.Sigmoid), reads=[r_bk], writes=[r_gts])
                    fw.dma("sp", atl[:], attn_d[t0:t0 + 128, :], writes=[r_atl])
                    fw.dma("sp", spl[:], sproj_d[t0:t0 + 128, :], writes=[r_spl])
                    fw.op("dve", lambda e: e.tensor_copy(out=bfb[:], in_=atl[:]), reads=[r_atl], writes=[r_bfb])
                    to_T(bfb, r_bfb, tT, r_tT, 4)
                    for cg in range(2):
                        bk, r_bk = banks[2 + cg]
                        for ch in range(8):
                            fw.op("pe", lambda e, ch=ch, cg=cg, bk=bk: e.matmul(bk[:], lhsT=tT[:, ch, :], rhs=wa[:, ch, cg * 512:(cg + 1) * 512], start=(ch == 0), stop=(ch == 7)),
                                  reads=[r_tT, r_wa], writes=[r_bk], skip_self=True)
                        fw.op("dve", lambda e, cg=cg, bk=bk: e.tensor_tensor(out=mg[:, cg * 512:(cg + 1) * 512], in0=bk[:], in1=gts[:, cg * 512:(cg + 1) * 512], op=ALU.mult),
                              reads=[r_bk, r_gts], writes=[r_mg])
                    fw.op("dve", lambda e: e.tensor_tensor(out=spl[:], in0=spl[:], in1=gts[:, 1024:2048], op=ALU.mult), reads=[r_spl, r_gts], writes=[r_spl])
                    fw.op("dve", lambda e: e.tensor_tensor(out=mg[:], in0=mg[:], in1=spl[:], op=ALU.add), reads=[r_mg, r_spl], writes=[r_mg])
                    fw.op("dve", lambda e: e.tensor_copy(out=bfb[:], in_=mg[:]), reads=[r_mg], writes=[r_bfb])
                    to_T(bfb, r_bfb, tT, r_tT, 5)
                    xt_cur, r_xt_cur = xt_bufs[0][0], xt_bufs[0][1]
                    for cg in range(2):
                        bk, r_bk = banks[2 + cg]
                        for ch in range(8):
                            fw.op("pe", lambda e, ch=ch, cg=cg, bk=bk: e.matmul(bk[:], lhsT=tT[:, ch, :], rhs=wo[:, ch, cg * 512:(cg + 1) * 512], start=(ch == 0), stop=(ch == 7)),
                                  reads=[r_tT, r_wo], writes=[r_bk], skip_self=True)
                        fw.op("dve", lambda e, cg=cg, bk=bk: e.tensor_tensor(out=h1[:, cg * 512:(cg + 1) * 512], in0=bk[:], in1=xt_cur[:, cg * 512:(cg + 1) * 512], op=ALU.add),
                              reads=[r_bk, r_xt_cur], writes=[r_h1])
                    rs = rstd_of(h1, r_h1, 0)
                    fw.op("dve", lambda e: e.tensor_scalar(out=bfb[:], in0=h1[:], scalar1=rs, scalar2=None, op0=ALU.mult), reads=[r_h1, r_sq], writes=[r_bfb])
                    to_T(bfb, r_bfb, tT, r_tT, 4)
                    for half in range(2):
                        bk, r_bk = banks[2 + half]
                        for b4 in range(4):
                            blk = half * 4 + b4
                            for ch in range(8):
                                fw.op("pe", lambda e, ch=ch, blk=blk, b4=b4, bk=bk: e.matmul(bk[:, b4 * 128:(b4 + 1) * 128], lhsT=wcq[:, ch, blk * 128:(blk + 1) * 128], rhs=tT[:, ch, :], start=(ch == 0), stop=(ch == 7)),
                                      reads=[r_wcq, r_tT], writes=[r_bk], skip_self=True)
                        fw.op("act", lambda e, half=half, bk=bk: e.activation(out=qcT[:, half * 4:(half + 1) * 4, :], in_=bk[:].rearrange("p (b t) -> p b t", b=4), func=ACT.Copy, scale=1.0 / 16.0),
                              reads=[r_bk], writes=[r_qcT])
                    for hp in range(2):
                        bk, r_bk = banks[2 + hp]
                        for j in range(2):
                            h = hp * 2 + j
                            for c in range(2):
                                fw.op("pe", lambda e, h=h, j=j, c=c, bk=bk: e.matmul(bk[:, j * 256:(j + 1) * 256], lhsT=qcT[:, 2 * h + c, :], rhs=KcT[:, 2 * h + c, :], start=(c == 0), stop=(c == 1)),
                                      reads=[r_qcT, r_KcT], writes=[r_bk], skip_self=True)
                        fw.op("act", lambda e, hp=hp, bk=bk: e.activation(out=pc[:, hp * 2:(hp + 1) * 2, :], in_=bk[:].rearrange("p (j m) -> p j m", j=2), func=ACT.Copy), reads=[r_bk], writes=[r_pc])
                    fw.op("dve", lambda e: e.tensor_reduce(out=sm[:, 0:4], in_=pc[:], axis=AX.X, op=ALU.max), reads=[r_pc], writes=[r_sm])
                    fw.op("dve", lambda e: e.tensor_scalar(out=sm[:, 4:8], in0=sm[:, 0:4], scalar1=-1.0, scalar2=None, op0=ALU.mult), reads=[r_sm], writes=[r_sm])
                    for h in range(4):
                        fw.op("act", lambda e, h=h: e.activation(out=pc[:, h, :], in_=pc[:, h, :], func=ACT.Exp, bias=sm[:, 4 + h:5 + h], accum_out=sm[:, 8 + h:9 + h]), reads=[r_pc, r_sm], writes=[r_pc, r_sm])
                    fw.op("dve", lambda e: e.reciprocal(out=sm[:, 12:16], in_=sm[:, 8:12]), reads=[r_sm], writes=[r_sm])
                    fw.op("dve", lambda e: e.tensor_tensor(out=pcb[:], in0=pc[:], in1=sm[:, 12:16].unsqueeze(2).to_broadcast([128, 4, 256]), op=ALU.mult), reads=[r_pc, r_sm], writes=[r_pcb])
                    bkt, r_bkt = banks[4]
                    bktb = bkt[:].bitcast(BF16)
                    for h in range(4):
                        for mc in range(2):
                            fw.op("pe", lambda e, h=h, mc=mc: e.transpose(out=bktb[:, (2 * h + mc) * 128:(2 * h + mc + 1) * 128], in_=pcb[:, h, mc * 128:(mc + 1) * 128], identity=ident_b),
                                  reads=[r_pcb, r_cstb], writes=[r_bkt], skip_self=True)
                    fw.op("act", lambda e: e.activation(out=pT[:], in_=bktb.rearrange("p (c t) -> p c t", c=8), func=ACT.Copy), reads=[r_bkt], writes=[r_pT])
                    for half in range(2):
                        bk, r_bk = banks[2 + half]
                        for b4 in range(4):
                            blk = half * 4 + b4
                            h, dc = blk // 2, blk % 2
                            for mc in range(2):
                                fw.op("pe", lambda e, h=h, dc=dc, mc=mc, b4=b4, bk=bk: e.matmul(bk[:, b4 * 128:(b4 + 1) * 128], lhsT=Vc[:, mc, h * 256 + dc * 128:h * 256 + (dc + 1) * 128], rhs=pT[:, 2 * h + mc, :],
                                                                                        start=(mc == 0), stop=(mc == 1)),
                                      reads=[r_Vc, r_pT], writes=[r_bk], skip_self=True)
                        fw.op("act", lambda e, half=half, bk=bk: e.activation(out=oT[:, half * 4:(half + 1) * 4, :], in_=bk[:].rearrange("p (b t) -> p b t", b=4), func=ACT.Copy), reads=[r_bk], writes=[r_oT])
                    for cg in range(2):
                        bk, r_bk = banks[2 + cg]
                        for ch in range(8):
                            fw.op("pe", lambda e, ch=ch, cg=cg, bk=bk: e.matmul(bk[:], lhsT=oT[:, ch, :], rhs=wco[:, ch, cg * 512:(cg + 1) * 512], start=(ch == 0), stop=(ch == 7)),
                                  reads=[r_oT, r_wco], writes=[r_bk], skip_self=True)
                        fw.op("dve", lambda e, cg=cg, bk=bk: e.tensor_tensor(out=mg[:, cg * 512:(cg + 1) * 512], in0=bk[:], in1=h1[:, cg * 512:(cg + 1) * 512], op=ALU.add),
                              reads=[r_bk, r_h1], writes=[r_mg])
                    fw.dma("sp", h2_d[t0:t0 + 128, :], mg[:], reads=[r_mg])
                fw.barrier()

            with ExitStack() as es4:
                P = Pool(nc, es4)
                wstg = P.sb([128, 4096], F32, "wstg")
                wpq, r_wpq = P.sb([128, 8, 2048], BF16, "wpq")
                gfc, r_gfc = P.sb([128, 8], F32, "gfc")
                fw.dma("sp", gfc[:], g_ffn_c, writes=[r_gfc])
                load_weight(wstg, wpq, r_wpq, w_rows(w_peer_q), 0, 2048, gfc, r_gfc)
                gff, r_gff = P.sb([128, D], F32, "gff")
                gfin, r_gfin = P.sb([128, D], F32, "gfin")
                fw.dma("sp", gff[:], g_ffn, writes=[r_gff])
                fw.dma("sp", gfin[:], g_final, writes=[r_gfin])
                skT, r_skT = P.sb([128, 16, 128], BF16, "skT")
                skf, r_skf = P.sb([128, 16, 128], F32, "skf")
                skb, r_skb = P.sb([128, 16, 128], BF16, "skb")
                fw.dma("sp", skf[:], sub_keys.rearrange("b k d -> k b d"), writes=[r_skf])
                fw.op("dve", lambda e: e.tensor_copy(out=skb[:], in_=skf[:]), reads=[r_skf], writes=[r_skb])
                for half in range(2):
                    bkt, r_bkt = banks[half]
                    bktb = bkt[:].bitcast(BF16)
                    for i in range(8):
                        fw.op("pe", lambda e, i=i, half=half, bktb=bktb: e.transpose(out=bktb[:, i * 128:(i + 1) * 128], in_=skb[:, half * 8 + i, :], identity=ident_b),
                              reads=[r_skb, r_cstb], writes=[r_bkt], skip_self=True)
                    fw.op("act", lambda e, half=half, bktb=bktb: e.activation(out=skT[:, half * 8:(half + 1) * 8, :], in_=bktb.rearrange("p (c t) -> p c t", c=8), func=ACT.Copy), reads=[r_bkt], writes=[r_skT])
                iota16, r_iota = P.sb([128, 16], F32, "iota16")
                for i in range(16):
                    fw.op("pool", lambda e, i=i: e.memset(iota16[:, i:i + 1], float(i)), writes=[r_iota])
                h2, r_h2 = P.sb([128, D], F32, "h2")
                a3, r_a3 = P.sb([128, D], F32, "a3")
                bfb, r_bfb = P.sb([128, D], BF16, "bfb4")
                tT, r_tT = P.sb([128, 8, 128], BF16, "tT4")
                qpT, r_qpT = P.sb([128, 16, 128], BF16, "qpT")
                sc, r_sc = P.sb([128, 16, 128], F32, "sc")
                scw, r_scw = P.sb([128, 16, 128], F32, "scw")
                tops, r_tops = P.sb([128, 16, 16], F32, "tops")
                topi, r_topi = P.sb([128, 16, 16], U32, "topi")
                topf, r_topf = P.sb([128, 16, 16], F32, "topf")
                cand, r_cand = P.sb([128, 8, 256], F32, "cand")
                candw, r_candw = P.sb([128, 8, 256], F32, "candw")
                selv, r_selv = P.sb([128, 8, 16], F32, "selv")
                seli, r_seli = P.sb([128, 8, 16], U32, "seli")
                selt, r_selt = P.sb([128, 8, 16], U32, "selt")
                sif, r_sif = P.sb([128, 8, 16], F32, "sif")
                sjf, r_sjf = P.sb([128, 8, 16], F32, "sjf")
                oh, r_oh = P.sb([128, 8, 16, 16], F32, "oh")
                eaf, r_eaf = P.sb([128, 8, 16], F32, "eaf")
                ebf, r_ebf = P.sb([128, 8, 16], F32, "ebf")
                eidx, r_eidx = P.sb([128, 128], I32, "eidx")
                gw, r_gw = P.sb([128, 8, 16], F32, "gw")
                g8, r_g8 = P.sb([128, 16], F32, "g8")
                dots, r_dots = P.sb([128, 128], F32, "dots")
                wts, r_wts = P.sb([128, 128], F32, "wts")
                Ub = [P.sb([128, D], F32, "Ub") for _ in range(8)]
                prod, r_prod = P.sb([128, D], F32, "prod")
                acc, r_acc = P.sb([128, D], F32, "acc")
                sq, r_sq = P.sb([128, 8], F32, "sq4")
                junk, r_junk = P.sb([128, D], F32, "junk4")
                for it in range(16):
                    t0 = it * 128
                    fw.dma("sp", h2[:], h2_d[t0:t0 + 128, :], writes=[r_h2])
                    fw.op("act", lambda e: e.activation(out=junk[:], in_=h2[:], func=ACT.Square, accum_out=sq[:, 0:1]), reads=[r_h2], writes=[r_junk, r_sq])
                    fw.op("dve", lambda e: e.tensor_scalar(out=sq[:, 1:2], in0=sq[:, 0:1], scalar1=1.0 / D, scalar2=EPS, op0=ALU.mult, op1=ALU.add), reads=[r_sq], writes=[r_sq])
                    fw.op("act", lambda e: e.activation(out=sq[:, 2:3], in_=sq[:, 1:2], func=ACT.Ln), reads=[r_sq], writes=[r_sq])
                    fw.op("act", lambda e: e.activation(out=sq[:, 3:4], in_=sq[:, 2:3], func=ACT.Exp, scale=-0.5), reads=[r_sq], writes=[r_sq])
                    fw.op("dve", lambda e: e.tensor_scalar(out=bfb[:], in0=h2[:], scalar1=sq[:, 3:4], scalar2=None, op0=ALU.mult), reads=[r_h2, r_sq], writes=[r_bfb])
                    fw.op("dve", lambda e: e.scalar_tensor_tensor(out=a3[:], in0=h2[:], scalar=sq[:, 3:4], in1=gff[:], op0=ALU.mult, op1=ALU.mult), reads=[r_h2, r_sq, r_gff], writes=[r_a3])
                    bkt, r_bkt = banks[0]
                    bktb = bkt[:].bitcast(BF16)
                    for ch in range(8):
                        fw.op("pe", lambda e, ch=ch: e.transpose(out=bktb[:, ch * 128:(ch + 1) * 128], in_=bfb[:, ch * 128:(ch + 1) * 128], identity=ident_b),
                              reads=[r_bfb, r_cstb], writes=[r_bkt], skip_self=True)
                    fw.op("act", lambda e: e.activation(out=tT[:], in_=bktb.rearrange("p (c t) -> p c t", c=8), func=ACT.Copy), reads=[r_bkt], writes=[r_tT])
                    for q4 in range(4):
                        bk, r_bk = banks[1 + (q4 % 2)]
                        for b4 in range(4):
                            blk = q4 * 4 + b4
                            for ch in range(8):
                                fw.op("pe", lambda e, ch=ch, blk=blk, b4=b4, bk=bk: e.matmul(bk[:, b4 * 128:(b4 + 1) * 128], lhsT=wpq[:, ch, blk * 128:(blk + 1) * 128], rhs=tT[:, ch, :], start=(ch == 0), stop=(ch == 7)),
                                      reads=[r_wpq, r_tT], writes=[r_bk], skip_self=True)
                        fw.op("act", lambda e, q4=q4, bk=bk: e.activation(out=qpT[:, q4 * 4:(q4 + 1) * 4, :], in_=bk[:].rearrange("p (b t) -> p b t", b=4), func=ACT.Copy), reads=[r_bk], writes=[r_qpT])
                    for q4 in range(4):
                        bk, r_bk = banks[3 + (q4 % 2)]
                        for b4 in range(4):
                            blk = q4 * 4 + b4
                            fw.op("pe", lambda e, blk=blk, b4=b4, bk=bk: e.matmul(bk[:, b4 * 128:(b4 + 1) * 128], lhsT=qpT[:, blk, :], rhs=skT[:, blk, :], start=True, stop=True),
                                  reads=[r_qpT, r_skT], writes=[r_bk], skip_self=True)
                        fw.op("act", lambda e, q4=q4, bk=bk: e.activation(out=sc[:, q4 * 4:(q4 + 1) * 4, :], in_=bk[:].rearrange("p (b t) -> p b t", b=4), func=ACT.Copy), reads=[r_bk], writes=[r_sc])
                    for blk in range(16):
                        fw.op("dve", lambda e, blk=blk: e.max(out=tops[:, blk, 0:8], in_=sc[:, blk, :]), reads=[r_sc], writes=[r_tops])
                        fw.op("dve", lambda e, blk=blk: e.max_index(out=topi[:, blk, 0:8], in_max=tops[:, blk, 0:8], in_values=sc[:, blk, :]), reads=[r_sc, r_tops], writes=[r_topi])
                        fw.op("dve", lambda e, blk=blk: e.match_replace(out=scw[:, blk, :], in_to_replace=tops[:, blk, 0:8], in_values=sc[:, blk, :], imm_value=NEG), reads=[r_sc, r_tops], writes=[r_scw])
                        fw.op("dve", lambda e, blk=blk: e.max(out=tops[:, blk, 8:16], in_=scw[:, blk, :]), reads=[r_scw], writes=[r_tops])
                        fw.op("dve", lambda e, blk=blk: e.max_index(out=topi[:, blk, 8:16], in_max=tops[:, blk, 8:16], in_values=scw[:, blk, :]), reads=[r_scw, r_tops], writes=[r_topi])
                    fw.op("dve", lambda e: e.tensor_copy(out=topf[:], in_=topi[:]), reads=[r_topi], writes=[r_topf])
                    tv = tops[:].rearrange("p (h q) k -> p h q k", q=2)
                    fw.op("dve", lambda e: e.tensor_tensor(out=cand[:].rearrange("p h (i j) -> p h i j", j=16), in0=tv[:, :, 0, :].unsqueeze(3).to_broadcast([128, 8, 16, 16]),
                                                           in1=tv[:, :, 1, :].unsqueeze(2).to_broadcast([128, 8, 16, 16]), op=ALU.add), reads=[r_tops], writes=[r_cand])
                    for h in range(8):
                        fw.op("dve", lambda e, h=h: e.max(out=selv[:, h, 0:8], in_=cand[:, h, :]), reads=[r_cand], writes=[r_selv])
                        fw.op("dve", lambda e, h=h: e.max_index(out=seli[:, h, 0:8], in_max=selv[:, h, 0:8], in_values=cand[:, h, :]), reads=[r_cand, r_selv], writes=[r_seli])
                        fw.op("dve", lambda e, h=h: e.match_replace(out=candw[:, h, :], in_to_replace=selv[:, h, 0:8], in_values=cand[:, h, :], imm_value=NEG), reads=[r_cand, r_selv], writes=[r_candw])
                        fw.op("dve", lambda e, h=h: e.max(out=selv[:, h, 8:16], in_=candw[:, h, :]), reads=[r_candw], writes=[r_selv])
                        fw.op("dve", lambda e, h=h: e.max_index(out=seli[:, h, 8:16], in_max=selv[:, h, 8:16], in_values=candw[:, h, :]), reads=[r_candw, r_selv], writes=[r_seli])
                    fw.op("dve", lambda e: e.tensor_single_scalar(out=selt[:], in_=seli[:], scalar=4, op=ALU.logical_shift_right), reads=[r_seli], writes=[r_selt])
                    fw.op("dve", lambda e: e.tensor_copy(out=sif[:], in_=selt[:]), reads=[r_selt], writes=[r_sif])
                    fw.op("dve", lambda e: e.tensor_single_scalar(out=selt[:], in_=seli[:], scalar=15, op=ALU.bitwise_and), reads=[r_seli], writes=[r_selt])
                    fw.op("dve", lambda e: e.tensor_copy(out=sjf[:], in_=selt[:]), reads=[r_selt], writes=[r_sjf])
                    tf = topf[:].rearrange("p (h q) k -> p h q k", q=2)
                    for (srcf, r_srcf, q, dst, r_dst) in ((sif, r_sif, 0, eaf, r_eaf), (sjf, r_sjf, 1, ebf, r_ebf)):
                        fw.op("dve", lambda e, srcf=srcf: e.tensor_tensor(out=oh[:], in0=srcf[:].unsqueeze(3).to_broadcast([128, 8, 16, 16]),
                                                                         in1=iota16[:].unsqueeze(1).unsqueeze(2).to_broadcast([128, 8, 16, 16]), op=ALU.is_equal),
                              reads=[r_srcf, r_iota], writes=[r_oh])
                        fw.op("dve", lambda e, q=q: e.tensor_tensor(out=oh[:], in0=oh[:], in1=tf[:, :, q, :].unsqueeze(2).to_broadcast([128, 8, 16, 16]), op=ALU.mult),
                              reads=[r_oh, r_topf], writes=[r_oh])
                        fw.op("dve", lambda e, dst=dst: e.tensor_reduce(out=dst[:], in_=oh[:], axis=AX.X, op=ALU.add), reads=[r_oh], writes=[r_dst])
                    fw.op("dve", lambda e: e.scalar_tensor_tensor(out=eaf[:], in0=eaf[:], scalar=128.0, in1=ebf[:], op0=ALU.mult, op1=ALU.add), reads=[r_eaf, r_ebf], writes=[r_eaf])
                    fw.op("dve", lambda e: e.tensor_copy(out=eidx[:], in_=eaf[:].rearrange("p h k -> p (h k)")), reads=[r_eaf], writes=[r_eidx])
                    fw.op("dve", lambda e: e.tensor_tensor(out=gw[:], in0=selv[:], in1=selv[:, :, 0:1].to_broadcast([128, 8, 16]), op=ALU.subtract), reads=[r_selv], writes=[r_gw])
                    fw.op("act", lambda e: e.activation(out=gw[:], in_=gw[:], func=ACT.Exp), reads=[r_gw], writes=[r_gw])
                    fw.op("dve", lambda e: e.tensor_reduce(out=g8[:, 0:8], in_=gw[:], axis=AX.X, op=ALU.add), reads=[r_gw], writes=[r_g8])
                    fw.op("dve", lambda e: e.reciprocal(out=g8[:, 8:16], in_=g8[:, 0:8]), reads=[r_g8], writes=[r_g8])
                    fw.op("dve", lambda e: e.tensor_tensor(out=gw[:], in0=gw[:], in1=g8[:, 8:16].unsqueeze(2).to_broadcast([128, 8, 16]), op=ALU.mult), reads=[r_gw, r_g8], writes=[r_gw])
                    for sl in range(128):
                        ub, r_ub = Ub[sl % 8]
                        fw.dma("pool", ub[:], peer_u, reads=[r_eidx], writes=[r_ub], indirect=bass.IndirectOffsetOnAxis(ap=eidx[:, sl:sl + 1], axis=0))
                        fw.op("dve", lambda e, ub=ub: e.tensor_tensor(out=prod[:], in0=ub[:], in1=a3[:], op=ALU.mult), reads=[r_ub, r_a3], writes=[r_prod])
                        fw.op("act", lambda e, sl=sl: e.activation(out=junk[:], in_=prod[:], func=ACT.Copy, accum_out=dots[:, sl:sl + 1]), reads=[r_prod], writes=[r_junk, r_dots])
                    fw.op("act", lambda e: e.activation(out=wts[:], in_=dots[:], func=ACT.Gelu), reads=[r_dots], writes=[r_wts])
                    fw.op("dve", lambda e: e.tensor_tensor(out=wts[:], in0=wts[:], in1=gw[:].rearrange("p h k -> p (h k)"), op=ALU.mult), reads=[r_wts, r_gw], writes=[r_wts])
                    for sl in range(128):
                        ub, r_ub = Ub[sl % 8]
                        fw.dma("pool", ub[:], peer_v, reads=[r_eidx], writes=[r_ub], indirect=bass.IndirectOffsetOnAxis(ap=eidx[:, sl:sl + 1], axis=0))
                        if sl == 0:
                            fw.op("dve", lambda e, ub=ub: e.tensor_scalar(out=acc[:], in0=ub[:], scalar1=wts[:, 0:1], scalar2=None, op0=ALU.mult), reads=[r_ub, r_wts], writes=[r_acc])
                        else:
                            fw.op("dve", lambda e, ub=ub, sl=sl: e.scalar_tensor_tensor(out=acc[:], in0=ub[:], scalar=wts[:, sl:sl + 1], in1=acc[:], op0=ALU.mult, op1=ALU.add),
                                  reads=[r_ub, r_wts, r_acc], writes=[r_acc])
                    fw.op("dve", lambda e: e.tensor_tensor(out=acc[:], in0=acc[:], in1=h2[:], op=ALU.add), reads=[r_acc, r_h2], writes=[r_acc])
                    fw.op("act", lambda e: e.activation(out=junk[:], in_=acc[:], func=ACT.Square, accum_out=sq[:, 4:5]), reads=[r_acc], writes=[r_junk, r_sq])
                    fw.op("dve", lambda e: e.tensor_scalar(out=sq[:, 5:6], in0=sq[:, 4:5], scalar1=1.0 / D, scalar2=EPS, op0=ALU.mult, op1=ALU.add), reads=[r_sq], writes=[r_sq])
                    fw.op("act", lambda e: e.activation(out=sq[:, 6:7], in_=sq[:, 5:6], func=ACT.Ln), reads=[r_sq], writes=[r_sq])
                    fw.op("act", lambda e: e.activation(out=sq[:, 7:8], in_=sq[:, 6:7], func=ACT.Exp, scale=-0.5), reads=[r_sq], writes=[r_sq])
                    fw.op("dve", lambda e: e.scalar_tensor_tensor(out=prod[:], in0=acc[:], scalar=sq[:, 7:8], in1=gfin[:], op0=ALU.mult, op1=ALU.mult), reads=[r_acc, r_sq, r_gfin], writes=[r_prod])
                    fw.dma("sp", y_out[t0:t0 + 128, :], prod[:], reads=[r_prod])
                fw.barrier()

        if 3 not in phases:
            zt, r_zt = P0.sb([128, D], F32, "zt")
            fw.op("pool", lambda e: e.memset(zt[:], 0.0), writes=[r_zt])
            for ti in range(16):
                fw.dma("sp", y_out[ti * 128:(ti + 1) * 128, :], zt[:], reads=[r_zt])
        fw.barrier()
    return nc


def _prep_inputs(inputs):
    f32 = np.float32
    x = np.asarray(inputs["x"], f32)
    pos = np.asarray(inputs["positions"])
    ident = np.eye(128, dtype=f32)
    k_ = np.arange(128)
    tri = (k_[:, None] <= k_[None, :]).astype(f32)
    ones = np.ones((128, 128), f32)
    tri_pen = np.where(k_[None, :] <= k_[:, None], 0.0, NEG).astype(f32)
    consts = np.concatenate([ident, tri, ones, tri_pen, np.zeros((128, 128), f32)], axis=1)
    inv16 = (500000.0 ** (-2.0 * np.arange(16, dtype=f32) / 32)).astype(f32)
    inv8 = (500000.0 ** (-2.0 * np.arange(8, dtype=f32) / 16)).astype(f32)
    invf = np.tile(np.concatenate([inv16, inv8])[None, :], (128, 1)).astype(f32)

    def pc(v, n):
        return np.ascontiguousarray(np.asarray(v, f32).reshape(n, 128).T)

    def bc(v):
        return np.ascontiguousarray(np.tile(np.asarray(v, f32).reshape(1, -1), (128, 1)))

    cw = np.asarray(inputs["conv_w"], f32)[0, :, 0, :]
    convw = np.ascontiguousarray(cw.reshape(4, 24, 128).transpose(2, 1, 0).reshape(128, 96))
    shared = {
        "consts": consts, "invf": invf,
        "w_in": np.asarray(inputs["w_in"], f32)[0],
        "g_mix": pc(inputs["norm_mix_g"][0], 8),
        "convw": convw, "convb": pc(inputs["conv_b"][0], 24),
        "hp3": np.concatenate([bc(inputs["dt_bias"][0]), bc(inputs["a_log"][0]), bc(inputs["d_skip"][0])], axis=1),
        "g_ssd": pc(inputs["ssd_norm_g"][0], 16),
        "w_attn_branch": np.asarray(inputs["w_attn_branch"], f32)[0],
        "w_ssd_branch": np.asarray(inputs["w_ssd_branch"], f32)[0],
        "w_out": np.asarray(inputs["w_out"], f32)[0],
        "g_cross": pc(inputs["norm_cross_g"][0], 8), "g_mem": pc(inputs["norm_mem_g"][0], 8),
        "w_cross_q": np.asarray(inputs["w_cross_q"], f32)[0],
        "w_cross_kv": np.asarray(inputs["w_cross_kv"], f32)[0],
        "w_cross_out": np.asarray(inputs["w_cross_out"], f32)[0],
        "g_ffn": bc(inputs["norm_ffn_g"][0]), "g_ffn_c": pc(inputs["norm_ffn_g"][0], 8),
        "w_peer_q": np.asarray(inputs["w_peer_q"], f32)[0],
        "sub_keys": np.ascontiguousarray(np.asarray(inputs["peer_sub_keys"], f32)[0].reshape(16, 128, 128)),
        "peer_u": np.asarray(inputs["peer_u"], f32)[0],
        "peer_v": np.asarray(inputs["peer_v"], f32)[0],
        "g_final": bc(inputs["norm_final_g"]),
    }
    maps = []
    for c in range(NCORE):
        b, j = c // 4, c % 4
        xs = np.zeros((NSEG, SEGT, D), f32)
        ps = np.zeros((NSEG, SEGT), f32)
        vs = np.zeros((NSEG, SEGT), f32)
        pen = np.full((6,), NEG, f32)
        for s in range(NSEG):
            t0 = 2048 * j - NCTX + s * SEGT
            if t0 >= 0:
                xs[s] = x[b, t0:t0 + SEGT]
                ps[s] = pos[b, t0:t0 + SEGT].astype(f32)
                vs[s] = 1.0
                if s < 6:
                    pen[s] = 0.0
        m = dict(shared)
        m["x_seg"] = xs
        m["pos_seg"] = np.ascontiguousarray(ps.reshape(NSEG * 8, 128).T)
        m["valid_seg"] = np.ascontiguousarray(vs.reshape(NSEG * 8, 128).T)
        m["pen_ctx"] = np.ascontiguousarray(np.tile(pen[None, :], (128, 1)))
        m["mem"] = np.asarray(inputs["mem"], f32)[b]
        maps.append(m)
    return maps


def kernel(**inputs):
    nc = build()
    maps = _prep_inputs(inputs)
    res = run_bass_kernel_spmd(nc, maps, core_ids=list(range(NCORE)))
    out = np.zeros((2, 8192, D), np.float32)
    for c in range(NCORE):
        b, j = c // 4, c % 4
        out[b, 2048 * j:2048 * (j + 1)] = res.results[c]["y_out"]
    return out
```

```python
import math
import numpy as np
import ml_dtypes
from contextlib import ExitStack
import concourse.bass as bass
import concourse.mybir as mybir
from concourse.bass_utils import run_bass_kernel_spmd

F32 = mybir.dt.float32
BF16 = mybir.dt.bfloat16
I32 = mybir.dt.int32
U32 = mybir.dt.uint32
ALU = mybir.AluOpType
ACT = mybir.ActivationFunctionType
AX = mybir.AxisListType

D = 1024
NCORE = 8
TOWN = 2048
SEGT = 1024
NSEG = 8
NCTX = 6144
EPS = 1e-6
NEG = -1.0e30
OQ, OK_, OV, OIQ, OIK, OIW, OZ, OXBC, ODT, OGATE = 0, 1024, 1280, 1536, 1792, 1856, 1860, 3908, 6980, 7012
TWO_PI = 2.0 * math.pi
import os
KSKIP = set(os.environ.get('KSKIP', '').split(','))


class Res:
    __slots__ = ("name", "w", "r")

    def __init__(self, name=""):
        self.name = name
        self.w = None
        self.r = []


class FW:
    NDMA = 24

    def __init__(self, nc, es):
        self.nc = nc
        self.eng = {"pe": nc.tensor, "dve": nc.vector, "act": nc.scalar, "pool": nc.gpsimd, "sp": nc.sync}
        self.sem = {k: es.enter_context(nc.semaphore("s_" + k)) for k in self.eng}
        self.cnt = {k: 0 for k in self.eng}
        self.dsem = [es.enter_context(nc.semaphore(f"d{i}")) for i in range(self.NDMA)]
        self.dcnt = [0] * self.NDMA
        self.dnext = 0
        self.seen = {k: {} for k in self.eng}

    def _wait(self, e, tok):
        sem, val = tok
        if self.seen[e].get(sem.name, 0) >= val:
            return
        self.eng[e].wait_ge(sem, val)
        self.seen[e][sem.name] = val

    def _deps(self, e, reads, writes, skip_self=False):
        toks = []
        for r in reads:
            if r.w is not None:
                toks.append(r.w)
        for w in writes:
            if w.w is not None:
                toks.append(w.w)
            toks.extend(w.r)
        best = {}
        for sem, val in toks:
            if skip_self and sem is self.sem[e]:
                continue
            if sem.name not in best or best[sem.name][1] < val:
                best[sem.name] = (sem, val)
        for tok in best.values():
            self._wait(e, tok)

    def _commit(self, tok, reads, writes):
        for r in reads:
            r.r.append(tok)
            if len(r.r) > 32:
                best = {}
                for s, v in r.r:
                    if s.name not in best or best[s.name][1] < v:
                        best[s.name] = (s, v)
                r.r = list(best.values())
        for w in writes:
            w.w = tok
            w.r = []

    def op(self, e, fn, reads=(), writes=(), skip_self=False):
        self._deps(e, reads, writes, skip_self)
        ins = fn(self.eng[e])
        self.cnt[e] += 1
        ins.then_inc(self.sem[e], 1)
        tok = (self.sem[e], self.cnt[e])
        self._commit(tok, reads, writes)
        return tok

    def dma(self, e, out, in_, reads=(), writes=(), indirect=None, **kw):
        i = self.dnext
        self.dnext = (self.dnext + 1) % self.NDMA
        sem = self.dsem[i]
        if self.dcnt[i] > 0:
            self._wait(e, (sem, self.dcnt[i]))
        self._deps(e, reads, writes)
        if indirect is not None:
            ins = self.eng[e].indirect_dma_start(out=out, out_offset=None, in_=in_, in_offset=indirect)
        else:
            ins = self.eng[e].dma_start(out=out, in_=in_, **kw)
        self.dcnt[i] += 16
        ins.then_inc(sem, 16)
        tok = (sem, self.dcnt[i])
        self._commit(tok, reads, writes)
        return tok

    def barrier(self):
        toks = [(self.sem[k], self.cnt[k]) for k in self.eng if self.cnt[k] > 0]
        toks += [(self.dsem[i], self.dcnt[i]) for i in range(self.NDMA) if self.dcnt[i] > 0]
        for e in self.eng:
            for t in toks:
                if t[0] is self.sem[e]:
                    continue
                self._wait(e, t)


class Pool:
    N = [0]

    def __init__(self, nc, es):
        self.nc, self.es = nc, es

    def sb(self, shape, dt, name=None):
        Pool.N[0] += 1
        t = self.es.enter_context(self.nc.sbuf_tensor(f"{name or 't'}_{Pool.N[0]}", list(shape), dt))
        return t, Res(name or "t")


def build(dbg=False, phases=(1, 2, 3)):
    nc = bass.Bass("TRN2", target_bir_lowering=False)

    def din(name, shape, dt=F32):
        return nc.dram_tensor(name, list(shape), dt, kind="ExternalInput").ap()

    x_seg = din("x_seg", [NSEG, SEGT, D])
    pos_seg = din("pos_seg", [128, NSEG * 8])
    valid_seg = din("valid_seg", [128, NSEG * 8])
    pen_ctx = din("pen_ctx", [128, 6])
    consts = din("consts", [128, 5 * 128])
    invf = din("invf", [128, 24])
    mem = din("mem", [256, D])
    w_in = din("w_in", [D, 9060])
    g_mix = din("g_mix", [128, 8])
    convw = din("convw", [128, 24 * 4])
    convb = din("convb", [128, 24])
    hp3 = din("hp3", [128, 96])
    g_ssd = din("g_ssd", [128, 16])
    w_attn_branch = din("w_attn_branch", [D, D])
    w_ssd_branch = din("w_ssd_branch", [2048, D])
    w_out = din("w_out", [D, D])
    g_cross = din("g_cross", [128, 8])
    g_mem = din("g_mem", [128, 8])
    w_cross_q = din("w_cross_q", [D, D])
    w_cross_kv = din("w_cross_kv", [D, 2048])
    w_cross_out = din("w_cross_out", [D, D])
    g_ffn = din("g_ffn", [128, D])
    g_ffn_c = din("g_ffn_c", [128, 8])
    w_peer_q = din("w_peer_q", [D, 2048])
    sub_keys = din("sub_keys", [16, 128, 128])
    peer_u = din("peer_u", [16384, D])
    peer_v = din("peer_v", [16384, D])
    g_final = din("g_final", [128, D])
    y_out = nc.dram_tensor("y_out", [TOWN, D], F32, kind="ExternalOutput").ap()
    sproj_d = nc.dram_tensor("sproj_d", [TOWN, D], F32, kind="Internal" if 1 in phases else "ExternalInput").ap()
    attn_d = nc.dram_tensor("attn_d", [TOWN, D], F32, kind="Internal" if 2 in phases else "ExternalInput").ap()
    if dbg:
        dbg_sproj = nc.dram_tensor("dbg_sproj", [TOWN, D], F32, kind="ExternalOutput").ap()
        dbg_attn = nc.dram_tensor("dbg_attn", [TOWN, D], F32, kind="ExternalOutput").ap()

    with ExitStack() as es0:
        fw = FW(nc, es0)
        P0 = Pool(nc, es0)
        banks = []
        for i in range(8):
            t = es0.enter_context(nc.psum_tensor(f"bank{i}", [128, 512], F32))
            banks.append((t, Res(f"bank{i}")))
        cst, r_cst = P0.sb([128, 5 * 128], F32, "cst")
        cstb, r_cstb = P0.sb([128, 5 * 128], BF16, "cstb")
        fw.dma("sp", cst[:], consts, writes=[r_cst])
        fw.op("dve", lambda e: e.tensor_copy(out=cstb[:], in_=cst[:]), reads=[r_cst], writes=[r_cstb])
        ident_b = cstb[:, 0:128]
        tri_f = cst[:, 128:256]
        ones_f = cst[:, 256:384]
        tri_pen = cst[:, 384:512]
        ones_b = cstb[:, 256:384]
        gm, r_gm = P0.sb([128, 8], F32, "gm")
        fw.dma("sp", gm[:], g_mix, writes=[r_gm])
        posv, r_posv = P0.sb([128, NSEG * 8], F32, "posv")
        fw.dma("sp", posv[:], pos_seg, writes=[r_posv])
        validv, r_validv = P0.sb([128, NSEG * 8], F32, "validv")
        fw.dma("sp", validv[:], valid_seg, writes=[r_validv])
        invt, r_invt = P0.sb([128, 24], F32, "invt")
        fw.dma("sp", invt[:], invf, writes=[r_invt])

        def w_rows(ap2d):
            return ap2d.rearrange("(c p) n -> p c n", p=128)

        w_in_r = w_rows(w_in)

        def load_weight(pool, dst, r_dst, src_r, c0, ncols, gt, r_gt, nch=8, eng="pool", stg_cols=512):
            stgf, r_stg = pool
            stg_cols = 4096 // nch
            stg = stgf[:, 0:nch * stg_cols].rearrange("p (c n) -> p c n", c=nch)
            done = 0
            while done < ncols:
                n = min(stg_cols, ncols - done)
                fw.dma("sp", stg[:, 0:nch, 0:n], src_r[:, 0:nch, c0 + done:c0 + done + n], writes=[r_stg])
                if gt is None:
                    fw.op(eng, lambda e, n=n, done=done: e.tensor_copy(out=dst[:, 0:nch, done:done + n], in_=stg[:, 0:nch, 0:n]),
                          reads=[r_stg], writes=[r_dst])
                else:
                    fw.op(eng, lambda e, n=n, done=done: e.tensor_tensor(
                        out=dst[:, 0:nch, done:done + n], in0=stg[:, 0:nch, 0:n],
                        in1=gt[:, 0:nch].unsqueeze(2).to_broadcast([128, nch, n]), op=ALU.mult),
                        reads=[r_stg, r_gt], writes=[r_dst])
                done += n

        def build_xT(pool, seg, xT, r_xT, xt_bufs, ntile=8, src=None, gdiv=D):
            for ti in range(ntile):
                xt, r_xt, xb, r_xb, ss, r_ss = xt_bufs[ti % 2]
                srcap = x_seg[seg, ti * 128:(ti + 1) * 128, :] if src is None else src(ti)
                fw.dma("sp", xt[:], srcap, writes=[r_xt])
                fw.op("act", lambda e: e.activation(out=xb[:], in_=xt[:], func=ACT.Square, accum_out=ss[:, 0:1]),
                      reads=[r_xt], writes=[r_xb, r_ss])
                fw.op("dve", lambda e: e.tensor_scalar(out=ss[:, 1:2], in0=ss[:, 0:1], scalar1=1.0 / gdiv, scalar2=EPS,
                                                       op0=ALU.mult, op1=ALU.add), reads=[r_ss], writes=[r_ss])
                fw.op("act", lambda e: e.activation(out=ss[:, 3:4], in_=ss[:, 1:2], func=ACT.Ln), reads=[r_ss], writes=[r_ss])
                fw.op("act", lambda e: e.activation(out=ss[:, 2:3], in_=ss[:, 3:4], func=ACT.Exp, scale=-0.5), reads=[r_ss], writes=[r_ss])
                fw.op("dve", lambda e: e.tensor_scalar(out=xb[:], in0=xt[:], scalar1=ss[:, 2:3], scalar2=None, op0=ALU.mult),
                      reads=[r_xt, r_ss], writes=[r_xb])
                bk, r_bk = banks[ti % 2]
                bkb = bk[:].bitcast(BF16)
                for ch in range(8):
                    fw.op("pe", lambda e, ch=ch: e.transpose(out=bkb[:, ch * 128:(ch + 1) * 128], in_=xb[:, ch * 128:(ch + 1) * 128],
                                                            identity=ident_b),
                          reads=[r_xb, r_cstb], writes=[r_bk], skip_self=True)
                fw.op("act", lambda e, ti=ti: e.activation(out=xT[:, :, ti * 128:(ti + 1) * 128],
                                                          in_=bkb.rearrange("p (c t) -> p c t", c=8), func=ACT.Copy),
                      reads=[r_bk], writes=[r_xT])

        if 1 in phases:
            with ExitStack() as es1:
                P = Pool(nc, es1)
                xT, r_xT = P.sb([128, 8, SEGT], BF16, "xT")
                xt_bufs = []
                for i in range(2):
                    xt, r_xt = P.sb([128, D], F32, "xt")
                    xb, r_xb = P.sb([128, D], BF16, "xb")
                    ss, r_ss = P.sb([128, 4], F32, "ss")
                    xt_bufs.append((xt, r_xt, xb, r_xb, ss, r_ss))
                wstg = P.sb([128, 4096], F32, "wstg")
                cw, r_cw = P.sb([128, 24, 4], F32, "cw")
                cb, r_cb = P.sb([128, 24], F32, "cb")
                fw.dma("sp", cw[:], convw.rearrange("p (r k) -> p r k", k=4), writes=[r_cw])
                fw.dma("sp", cb[:], convb, writes=[r_cb])
                hp, r_hp = P.sb([128, 96], F32, "hp")
                fw.dma("sp", hp[:], hp3, writes=[r_hp])
                gs, r_gs = P.sb([128, 16], F32, "gs")
                fw.dma("sp", gs[:], g_ssd, writes=[r_gs])
                a_bc, r_abc = P.sb([128, 32], F32, "a_bc")
                fw.op("act", lambda e: e.activation(out=a_bc[:], in_=hp[:, 32:64], func=ACT.Exp), reads=[r_hp], writes=[r_abc])
                fw.op("dve", lambda e: e.tensor_scalar(out=a_bc[:], in0=a_bc[:], scalar1=-1.0, scalar2=None, op0=ALU.mult),
                      reads=[r_abc], writes=[r_abc])
                wdt, r_wdt = P.sb([128, 8, 32], BF16, "wdt")
                load_weight(wstg, wdt, r_wdt, w_in_r, ODT, 32, gm, r_gm)
                halo, r_halo = P.sb([128, 24, 3], F32, "halo")
                fw.op("pool", lambda e: e.memset(halo[:], 0.0), writes=[r_halo])
                state, r_state = P.sb([128, 4, 512], F32, "state")
                fw.op("pool", lambda e: e.memset(state[:], 0.0), writes=[r_state])
                state_bf, r_statebf = P.sb([128, 512], BF16, "state_bf")
                NT = SEGT // 128
                dtr, r_dtr = P.sb([128, NT, 32], F32, "dtr")
                tmpa, r_tmpa = P.sb([128, NT, 32], F32, "tmpa")
                tmpb, r_tmpb = P.sb([128, NT, 32], F32, "tmpb")
                dt_all, r_dt = P.sb([128, NT, 32], F32, "dt_all")
                adt, r_adt = P.sb([128, NT, 32], F32, "adt")
                acs, r_acs = P.sb([128, NT, 32], F32, "acs")
                tot, r_tot = P.sb([128, NT, 32], F32, "tot")
                dtdte, r_dtdte = P.sb([128, NT, 32], F32, "dtdte")
                cdec, r_cdec = P.sb([128, NT, 32], F32, "cdec")
                eacs, r_eacs = P.sb([128, NT, 32], F32, "eacs")
                wrow, r_wrow = P.sb([128, 8, 128], BF16, "wrow")
                rowbufs = [P.sb([128, 3 + SEGT], F32, "rowbuf") for _ in range(2)]
                convacc = [P.sb([128, SEGT], F32, "convacc") for _ in range(2)]
                rowact = [P.sb([128, SEGT], BF16, "rowact") for _ in range(2)]
                xs_tok, r_xstok = P.sb([128, NT, 512], BF16, "xs_tok")
                B_tok, r_Btok = P.sb([128, NT, 128], BF16, "B_tok")
                BT, r_BT = P.sb([128, SEGT], BF16, "BT")
                CT, r_CT = P.sb([128, SEGT], BF16, "CT")
                xdt, r_xdt = P.sb([128, 512], BF16, "xdt")
                xdd, r_xdd = P.sb([128, 512], BF16, "xdd")
                y_store, r_ys = P.sb([128, NT, 2048], BF16, "y_store")
                cbm, r_cbm = P.sb([128, 128], F32, "cbm")
                triadt, r_triadt = P.sb([128, 8, 128], F32, "triadt")
                segb, r_segb = P.sb([128, 8, 128], F32, "segb")
                mT, r_mT = P.sb([128, 8, 128], BF16, "mT")
                t1, r_t1 = P.sb([128, 512], F32, "t1")
                t2, r_t2 = P.sb([128, 512], F32, "t2")
                wz, r_wz = P.sb([128, 8, 512], BF16, "wz")
                ws, r_ws = P.sb([128, 16, 1024], BF16, "ws")
                zact, r_zact = P.sb([128, 512], F32, "zact")
                ssq, r_ssq = P.sb([128, NT, 8], F32, "ssq")
                ygT, r_ygT = P.sb([128, 16, 128], BF16, "ygT")
                spt, r_spt = P.sb([128, D], F32, "spt")
                junk, r_junk = P.sb([128, 512], F32, "junk")

                for seg in range(NSEG):
                    own = seg >= 6
                    build_xT(P, seg, xT, r_xT, xt_bufs)
                    bk, r_bk = banks[2]
                    for ti in range(NT):
                        for ch in range(8):
                            fw.op("pe", lambda e, ti=ti, ch=ch: e.matmul(bk[:, ti * 32:(ti + 1) * 32], lhsT=xT[:, ch, ti * 128:(ti + 1) * 128],
                                                                          rhs=wdt[:, ch, :], start=(ch == 0), stop=(ch == 7)),
                                  reads=[r_xT, r_wdt], writes=[r_bk], skip_self=True)
                    bkv = bk[:, 0:NT * 32].rearrange("p (t h) -> p t h", h=32)
                    fw.op("dve", lambda e: e.tensor_tensor(out=dtr[:], in0=bkv, in1=hp[:, 0:32].unsqueeze(1).to_broadcast([128, NT, 32]), op=ALU.add),
                          reads=[r_bk, r_hp], writes=[r_dtr])
                    fw.op("dve", lambda e: e.tensor_scalar(out=tmpa[:], in0=dtr[:], scalar1=-1.0, scalar2=None, op0=ALU.mult), reads=[r_dtr], writes=[r_tmpa])
                    fw.op("dve", lambda e: e.tensor_tensor(out=tmpa[:], in0=tmpa[:], in1=dtr[:], op=ALU.max), reads=[r_dtr, r_tmpa], writes=[r_tmpa])
                    fw.op("act", lambda e: e.activation(out=tmpa[:], in_=tmpa[:], func=ACT.Exp, scale=-1.0), reads=[r_tmpa], writes=[r_tmpa])
                    fw.op("act", lambda e: e.activation(out=tmpa[:], in_=tmpa[:], func=ACT.Ln, bias=1.0), reads=[r_tmpa], writes=[r_tmpa])
                    fw.op("dve", lambda e: e.tensor_single_scalar(out=tmpb[:], in_=dtr[:], scalar=0.0, op=ALU.max), reads=[r_dtr], writes=[r_tmpb])
                    fw.op("dve", lambda e: e.tensor_tensor(out=tmpb[:], in0=tmpb[:], in1=tmpa[:], op=ALU.add), reads=[r_tmpa, r_tmpb], writes=[r_tmpb])
                    fw.op("dve", lambda e, seg=seg: e.tensor_tensor(out=dt_all[:], in0=tmpb[:],
                                                                  in1=validv[:, seg * NT:(seg + 1) * NT].unsqueeze(2).to_broadcast([128, NT, 32]), op=ALU.mult),
                          reads=[r_tmpb, r_validv], writes=[r_dt])
                    fw.op("dve", lambda e: e.tensor_tensor(out=adt[:], in0=dt_all[:], in1=a_bc[:].unsqueeze(1).to_broadcast([128, NT, 32]), op=ALU.mult),
                          reads=[r_dt, r_abc], writes=[r_adt])
                    bk3, r_bk3 = banks[3]
                    for ti in range(NT):
                        fw.op("pe", lambda e, ti=ti: e.matmul(bk[:, ti * 32:(ti + 1) * 32], lhsT=tri_f, rhs=adt[:, ti, :], start=True, stop=True),
                              reads=[r_cst, r_adt], writes=[r_bk], skip_self=True)
                        fw.op("pe", lambda e, ti=ti: e.matmul(bk3[:, ti * 32:(ti + 1) * 32], lhsT=ones_f, rhs=adt[:, ti, :], start=True, stop=True),
                              reads=[r_cst, r_adt], writes=[r_bk3], skip_self=True)
                    fw.op("act", lambda e: e.activation(out=acs[:], in_=bkv, func=ACT.Copy), reads=[r_bk], writes=[r_acs])
                    bk3v = bk3[:, 0:NT * 32].rearrange("p (t h) -> p t h", h=32)
                    fw.op("act", lambda e: e.activation(out=tot[:], in_=bk3v, func=ACT.Copy), reads=[r_bk3], writes=[r_tot])
                    fw.op("dve", lambda e: e.tensor_tensor(out=tmpa[:], in0=tot[:], in1=acs[:], op=ALU.subtract), reads=[r_tot, r_acs], writes=[r_tmpa])
                    fw.op("act", lambda e: e.activation(out=tmpa[:], in_=tmpa[:], func=ACT.Exp), reads=[r_tmpa], writes=[r_tmpa])
                    fw.op("dve", lambda e: e.tensor_tensor(out=dtdte[:], in0=tmpa[:], in1=dt_all[:], op=ALU.mult), reads=[r_tmpa, r_dt], writes=[r_dtdte])
                    fw.op("act", lambda e: e.activation(out=cdec[:], in_=tot[:], func=ACT.Exp), reads=[r_tot], writes=[r_cdec])
                    if own:
                        fw.op("act", lambda e: e.activation(out=eacs[:], in_=acs[:], func=ACT.Exp), reads=[r_acs], writes=[r_eacs])
                        fw.op("pool", lambda e: e.memset(ssq[:], 0.0), writes=[r_ssq])

                    for g in range(4):
                        rows = [(4 * g + i, "xs", i) for i in range(4)] + [(16 + g, "B", 0)] + ([(20 + g, "C", 0)] if seg >= 5 else [])
                        for ri, (r, kind, sub) in enumerate(rows):
                            rb, r_rb = rowbufs[ri % 2]
                            ca, r_ca = convacc[ri % 2]
                            ra, r_ra = rowact[ri % 2]
                            load_weight(wstg, wrow, r_wrow, w_in_r, OXBC + r * 128, 128, gm, r_gm)
                            for tg in range(SEGT // 512):
                                bkp, r_bkp = banks[4 + (tg % 2)]
                                for ch in range(8):
                                    fw.op("pe", lambda e, ch=ch, tg=tg, bkp=bkp: e.matmul(bkp[:], lhsT=wrow[:, ch, :], rhs=xT[:, ch, tg * 512:(tg + 1) * 512],
                                                                                          start=(ch == 0), stop=(ch == 7)),
                                          reads=[r_wrow, r_xT], writes=[r_bkp], skip_self=True)
                                fw.op("act", lambda e, tg=tg, bkp=bkp, rb=rb: e.activation(out=rb[:, 3 + tg * 512:3 + (tg + 1) * 512], in_=bkp[:], func=ACT.Copy),
                                      reads=[r_bkp], writes=[r_rb])
                            fw.op("pool", lambda e, rb=rb, r=r: e.tensor_copy(out=rb[:, 0:3], in_=halo[:, r, :]), reads=[r_halo], writes=[r_rb])
                            fw.op("pool", lambda e, rb=rb, r=r: e.tensor_copy(out=halo[:, r, :], in_=rb[:, SEGT:SEGT + 3]), reads=[r_rb], writes=[r_halo])
                            if kind == "C" and not own:
                                continue
                            fw.op("dve", lambda e, rb=rb, ca=ca, r=r: e.tensor_scalar(out=ca[:], in0=rb[:, 0:SEGT], scalar1=cw[:, r, 0:1], scalar2=None, op0=ALU.mult),
                                  reads=[r_rb, r_cw], writes=[r_ca])
                            for k in range(1, 4):
                                fw.op("dve", lambda e, rb=rb, ca=ca, r=r, k=k: e.scalar_tensor_tensor(out=ca[:], in0=rb[:, k:k + SEGT], scalar=cw[:, r, k:k + 1], in1=ca[:],
                                                                                                     op0=ALU.mult, op1=ALU.add),
                                      reads=[r_rb, r_cw, r_ca], writes=[r_ca])
                            fw.op("act", lambda e, ca=ca, ra=ra, r=r: e.activation(out=ra[:], in_=ca[:], func=ACT.Silu, bias=cb[:, r:r + 1]),
                                  reads=[r_ca, r_cb], writes=[r_ra])
                            if kind == "C":
                                fw.op("pool", lambda e, ra=ra: e.tensor_copy(out=CT[:], in_=ra[:]), reads=[r_ra], writes=[r_CT])
                                continue
                            if kind == "B" and own:
                                fw.op("pool", lambda e, ra=ra: e.tensor_copy(out=BT[:], in_=ra[:]), reads=[r_ra], writes=[r_BT])
                            bkt, r_bkt = banks[6 + (ri % 2)]
                            bktb = bkt[:].bitcast(BF16)
                            for ti in range(NT):
                                fw.op("pe", lambda e, ti=ti, ra=ra, bktb=bktb: e.transpose(out=bktb[:, ti * 128:(ti + 1) * 128], in_=ra[:, ti * 128:(ti + 1) * 128], identity=ident_b),
                                      reads=[r_ra, r_cstb], writes=[r_bkt], skip_self=True)
                            src = bktb[:, 0:NT * 128].rearrange("p (t c) -> p t c", c=128)
                            if kind == "xs":
                                fw.op("act", lambda e, src=src, sub=sub: e.activation(out=xs_tok[:, :, sub * 128:(sub + 1) * 128], in_=src, func=ACT.Copy),
                                      reads=[r_bkt], writes=[r_xstok])
                            else:
                                fw.op("act", lambda e, src=src: e.activation(out=B_tok[:], in_=src, func=ACT.Copy), reads=[r_bkt], writes=[r_Btok])
                        for c in range(NT):
                            hs = slice(8 * g, 8 * g + 8)
                            xsv = xs_tok[:, c, :].rearrange("p (h q) -> p h q", q=64)
                            fw.op("dve", lambda e, c=c, xsv=xsv, hs=hs: e.tensor_tensor(out=xdd[:].rearrange("p (h q) -> p h q", q=64), in0=xsv,
                                                                                        in1=dtdte[:, c, hs].unsqueeze(2).to_broadcast([128, 8, 64]), op=ALU.mult),
                                  reads=[r_xstok, r_dtdte], writes=[r_xdd])
                            bks, r_bks = banks[2]
                            fw.op("pe", lambda e, c=c, bks=bks: e.matmul(bks[:], lhsT=B_tok[:, c, :], rhs=xdd[:], start=True, stop=True),
                                  reads=[r_Btok, r_xdd], writes=[r_bks], skip_self=True)
                            if own:
                                fw.op("act", lambda e, g=g: e.activation(out=state_bf[:], in_=state[:, g, :], func=ACT.Copy), reads=[r_state], writes=[r_statebf])
                                fw.op("dve", lambda e, c=c, xsv=xsv, hs=hs: e.tensor_tensor(out=xdt[:].rearrange("p (h q) -> p h q", q=64), in0=xsv,
                                                                                            in1=dt_all[:, c, hs].unsqueeze(2).to_broadcast([128, 8, 64]), op=ALU.mult),
                                      reads=[r_xstok, r_dt], writes=[r_xdt])
                                cs = slice(c * 128, (c + 1) * 128)
                                bkc, r_bkc = banks[3]
                                fw.op("pe", lambda e, cs=cs, bkc=bkc: e.matmul(bkc[:, 0:128], lhsT=BT[:, cs], rhs=CT[:, cs], start=True, stop=True),
                                      reads=[r_BT, r_CT], writes=[r_bkc], skip_self=True)
                                fw.op("dve", lambda e, bkc=bkc: e.tensor_tensor(out=cbm[:], in0=bkc[:, 0:128], in1=tri_f, op=ALU.mult), reads=[r_bkc, r_cst], writes=[r_cbm])
                                fw.op("pool", lambda e, c=c, hs=hs: e.tensor_tensor(out=triadt[:], in0=tri_f.unsqueeze(1).to_broadcast([128, 8, 128]),
                                                                                   in1=adt[:, c, hs].unsqueeze(2).to_broadcast([128, 8, 128]), op=ALU.mult),
                                      reads=[r_cst, r_adt], writes=[r_triadt])
                                bka, r_bka = banks[4]
                                bkb_, r_bkb_ = banks[5]
                                fw.op("pe", lambda e, bka=bka: e.matmul(bka[:], lhsT=ones_f, rhs=triadt[:, 0:4, :].rearrange("p h l -> p (h l)"), start=True, stop=True),
                                      reads=[r_cst, r_triadt], writes=[r_bka], skip_self=True)
                                fw.op("pe", lambda e, bkb_=bkb_: e.matmul(bkb_[:], lhsT=ones_f, rhs=triadt[:, 4:8, :].rearrange("p h l -> p (h l)"), start=True, stop=True),
                                      reads=[r_cst, r_triadt], writes=[r_bkb_], skip_self=True)
                                for h in range(8):
                                    bsrc, r_bsrc = (bka, r_bka) if h < 4 else (bkb_, r_bkb_)
                                    hh = h % 4
                                    fw.op("dve", lambda e, h=h, hh=hh, bsrc=bsrc, c=c, g=g: e.scalar_tensor_tensor(
                                        out=segb[:, h, :], in0=bsrc[:, hh * 128:(hh + 1) * 128], scalar=acs[:, c, 8 * g + h:8 * g + h + 1], in1=tri_f,
                                        op0=ALU.subtract, op1=ALU.mult), reads=[r_bsrc, r_acs, r_cst], writes=[r_segb])
                                fw.op("act", lambda e: e.activation(out=segb[:], in_=segb[:], func=ACT.Exp), reads=[r_segb], writes=[r_segb])
                                fw.op("dve", lambda e: e.tensor_tensor(out=mT[:], in0=segb[:], in1=cbm[:].unsqueeze(1).to_broadcast([128, 8, 128]), op=ALU.mult),
                                      reads=[r_segb, r_cbm], writes=[r_mT])
                                bky, r_bky = banks[6]
                                for h in range(8):
                                    fw.op("pe", lambda e, h=h, bky=bky: e.matmul(bky[:, h * 64:(h + 1) * 64], lhsT=mT[:, h, :], rhs=xdt[:, h * 64:(h + 1) * 64], start=True, stop=True),
                                          reads=[r_mT, r_xdt], writes=[r_bky], skip_self=True)
                                bko, r_bko = banks[7]
                                fw.op("pe", lambda e, cs=cs, bko=bko: e.matmul(bko[:], lhsT=CT[:, cs], rhs=state_bf[:], start=True, stop=True),
                                      reads=[r_CT, r_statebf], writes=[r_bko], skip_self=True)
                                fw.op("dve", lambda e, c=c, hs=hs, bko=bko: e.tensor_tensor(out=t1[:].rearrange("p (h q) -> p h q", q=64),
                                                                                          in0=bko[:].rearrange("p (h q) -> p h q", q=64),
                                                                                          in1=eacs[:, c, hs].unsqueeze(2).to_broadcast([128, 8, 64]), op=ALU.mult),
                                      reads=[r_bko, r_eacs], writes=[r_t1])
                                fw.op("pool", lambda e, xsv=xsv, hs=hs: e.tensor_tensor(out=t2[:].rearrange("p (h q) -> p h q", q=64), in0=xsv,
                                                                                       in1=hp[:, 64 + hs.start:64 + hs.stop].unsqueeze(2).to_broadcast([128, 8, 64]), op=ALU.mult),
                                      reads=[r_xstok, r_hp], writes=[r_t2])
                                fw.op("dve", lambda e: e.tensor_tensor(out=t1[:], in0=t1[:], in1=t2[:], op=ALU.add), reads=[r_t1, r_t2], writes=[r_t1])
                                fw.op("dve", lambda e, c=c, g=g, bky=bky: e.tensor_tensor(out=y_store[:, c, g * 512:(g + 1) * 512], in0=bky[:], in1=t1[:], op=ALU.add),
                                      reads=[r_bky, r_t1], writes=[r_ys])
                            stv = state[:, g, :].rearrange("p (h q) -> p h q", q=64)
                            fw.op("dve", lambda e, c=c, hs=hs, stv=stv: e.tensor_tensor(out=stv, in0=stv, in1=cdec[:, c, hs].unsqueeze(2).to_broadcast([128, 8, 64]), op=ALU.mult),
                                  reads=[r_state, r_cdec], writes=[r_state])
                            fw.op("dve", lambda e, g=g, bks=bks: e.tensor_tensor(out=state[:, g, :], in0=state[:, g, :], in1=bks[:], op=ALU.add),
                                  reads=[r_state, r_bks], writes=[r_state])
                        if own:
                            load_weight(wstg, wz, r_wz, w_in_r, OZ + g * 512, 512, gm, r_gm)
                            for c in range(NT):
                                bkz, r_bkz = banks[2 + (c % 2)]
                                for ch in range(8):
                                    fw.op("pe", lambda e, c=c, ch=ch, bkz=bkz: e.matmul(bkz[:], lhsT=xT[:, ch, c * 128:(c + 1) * 128], rhs=wz[:, ch, :], start=(ch == 0), stop=(ch == 7)),
                                          reads=[r_xT, r_wz], writes=[r_bkz], skip_self=True)
                                fw.op("act", lambda e, bkz=bkz: e.activation(out=zact[:], in_=bkz[:], func=ACT.Silu), reads=[r_bkz], writes=[r_zact])
                                ysl = y_store[:, c, g * 512:(g + 1) * 512]
                                fw.op("dve", lambda e, ysl=ysl: e.tensor_tensor(out=zact[:], in0=zact[:], in1=ysl, op=ALU.mult), reads=[r_zact, r_ys], writes=[r_zact])
                                fw.op("act", lambda e, c=c, g=g: e.activation(out=junk[:], in_=zact[:], func=ACT.Square, accum_out=ssq[:, c, g:g + 1]),
                                      reads=[r_zact], writes=[r_junk, r_ssq])
                                fw.op("pool", lambda e, ysl=ysl: e.tensor_copy(out=ysl, in_=zact[:]), reads=[r_zact], writes=[r_ys])
                    if own:
                        load_weight(wstg, ws, r_ws, w_rows(w_ssd_branch), 0, 1024, gs, r_gs, nch=16, stg_cols=512)
                        for c in range(NT):
                            fw.op("dve", lambda e, c=c: e.tensor_reduce(out=ssq[:, c, 4:5], in_=ssq[:, c, 0:4], axis=AX.X, op=ALU.add), reads=[r_ssq], writes=[r_ssq])
                            fw.op("dve", lambda e, c=c: e.tensor_scalar(out=ssq[:, c, 5:6], in0=ssq[:, c, 4:5], scalar1=1.0 / 2048, scalar2=EPS, op0=ALU.mult, op1=ALU.add),
                                  reads=[r_ssq], writes=[r_ssq])
                            fw.op("act", lambda e, c=c: e.activation(out=ssq[:, c, 7:8], in_=ssq[:, c, 5:6], func=ACT.Ln), reads=[r_ssq], writes=[r_ssq])
                            fw.op("act", lambda e, c=c: e.activation(out=ssq[:, c, 6:7], in_=ssq[:, c, 7:8], func=ACT.Exp, scale=-0.5), reads=[r_ssq], writes=[r_ssq])
                            for half in range(2):
                                bkt, r_bkt = banks[4 + half]
                                bktb = bkt[:].bitcast(BF16)
                                for i in range(8):
                                    ch = half * 8 + i
                                    fw.op("pe", lambda e, c=c, ch=ch, i=i, bktb=bktb: e.transpose(out=bktb[:, i * 128:(i + 1) * 128], in_=y_store[:, c, ch * 128:(ch + 1) * 128], identity=ident_b),
                                          reads=[r_ys, r_cstb], writes=[r_bkt], skip_self=True)
                                fw.op("act", lambda e, half=half, bktb=bktb: e.activation(out=ygT[:, half * 8:(half + 1) * 8, :], in_=bktb.rearrange("p (c t) -> p c t", c=8), func=ACT.Copy),
                                      reads=[r_bkt], writes=[r_ygT])
                            for cg in range(2):
                                bko, r_bko = banks[6 + cg]
                                for ch in range(16):
                                    fw.op("pe", lambda e, ch=ch, cg=cg, bko=bko: e.matmul(bko[:], lhsT=ygT[:, ch, :], rhs=ws[:, ch, cg * 512:(cg + 1) * 512], start=(ch == 0), stop=(ch == 15)),
                                          reads=[r_ygT, r_ws], writes=[r_bko], skip_self=True)
                                fw.op("dve", lambda e, c=c, cg=cg, bko=bko: e.tensor_scalar(out=spt[:, cg * 512:(cg + 1) * 512], in0=bko[:], scalar1=ssq[:, c, 6:7], scalar2=None, op0=ALU.mult),
                                      reads=[r_bko, r_ssq], writes=[r_spt])
                            t0 = (seg - 6) * SEGT + c * 128
                            fw.dma("sp", sproj_d[t0:t0 + 128, :], spt[:], reads=[r_spt])
                            if dbg:
                                fw.dma("sp", dbg_sproj[t0:t0 + 128, :], spt[:], reads=[r_spt])
                fw.barrier()

        if 2 in phases:
            with ExitStack() as es2:
                P = Pool(nc, es2)
                NT = SEGT // 128
                KT, r_KT = P.sb([128, 2, 8192], BF16, "KT")
                Vx, r_Vx = P.sb([128, 64, 2, 129], BF16, "Vx")
                ikT, r_ikT = P.sb([128, 4096], BF16, "ikT")
                fw.op("pool", lambda e: e.memset(Vx[:], 1.0), writes=[r_Vx])
                wq, r_wq = P.sb([128, 8, 1024], BF16, "wq")
                wiq, r_wiq = P.sb([128, 8, 260], BF16, "wiq")
                penx, r_penx = P.sb([128, 8], F32, "penx")
                fw.op("pool", lambda e: e.memset(penx[:], 0.0), writes=[r_penx])
                fw.dma("sp", penx[:, 0:6], pen_ctx, writes=[r_penx])
                trig, r_trig = P.sb([128, 4, 24], F32, "trig")
                trigi, r_trigi = P.sb([128, 24], I32, "trigi")
                trigk, r_trigk = P.sb([128, 24], F32, "trigk")
                rt = [P.sb([128, 8, 16], F32, "rt") for _ in range(4)]
                kb, r_kb = P.sb([128, 1024], BF16, "kb")
                xt_bufs = []
                for i in range(1):
                    xt, r_xt = P.sb([128, D], F32, "xt")
                    xb, r_xb = P.sb([128, D], BF16, "xb")
                    ss, r_ss = P.sb([128, 4], F32, "ss")
                    xt_bufs.append((xt, r_xt, xb, r_xb, ss, r_ss))
                xt_bufs = xt_bufs * 2
                es2a = ExitStack()
                Pa = Pool(nc, es2a)
                xT, r_xT = Pa.sb([128, 8, SEGT], BF16, "xT2")
                wstg = Pa.sb([128, 4096], F32, "wstg")
                wkv, r_wkv = Pa.sb([128, 8, 512], BF16, "wkv")
                wik, r_wik = Pa.sb([128, 8, 64], BF16, "wik")
                kf, r_kf = Pa.sb([128, 1024], F32, "kf")
                load_weight(wstg, wkv, r_wkv, w_in_r, OK_, 512, gm, r_gm)
                load_weight(wstg, wik, r_wik, w_in_r, OIK, 64, gm, r_gm)
                load_weight(wstg, wq, r_wq, w_in_r, OQ, 1024, gm, r_gm)
                load_weight(wstg, wiq, r_wiq, w_in_r, OIQ, 256, gm, r_gm)
                stgf, r_stg = wstg
                stg4 = stgf[:, 0:32].rearrange("p (c n) -> p c n", c=8)
                fw.dma("sp", stg4, w_in_r[:, :, OIW:OIW + 4], writes=[r_stg])
                fw.op("pool", lambda e: e.tensor_tensor(out=wiq[:, :, 256:260], in0=stg4, in1=gm[:].unsqueeze(2).to_broadcast([128, 8, 4]), op=ALU.mult),
                      reads=[r_stg, r_gm], writes=[r_wiq])

                def trig_tables(col):
                    fw.op("dve", lambda e: e.tensor_scalar(out=trig[:, 0, :], in0=invt[:], scalar1=posv[:, col:col + 1], scalar2=None, op0=ALU.mult),
                          reads=[r_invt, r_posv], writes=[r_trig])
                    for which, shift in ((2, 0.0), (3, 0.25)):
                        fw.op("dve", lambda e, shift=shift: e.tensor_scalar(out=trig[:, 1, :], in0=trig[:, 0, :], scalar1=1.0 / TWO_PI, scalar2=shift, op0=ALU.mult, op1=ALU.add),
                              reads=[r_trig], writes=[r_trig])
                        fw.op("dve", lambda e: e.tensor_copy(out=trigi[:], in_=trig[:, 1, :]), reads=[r_trig], writes=[r_trigi])
                        fw.op("dve", lambda e: e.tensor_copy(out=trigk[:], in_=trigi[:]), reads=[r_trigi], writes=[r_trigk])
                        fw.op("dve", lambda e: e.scalar_tensor_tensor(out=trig[:, 1, :], in0=trigk[:], scalar=-TWO_PI, in1=trig[:, 0, :], op0=ALU.mult, op1=ALU.add),
                              reads=[r_trigk, r_trig], writes=[r_trig])
                        fw.op("dve", lambda e, shift=shift: e.tensor_scalar(out=trig[:, 1, :], in0=trig[:, 1, :], scalar1=shift * TWO_PI, scalar2=-math.pi, op0=ALU.add, op1=ALU.max),
                              reads=[r_trig], writes=[r_trig])
                        fw.op("dve", lambda e: e.tensor_scalar(out=trig[:, 1, :], in0=trig[:, 1, :], scalar1=math.pi, scalar2=None, op0=ALU.min),
                              reads=[r_trig], writes=[r_trig])
                        fw.op("act", lambda e, which=which: e.activation(out=trig[:, which, :], in_=trig[:, 1, :], func=ACT.Sin), reads=[r_trig], writes=[r_trig])

                def rope(buf, r_buf, nh, dh, half, toff):
                    v = buf[:, 0:nh * dh].rearrange("p (h d) -> p h d", d=dh)
                    x1, x2 = v[:, :, 0:half], v[:, :, half:2 * half]
                    sn = trig[:, 2, toff:toff + half].unsqueeze(1).to_broadcast([128, nh, half])
                    cs = trig[:, 3, toff:toff + half].unsqueeze(1).to_broadcast([128, nh, half])
                    tm = [(t[0][:, 0:nh, 0:half], t[1]) for t in rt]
                    fw.op("dve", lambda e: e.tensor_tensor(out=tm[0][0], in0=x1, in1=cs, op=ALU.mult), reads=[r_buf, r_trig], writes=[tm[0][1]])
                    fw.op("dve", lambda e: e.tensor_tensor(out=tm[1][0], in0=x2, in1=sn, op=ALU.mult), reads=[r_buf, r_trig], writes=[tm[1][1]])
                    fw.op("dve", lambda e: e.tensor_tensor(out=tm[2][0], in0=x2, in1=cs, op=ALU.mult), reads=[r_buf, r_trig], writes=[tm[2][1]])
                    fw.op("dve", lambda e: e.tensor_tensor(out=tm[3][0], in0=x1, in1=sn, op=ALU.mult), reads=[r_buf, r_trig], writes=[tm[3][1]])
                    fw.op("dve", lambda e: e.tensor_tensor(out=x1, in0=tm[0][0], in1=tm[1][0], op=ALU.subtract), reads=[tm[0][1], tm[1][1]], writes=[r_buf])
                    fw.op("dve", lambda e: e.tensor_tensor(out=x2, in0=tm[2][0], in1=tm[3][0], op=ALU.add), reads=[tm[2][1], tm[3][1]], writes=[r_buf])

                for seg in range(NSEG if 'keys' not in KSKIP else 0):
                    build_xT(P, seg, xT, r_xT, xt_bufs)
                    for ti in range(NT):
                        blk = seg * NT + ti
                        trig_tables(blk)
                        bk, r_bk = banks[2 + (ti % 2)]
                        for ch in range(8):
                            fw.op("pe", lambda e, ch=ch, ti=ti, bk=bk: e.matmul(bk[:], lhsT=xT[:, ch, ti * 128:(ti + 1) * 128], rhs=wkv[:, ch, :], start=(ch == 0), stop=(ch == 7)),
                                  reads=[r_xT, r_wkv], writes=[r_bk], skip_self=True)
                        bki, r_bki = banks[4 + (ti % 2)]
                        for ch in range(8):
                            fw.op("pe", lambda e, ch=ch, ti=ti, bki=bki: e.matmul(bki[:, 0:64], lhsT=xT[:, ch, ti * 128:(ti + 1) * 128], rhs=wik[:, ch, :], start=(ch == 0), stop=(ch == 7)),
                                  reads=[r_xT, r_wik], writes=[r_bki], skip_self=True)
                        fw.op("act", lambda e, bk=bk, blk=blk: e.activation(out=Vx[:, blk, :, 0:128], in_=bk[:, 256:512].rearrange("p (k d) -> p k d", d=128), func=ACT.Copy),
                              reads=[r_bk], writes=[r_Vx])
                        fw.op("act", lambda e, bk=bk: e.activation(out=kf[:, 0:256], in_=bk[:, 0:256], func=ACT.Copy), reads=[r_bk], writes=[r_kf])
                        fw.op("act", lambda e, bki=bki: e.activation(out=kf[:, 256:320], in_=bki[:, 0:64], func=ACT.Copy), reads=[r_bki], writes=[r_kf])
                        rope(kf, r_kf, 2, 128, 16, 0)
                        rope(kf[:, 256:320], r_kf, 1, 64, 8, 16)
                        fw.op("dve", lambda e: e.tensor_copy(out=kf[:, 320:384], in_=kf[:, 256:320]), reads=[r_kf], writes=[r_kf])
                        fw.op("dve", lambda e: e.tensor_copy(out=kb[:, 0:384], in_=kf[:, 0:384]), reads=[r_kf], writes=[r_kb])
                        bkt, r_bkt = banks[6 + (ti % 2)]
                        bktb = bkt[:].bitcast(BF16)
                        for kv in range(2):
                            fw.op("pe", lambda e, kv=kv, bktb=bktb: e.transpose(out=bktb[:, kv * 128:(kv + 1) * 128], in_=kb[:, kv * 128:(kv + 1) * 128], identity=ident_b),
                                  reads=[r_kb, r_cstb], writes=[r_bkt], skip_self=True)
                        fw.op("pe", lambda e, bktb=bktb: e.transpose(out=bktb[:, 256:384], in_=kb[:, 256:384], identity=ident_b),
                              reads=[r_kb, r_cstb], writes=[r_bkt], skip_self=True)
                        fw.op("act", lambda e, bktb=bktb, blk=blk: e.activation(out=KT[:, :, blk * 128:(blk + 1) * 128], in_=bktb[:, 0:256].rearrange("p (k t) -> p k t", k=2), func=ACT.Copy),
                              reads=[r_bkt], writes=[r_KT])
                        pr = slice(0, 64) if blk < 32 else slice(64, 128)
                        fw.op("act", lambda e, bktb=bktb, blk=blk, pr=pr: e.activation(out=ikT[pr, (blk % 32) * 128:(blk % 32 + 1) * 128], in_=bktb[pr, 256:384], func=ACT.Copy),
                              reads=[r_bkt], writes=[r_ikT])

                fw.barrier()
                es2a.close()
                xT, r_xT = P.sb([128, 8, 128], BF16, "xTq")
                S, r_S = P.sb([128, 8192], F32, "S")
                mk, r_mk = P.sb([128, 8192], BF16, "mk")
                maskT, r_maskT = P.sb([128, 64, 128], BF16, "maskT")
                QT, r_QT = P.sb([128, 8, 128], BF16, "QT")
                iqT, r_iqT = P.sb([128, 4, 128], BF16, "iqT")
                qf, r_qf = P.sb([128, 1024], F32, "qf")
                wsg, r_wsg = P.sb([128, 16], F32, "wsg")
                bis, r_bis = P.sb([128, 8], F32, "bis")
                bis2, r_bis2 = P.sb([128, 2], F32, "bis2")
                r_bis2b = Res("bis2b")
                mk2, r_mk2 = mk, Res("mk2")
                rl = [P.sb([128, 512], F32, "rl") for _ in range(2)]
                eL = [P.sb([128, 512], BF16, "eL") for _ in range(4)]
                PT = [P.sb([128, 512], BF16, "PT") for _ in range(4)]
                m1c, r_m1c = P.sb([128, 1], F32, "m1c")
                fw.op("pool", lambda e: e.memset(m1c[:], -1.0), writes=[r_m1c])
                rden, r_rden = P.sb([128, 16], F32, "rden")
                at, r_at = P.sb([128, D], F32, "at")
                NITER = 24 if 'bisect' not in KSKIP else 0
                for it in range(16):
                    seg = 6 + it // 8
                    ti = it % 8
                    build_xT(P, seg, xT, r_xT, xt_bufs, ntile=1, src=lambda _t, seg=seg, ti=ti: x_seg[seg, ti * 128:(ti + 1) * 128, :])
                    trig_tables(seg * 8 + ti)
                    for cg in range(2):
                        bk, r_bk = banks[2 + cg]
                        for ch in range(8):
                            fw.op("pe", lambda e, ch=ch, cg=cg, bk=bk: e.matmul(bk[:], lhsT=xT[:, ch, :], rhs=wq[:, ch, cg * 512:(cg + 1) * 512], start=(ch == 0), stop=(ch == 7)),
                                  reads=[r_xT, r_wq], writes=[r_bk], skip_self=True)
                        fw.op("act", lambda e, cg=cg, bk=bk: e.activation(out=qf[:, cg * 512:(cg + 1) * 512], in_=bk[:], func=ACT.Copy), reads=[r_bk], writes=[r_qf])
                    rope(qf, r_qf, 8, 128, 16, 0)
                    fw.op("dve", lambda e: e.tensor_scalar(out=kb[:], in0=qf[:], scalar1=128.0 ** -0.5, scalar2=None, op0=ALU.mult), reads=[r_qf], writes=[r_kb])
                    bkt, r_bkt = banks[4]
                    bktb = bkt[:].bitcast(BF16)
                    for h in range(8):
                        fw.op("pe", lambda e, h=h, bktb=bktb: e.transpose(out=bktb[:, h * 128:(h + 1) * 128], in_=kb[:, h * 128:(h + 1) * 128], identity=ident_b),
                              reads=[r_kb, r_cstb], writes=[r_bkt], skip_self=True)
                    fw.op("act", lambda e, bktb=bktb: e.activation(out=QT[:], in_=bktb.rearrange("p (h t) -> p h t", h=8), func=ACT.Copy), reads=[r_bkt], writes=[r_QT])
                    bk, r_bk = banks[5]
                    for ch in range(8):
                        fw.op("pe", lambda e, ch=ch, bk=bk: e.matmul(bk[:, 0:260], lhsT=xT[:, ch, :], rhs=wiq[:, ch, :], start=(ch == 0), stop=(ch == 7)),
                              reads=[r_xT, r_wiq], writes=[r_bk], skip_self=True)
                    fw.op("act", lambda e, bk=bk: e.activation(out=qf[:, 0:260], in_=bk[:, 0:260], func=ACT.Copy), reads=[r_bk], writes=[r_qf])
                    rope(qf, r_qf, 4, 64, 8, 16)
                    fw.op("dve", lambda e: e.tensor_scalar(out=wsg[:, 0:4], in0=qf[:, 256:260], scalar1=0.0625, scalar2=None, op0=ALU.mult), reads=[r_qf], writes=[r_wsg])
                    fw.op("dve", lambda e: e.tensor_scalar(out=wsg[:, 12:16], in0=wsg[:, 0:4], scalar1=-1.0, scalar2=None, op0=ALU.mult), reads=[r_wsg], writes=[r_wsg])
                    fw.op("dve", lambda e: e.tensor_tensor(out=wsg[:, 4:8], in0=wsg[:, 0:4], in1=wsg[:, 12:16], op=ALU.max), reads=[r_wsg], writes=[r_wsg])
                    fw.op("dve", lambda e: e.tensor_scalar(out=wsg[:, 8:12], in0=wsg[:, 0:4], scalar1=0.0, scalar2=2.0, op0=ALU.is_ge, op1=ALU.mult), reads=[r_wsg], writes=[r_wsg])
                    fw.op("dve", lambda e: e.tensor_scalar(out=wsg[:, 8:12], in0=wsg[:, 8:12], scalar1=-1.0, scalar2=None, op0=ALU.add), reads=[r_wsg], writes=[r_wsg])
                    fw.op("dve", lambda e: e.tensor_tensor(out=kb[:, 0:512].rearrange("p (h r d) -> p h r d", h=4, r=2),
                                                           in0=qf[:, 0:256].rearrange("p (h d) -> p h d", d=64).unsqueeze(2).to_broadcast([128, 4, 2, 64]),
                                                           in1=wsg[:, 4:8].unsqueeze(2).unsqueeze(3).to_broadcast([128, 4, 2, 64]), op=ALU.mult), reads=[r_qf, r_wsg], writes=[r_kb])
                    bkt, r_bkt = banks[6]
                    bktb = bkt[:].bitcast(BF16)
                    for h in range(4):
                        fw.op("pe", lambda e, h=h, bktb=bktb: e.transpose(out=bktb[:, h * 128:(h + 1) * 128], in_=kb[:, h * 128:(h + 1) * 128], identity=ident_b),
                              reads=[r_kb, r_cstb], writes=[r_bkt], skip_self=True)
                    fw.op("act", lambda e, bktb=bktb: e.activation(out=iqT[:], in_=bktb[:, 0:512].rearrange("p (h t) -> p h t", h=4), func=ACT.Copy), reads=[r_bkt], writes=[r_iqT])
                    Ls = NCTX + 128 * (it + 1)
                    ngr = (Ls + 511) // 512
                    for sg in range(ngr):
                        wd = min(512, Ls - sg * 512)
                        pr = slice(0, 64) if sg < 8 else slice(64, 128)
                        c0 = (sg % 8) * 512
                        for h in range(4):
                            bk, r_bk = banks[2 + ((sg * 4 + h) % 2)]
                            rlb, r_rlb = rl[(sg * 4 + h) % 2]
                            fw.op("pe", lambda e, h=h, wd=wd, bk=bk, pr=pr, c0=c0: e.matmul(bk[:, 0:wd], lhsT=iqT[pr, h, :], rhs=ikT[pr, c0:c0 + wd], start=True, stop=True),
                                  reads=[r_iqT, r_ikT], writes=[r_bk], skip_self=True)
                            fw.op("act", lambda e, wd=wd, bk=bk, rlb=rlb: e.activation(out=rlb[:, 0:wd], in_=bk[:, 0:wd], func=ACT.Relu), reads=[r_bk], writes=[r_rlb])
                            if h == 0:
                                fw.op("dve", lambda e, sg=sg, wd=wd, rlb=rlb: e.tensor_scalar(out=S[:, sg * 512:sg * 512 + wd], in0=rlb[:, 0:wd], scalar1=wsg[:, 8:9], scalar2=None, op0=ALU.mult),
                                      reads=[r_rlb, r_wsg], writes=[r_S])
                            else:
                                fw.op("dve", lambda e, sg=sg, wd=wd, rlb=rlb, h=h: e.scalar_tensor_tensor(out=S[:, sg * 512:sg * 512 + wd], in0=rlb[:, 0:wd], scalar=wsg[:, 8 + h:9 + h],
                                                                                                         in1=S[:, sg * 512:sg * 512 + wd], op0=ALU.mult, op1=ALU.add),
                                      reads=[r_rlb, r_wsg, r_S], writes=[r_S])
                    fw.op("dve", lambda e, Ls=Ls: e.tensor_reduce(out=bis[:, 0:1], in_=S[:, 0:Ls], axis=AX.X, op=ALU.max), reads=[r_S], writes=[r_bis])
                    fw.op("dve", lambda e, Ls=Ls: e.tensor_reduce(out=bis[:, 1:2], in_=S[:, 0:Ls], axis=AX.X, op=ALU.min), reads=[r_S], writes=[r_bis])
                    for cs in range(6):
                        fw.op("pool", lambda e, cs=cs: e.tensor_scalar(out=S[:, cs * 1024:(cs + 1) * 1024], in0=S[:, cs * 1024:(cs + 1) * 1024], scalar1=penx[:, cs:cs + 1], scalar2=None, op0=ALU.add),
                              reads=[r_S, r_penx], writes=[r_S])
                    fw.op("dve", lambda e, Ls=Ls: e.tensor_tensor(out=S[:, Ls - 128:Ls], in0=S[:, Ls - 128:Ls], in1=tri_pen, op=ALU.add), reads=[r_S, r_cst], writes=[r_S])
                    fw.op("dve", lambda e: e.tensor_tensor(out=bis[:, 3:4], in0=bis[:, 0:1], in1=bis[:, 1:2], op=ALU.subtract), reads=[r_bis], writes=[r_bis])
                    fw.op("dve", lambda e: e.tensor_scalar(out=bis[:, 3:4], in0=bis[:, 3:4], scalar1=0.501, scalar2=1e-6, op0=ALU.mult, op1=ALU.add), reads=[r_bis], writes=[r_bis])
                    fw.op("dve", lambda e: e.scalar_tensor_tensor(out=bis[:, 2:3], in0=bis[:, 3:4], scalar=-0.002, in1=bis[:, 1:2], op0=ALU.mult, op1=ALU.add), reads=[r_bis], writes=[r_bis])
                    fw.op("dve", lambda e: e.tensor_scalar(out=bis[:, 2:3], in0=bis[:, 2:3], scalar1=-1e-6, scalar2=None, op0=ALU.add), reads=[r_bis], writes=[r_bis])
                    fw.op("dve", lambda e: e.tensor_scalar(out=bis[:, 3:4], in0=bis[:, 3:4], scalar1=2.0, scalar2=None, op0=ALU.mult), reads=[r_bis], writes=[r_bis])
                    La = (int(Ls * 0.45) // 128) * 128
                    nA = Ls - La
                    for itn in range(NITER):
                        fw.op("dve", lambda e: e.tensor_scalar(out=bis[:, 3:4], in0=bis[:, 3:4], scalar1=0.5, scalar2=None, op0=ALU.mult), reads=[r_bis], writes=[r_bis])
                        fw.op("dve", lambda e: e.tensor_tensor(out=bis[:, 4:5], in0=bis[:, 2:3], in1=bis[:, 3:4], op=ALU.add), reads=[r_bis], writes=[r_bis])
                        fw.op("dve", lambda e: e.tensor_scalar(out=bis2[:, 0:1], in0=bis[:, 4:5], scalar1=-1.0, scalar2=None, op0=ALU.mult), reads=[r_bis], writes=[r_bis2])
                        fw.op("act", lambda e, La=La, Ls=Ls: e.activation(out=mk2[:, La:Ls], in_=S[:, La:Ls], func=ACT.Sign, bias=bis2[:, 0:1], accum_out=bis2[:, 1:2]),
                              reads=[r_S, r_bis2], writes=[r_mk2, r_bis2b])
                        fw.op("dve", lambda e, La=La: e.tensor_scalar(out=mk[:, 0:La], in0=S[:, 0:La], scalar1=bis[:, 4:5], scalar2=None, op0=ALU.is_ge, op1=ALU.add, accum_out=bis[:, 5:6]),
                              reads=[r_S, r_bis], writes=[r_mk, r_bis])
                        fw.op("dve", lambda e: e.scalar_tensor_tensor(out=bis[:, 5:6], in0=bis2[:, 1:2], scalar=0.5, in1=bis[:, 5:6], op0=ALU.mult, op1=ALU.add), reads=[r_bis, r_bis2b], writes=[r_bis])
                        fw.op("dve", lambda e, nA=nA: e.tensor_single_scalar(out=bis[:, 6:7], in_=bis[:, 5:6], scalar=255.5 - 0.5 * nA, op=ALU.is_ge), reads=[r_bis], writes=[r_bis])
                        fw.op("dve", lambda e: e.scalar_tensor_tensor(out=bis[:, 2:3], in0=bis[:, 6:7], scalar=bis[:, 3:4], in1=bis[:, 2:3], op0=ALU.mult, op1=ALU.add), reads=[r_bis], writes=[r_bis])
                    fw.op("dve", lambda e, Ls=Ls: e.tensor_scalar(out=mk[:, 0:Ls], in0=S[:, 0:Ls], scalar1=bis[:, 2:3], scalar2=None, op0=ALU.is_ge), reads=[r_S, r_bis], writes=[r_mk, r_mk2])
                    nb = Ls // 128
                    for b0 in range(0, nb, 8):
                        nn = min(8, nb - b0)
                        bkt, r_bkt = banks[4 + ((b0 // 8) % 2)]
                        bktb = bkt[:].bitcast(BF16)
                        for bb in range(nn):
                            fw.op("pe", lambda e, bb=bb, b0=b0, bktb=bktb: e.transpose(out=bktb[:, bb * 128:(bb + 1) * 128], in_=mk[:, (b0 + bb) * 128:(b0 + bb + 1) * 128], identity=ident_b),
                                  reads=[r_mk, r_mk2, r_cstb], writes=[r_bkt], skip_self=True)
                        fw.op("act", lambda e, b0=b0, nn=nn, bktb=bktb: e.activation(out=maskT[:, b0:b0 + nn, :], in_=bktb[:, 0:nn * 128].rearrange("p (b t) -> p b t", t=128), func=ACT.Copy, scale=30000.0, bias=-30000.0),
                              reads=[r_bkt], writes=[r_maskT])
                    nblk = nb if 'attn' not in KSKIP else 1
                    def oslot(h):
                        bko, r_bko = banks[h // 3]
                        return bko[:, (h % 3) * 129:(h % 3) * 129 + 129], r_bko
                    steps = [(sb, hq) for sb in range(nblk) for hq in range(2)]
                    NQB = 5
                    LA = 4

                    def emit_qk(k):
                        sb, hq = steps[k]
                        bkq, r_bkq = banks[3 + (k % NQB)]
                        fw.op("pe", lambda e, sb=sb, hq=hq, bkq=bkq: e.matmul(bkq[:], lhsT=KT[:, hq, sb * 128:(sb + 1) * 128], rhs=QT[:, hq * 4:(hq + 1) * 4, :].rearrange("p h t -> p (h t)"),
                                                                       start=True, stop=False),
                              reads=[r_KT, r_QT], writes=[r_bkq], skip_self=True)
                        fw.op("pe", lambda e, sb=sb, bkq=bkq: e.matmul(bkq[:].rearrange("p (h t) -> p h t", h=4), lhsT=ident_b, rhs=maskT[:, sb, :].unsqueeze(1).to_broadcast([128, 4, 128]),
                                                               start=False, stop=True),
                              reads=[r_maskT, r_cstb], writes=[r_bkq], skip_self=True)

                    for k in range(min(LA, len(steps))):
                        emit_qk(k)
                    for k, (sb, hq) in enumerate(steps):
                        bkq, r_bkq = banks[3 + (k % NQB)]
                        PTb, r_PTb = PT[k % 4]
                        fw.op("act", lambda e, bkq=bkq, PTb=PTb: e.activation(out=PTb[:], in_=bkq[:], func=ACT.Exp), reads=[r_bkq], writes=[r_PTb])
                        for j in range(4):
                            h = hq * 4 + j
                            oap, r_o = oslot(h)
                            fw.op("pe", lambda e, oap=oap, PTb=PTb, j=j, sb=sb, hq=hq: e.matmul(oap, lhsT=PTb[:, j * 128:(j + 1) * 128], rhs=Vx[:, sb, hq, :],
                                                                                            start=(sb == 0), stop=(sb == nblk - 1)),
                                  reads=[r_PTb, r_Vx], writes=[r_o], skip_self=True)
                        if k + LA < len(steps):
                            emit_qk(k + LA)
                    for h in range(8):
                        oap, r_o = oslot(h)
                        fw.op("dve", lambda e, oap=oap, h=h: e.reciprocal(out=rden[:, h:h + 1], in_=oap[:, 128:129]), reads=[r_o], writes=[r_rden])
                        fw.op("dve", lambda e, oap=oap, h=h: e.tensor_scalar(out=at[:, h * 128:(h + 1) * 128], in0=oap[:, 0:128], scalar1=rden[:, h:h + 1], scalar2=None, op0=ALU.mult),
                              reads=[r_o, r_rden], writes=[r_at])
                    t0 = it * 128
                    fw.dma("sp", attn_d[t0:t0 + 128, :], at[:], reads=[r_at])
                    if dbg:
                        fw.dma("sp", dbg_attn[t0:t0 + 128, :], at[:], reads=[r_at])
                fw.barrier()

        if 3 in phases:
            h2_d = nc.dram_tensor("h2_d", [TOWN, D], F32, kind="Internal").ap()
            with ExitStack() as es3:
                P = Pool(nc, es3)
                wstg = P.sb([128, 4096], F32, "wstg")
                wgate, r_wgate = P.sb([128, 8, 2048], BF16, "wgate")
                wa, r_wa = P.sb([128, 8, 1024], BF16, "wa")
                wo, r_wo = P.sb([128, 8, 1024], BF16, "wo")
                wcq, r_wcq = P.sb([128, 8, 1024], BF16, "wcq")
                wco, r_wco = P.sb([128, 8, 1024], BF16, "wco")
                gc_, r_gc = P.sb([128, 8], F32, "gc")
                gmm, r_gmm = P.sb([128, 8], F32, "gmm")
                fw.dma("sp", gc_[:], g_cross, writes=[r_gc])
                fw.dma("sp", gmm[:], g_mem, writes=[r_gmm])
                load_weight(wstg, wgate, r_wgate, w_in_r, OGATE, 2048, gm, r_gm)
                load_weight(wstg, wa, r_wa, w_rows(w_attn_branch), 0, 1024, None, None)
                load_weight(wstg, wo, r_wo, w_rows(w_out), 0, 1024, None, None)
                load_weight(wstg, wcq, r_wcq, w_rows(w_cross_q), 0, 1024, gc_, r_gc)
                load_weight(wstg, wco, r_wco, w_rows(w_cross_out), 0, 1024, None, None)
                xT, r_xT = P.sb([128, 8, 256], BF16, "xT3")
                xt_bufs = []
                xt, r_xt = P.sb([128, D], F32, "xt")
                xb, r_xb = P.sb([128, D], BF16, "xb")
                ss, r_ss = P.sb([128, 4], F32, "ss")
                xt_bufs = [(xt, r_xt, xb, r_xb, ss, r_ss)] * 2
                KcT, r_KcT = P.sb([128, 8, 256], BF16, "KcT")
                Vc, r_Vc = P.sb([128, 2, 1024], BF16, "Vc")
                with ExitStack() as es3a:
                    Pa = Pool(nc, es3a)
                    wckv, r_wckv = Pa.sb([128, 8, 2048], BF16, "wckv")
                    load_weight(wstg, wckv, r_wckv, w_rows(w_cross_kv), 0, 2048, gmm, r_gmm)
                    build_xT(P, 0, xT, r_xT, xt_bufs, ntile=2, src=lambda ti: mem[ti * 128:(ti + 1) * 128, :])
                    for blk in range(8):
                        bk, r_bk = banks[blk % 2]
                        for ch in range(8):
                            fw.op("pe", lambda e, ch=ch, blk=blk, bk=bk: e.matmul(bk[:, 0:256], lhsT=wckv[:, ch, blk * 128:(blk + 1) * 128], rhs=xT[:, ch, :], start=(ch == 0), stop=(ch == 7)),
                                  reads=[r_wckv, r_xT], writes=[r_bk], skip_self=True)
                        fw.op("act", lambda e, blk=blk, bk=bk: e.activation(out=KcT[:, blk, :], in_=bk[:, 0:256], func=ACT.Copy), reads=[r_bk], writes=[r_KcT])
                    for mc in range(2):
                        for cg in range(2):
                            bk, r_bk = banks[2 + cg]
                            for ch in range(8):
                                fw.op("pe", lambda e, ch=ch, mc=mc, cg=cg, bk=bk: e.matmul(bk[:], lhsT=xT[:, ch, mc * 128:(mc + 1) * 128], rhs=wckv[:, ch, 1024 + cg * 512:1024 + (cg + 1) * 512], start=(ch == 0), stop=(ch == 7)),
                                      reads=[r_wckv, r_xT], writes=[r_bk], skip_self=True)
                            fw.op("act", lambda e, mc=mc, cg=cg, bk=bk: e.activation(out=Vc[:, mc, cg * 512:(cg + 1) * 512], in_=bk[:], func=ACT.Copy), reads=[r_bk], writes=[r_Vc])
                    fw.barrier()
                gts, r_gts = P.sb([128, 2048], F32, "gts")
                atl, r_atl = P.sb([128, D], F32, "atl")
                spl, r_spl = P.sb([128, D], F32, "spl")
                bfb, r_bfb = P.sb([128, D], BF16, "bfb")
                tT, r_tT = P.sb([128, 8, 128], BF16, "tT")
                mg, r_mg = P.sb([128, D], F32, "mg")
                h1, r_h1 = P.sb([128, D], F32, "h1")
                sq, r_sq = P.sb([128, 8], F32, "sq")
                qcT, r_qcT = P.sb([128, 8, 128], BF16, "qcT")
                pc, r_pc = P.sb([128, 4, 256], F32, "pc")
                pcb, r_pcb = P.sb([128, 4, 256], BF16, "pcb")
                pT, r_pT = P.sb([128, 8, 128], BF16, "pT")
                oT, r_oT = P.sb([128, 8, 128], BF16, "oT")
                sm, r_sm = P.sb([128, 16], F32, "sm")
                junk, r_junk = P.sb([128, D], F32, "junk3")

                def to_T(src_bf, r_src, dstT, r_dst, bank_i):
                    bkt, r_bkt = banks[bank_i]
                    bktb = bkt[:].bitcast(BF16)
                    for ch in range(8):
                        fw.op("pe", lambda e, ch=ch: e.transpose(out=bktb[:, ch * 128:(ch + 1) * 128], in_=src_bf[:, ch * 128:(ch + 1) * 128], identity=ident_b),
                              reads=[r_src, r_cstb], writes=[r_bkt], skip_self=True)
                    fw.op("act", lambda e: e.activation(out=dstT[:], in_=bktb.rearrange("p (c t) -> p c t", c=8), func=ACT.Copy), reads=[r_bkt], writes=[r_dst])

                def rstd_of(src, r_src, col, n=D):
                    fw.op("act", lambda e: e.activation(out=junk[:], in_=src[:], func=ACT.Square, accum_out=sq[:, col:col + 1]), reads=[r_src], writes=[r_junk, r_sq])
                    fw.op("dve", lambda e: e.tensor_scalar(out=sq[:, col + 1:col + 2], in0=sq[:, col:col + 1], scalar1=1.0 / n, scalar2=EPS, op0=ALU.mult, op1=ALU.add), reads=[r_sq], writes=[r_sq])
                    fw.op("act", lambda e: e.activation(out=sq[:, col + 2:col + 3], in_=sq[:, col + 1:col + 2], func=ACT.Ln), reads=[r_sq], writes=[r_sq])
                    fw.op("act", lambda e: e.activation(out=sq[:, col + 3:col + 4], in_=sq[:, col + 2:col + 3], func=ACT.Exp, scale=-0.5), reads=[r_sq], writes=[r_sq])
                    return sq[:, col + 3:col + 4]

                for it in range(16):
                    seg, ti = 6 + it // 8, it % 8
                    t0 = it * 128
                    build_xT(P, seg, xT[:, :, 0:128], r_xT, xt_bufs, ntile=1, src=lambda _t, seg=seg, ti=ti: x_seg[seg, ti * 128:(ti + 1) * 128, :])
                    for cg in range(4):
                        bk, r_bk = banks[2 + (cg % 2)]
                        for ch in range(8):
                            fw.op("pe", lambda e, ch=ch, cg=cg, bk=bk: e.matmul(bk[:], lhsT=xT[:, ch, 0:128], rhs=wgate[:, ch, cg * 512:(cg + 1) * 512], start=(ch == 0), stop=(ch == 7)),
                                  reads=[r_xT, r_wgate], writes=[r_bk], skip_self=True)
                        fw.op("act", lambda e, cg=cg, bk=bk: e.activation(out=gts[:, cg * 512:(cg + 1) * 512], in_=bk[:], func=ACT.Sigmoid), reads=[r_bk], writes=[r_gts])
                    fw.dma("sp", atl[:], attn_d[t0:t0 + 128, :], writes=[r_atl])
                    fw.dma("sp", spl[:], sproj_d[t0:t0 + 128, :], writes=[r_spl])
                    fw.op("dve", lambda e: e.tensor_copy(out=bfb[:], in_=atl[:]), reads=[r_atl], writes=[r_bfb])
                    to_T(bfb, r_bfb, tT, r_tT, 4)
                    for cg in range(2):
                        bk, r_bk = banks[2 + cg]
                        for ch in range(8):
                            fw.op("pe", lambda e, ch=ch, cg=cg, bk=bk: e.matmul(bk[:], lhsT=tT[:, ch, :], rhs=wa[:, ch, cg * 512:(cg + 1) * 512], start=(ch == 0), stop=(ch == 7)),
                                  reads=[r_tT, r_wa], writes=[r_bk], skip_self=True)
                        fw.op("dve", lambda e, cg=cg, bk=bk: e.tensor_tensor(out=mg[:, cg * 512:(cg + 1) * 512], in0=bk[:], in1=gts[:, cg * 512:(cg + 1) * 512], op=ALU.mult),
                              reads=[r_bk, r_gts], writes=[r_mg])
                    fw.op("dve", lambda e: e.tensor_tensor(out=spl[:], in0=spl[:], in1=gts[:, 1024:2048], op=ALU.mult), reads=[r_spl, r_gts], writes=[r_spl])
                    fw.op("dve", lambda e: e.tensor_tensor(out=mg[:], in0=mg[:], in1=spl[:], op=ALU.add), reads=[r_mg, r_spl], writes=[r_mg])
                    fw.op("dve", lambda e: e.tensor_copy(out=bfb[:], in_=mg[:]), reads=[r_mg], writes=[r_bfb])
                    to_T(bfb, r_bfb, tT, r_tT, 5)
                    xt_cur, r_xt_cur = xt_bufs[0][0], xt_bufs[0][1]
                    for cg in range(2):
                        bk, r_bk = banks[2 + cg]
                        for ch in range(8):
                            fw.op("pe", lambda e, ch=ch, cg=cg, bk=bk: e.matmul(bk[:], lhsT=tT[:, ch, :], rhs=wo[:, ch, cg * 512:(cg + 1) * 512], start=(ch == 0), stop=(ch == 7)),
                                  reads=[r_tT, r_wo], writes=[r_bk], skip_self=True)
                        fw.op("dve", lambda e, cg=cg, bk=bk: e.tensor_tensor(out=h1[:, cg * 512:(cg + 1) * 512], in0=bk[:], in1=xt_cur[:, cg * 512:(cg + 1) * 512], op=ALU.add),
                              reads=[r_bk, r_xt_cur], writes=[r_h1])
                    rs = rstd_of(h1, r_h1, 0)
                    fw.op("dve", lambda e: e.tensor_scalar(out=bfb[:], in0=h1[:], scalar1=rs, scalar2=None, op0=ALU.mult), reads=[r_h1, r_sq], writes=[r_bfb])
                    to_T(bfb, r_bfb, tT, r_tT, 4)
                    for half in range(2):
                        bk, r_bk = banks[2 + half]
                        for b4 in range(4):
                            blk = half * 4 + b4
                            for ch in range(8):
                                fw.op("pe", lambda e, ch=ch, blk=blk, b4=b4, bk=bk: e.matmul(bk[:, b4 * 128:(b4 + 1) * 128], lhsT=wcq[:, ch, blk * 128:(blk + 1) * 128], rhs=tT[:, ch, :], start=(ch == 0), stop=(ch == 7)),
                                      reads=[r_wcq, r_tT], writes=[r_bk], skip_self=True)
                        fw.op("act", lambda e, half=half, bk=bk: e.activation(out=qcT[:, half * 4:(half + 1) * 4, :], in_=bk[:].rearrange("p (b t) -> p b t", b=4), func=ACT.Copy, scale=1.0 / 16.0),
                              reads=[r_bk], writes=[r_qcT])
                    for hp in range(2):
                        bk, r_bk = banks[2 + hp]
                        for j in range(2):
                            h = hp * 2 + j
                            for c in range(2):
                                fw.op("pe", lambda e, h=h, j=j, c=c, bk=bk: e.matmul(bk[:, j * 256:(j + 1) * 256], lhsT=qcT[:, 2 * h + c, :], rhs=KcT[:, 2 * h + c, :], start=(c == 0), stop=(c == 1)),
                                      reads=[r_qcT, r_KcT], writes=[r_bk], skip_self=True)
                        fw.op("act", lambda e, hp=hp, bk=bk: e.activation(out=pc[:, hp * 2:(hp + 1) * 2, :], in_=bk[:].rearrange("p (j m) -> p j m", j=2), func=ACT.Copy), reads=[r_bk], writes=[r_pc])
                    fw.op("dve", lambda e: e.tensor_reduce(out=sm[:, 0:4], in_=pc[:], axis=AX.X, op=ALU.max), reads=[r_pc], writes=[r_sm])
                    fw.op("dve", lambda e: e.tensor_scalar(out=sm[:, 4:8], in0=sm[:, 0:4], scalar1=-1.0, scalar2=None, op0=ALU.mult), reads=[r_sm], writes=[r_sm])
                    for h in range(4):
                        fw.op("act", lambda e, h=h: e.activation(out=pc[:, h, :], in_=pc[:, h, :], func=ACT.Exp, bias=sm[:, 4 + h:5 + h], accum_out=sm[:, 8 + h:9 + h]), reads=[r_pc, r_sm], writes=[r_pc, r_sm])
                    fw.op("dve", lambda e: e.reciprocal(out=sm[:, 12:16], in_=sm[:, 8:12]), reads=[r_sm], writes=[r_sm])
                    fw.op("dve", lambda e: e.tensor_tensor(out=pcb[:], in0=pc[:], in1=sm[:, 12:16].unsqueeze(2).to_broadcast([128, 4, 256]), op=ALU.mult), reads=[r_pc, r_sm], writes=[r_pcb])
                    bkt, r_bkt = banks[4]
                    bktb = bkt[:].bitcast(BF16)
                    for h in range(4):
                        for mc in range(2):
                            fw.op("pe", lambda e, h=h, mc=mc: e.transpose(out=bktb[:, (2 * h + mc) * 128:(2 * h + mc + 1) * 128], in_=pcb[:, h, mc * 128:(mc + 1) * 128], identity=ident_b),
                                  reads=[r_pcb, r_cstb], writes=[r_bkt], skip_self=True)
                    fw.op("act", lambda e: e.activation(out=pT[:], in_=bktb.rearrange("p (c t) -> p c t", c=8), func=ACT.Copy), reads=[r_bkt], writes=[r_pT])
                    for half in range(2):
                        bk, r_bk = banks[2 + half]
                        for b4 in range(4):
                            blk = half * 4 + b4
                            h, dc = blk // 2, blk % 2
                            for mc in range(2):
                                fw.op("pe", lambda e, h=h, dc=dc, mc=mc, b4=b4, bk=bk: e.matmul(bk[:, b4 * 128:(b4 + 1) * 128], lhsT=Vc[:, mc, h * 256 + dc * 128:h * 256 + (dc + 1) * 128], rhs=pT[:, 2 * h + mc, :],
                                                                                        start=(mc == 0), stop=(mc == 1)),
                                      reads=[r_Vc, r_pT], writes=[r_bk], skip_self=True)
                        fw.op("act", lambda e, half=half, bk=bk: e.activation(out=oT[:, half * 4:(half + 1) * 4, :], in_=bk[:].rearrange("p (b t) -> p b t", b=4), func=ACT.Copy), reads=[r_bk], writes=[r_oT])
                    for cg in range(2):
                        bk, r_bk = banks[2 + cg]
                        for ch in range(8):
                            fw.op("pe", lambda e, ch=ch, cg=cg, bk=bk: e.matmul(bk[:], lhsT=oT[:, ch, :], rhs=wco[:, ch, cg * 512:(cg + 1) * 512], start=(ch == 0), stop=(ch == 7)),
                                  reads=[r_oT, r_wco], writes=[r_bk], skip_self=True)
                        fw.op("dve", lambda e, cg=cg, bk=bk: e.tensor_tensor(out=mg[:, cg * 512:(cg + 1) * 512], in0=bk[:], in1=h1[:, cg * 512:(cg + 1) * 512], op=ALU.add),
                              reads=[r_bk, r_h1], writes=[r_mg])
                    fw.dma("sp", h2_d[t0:t0 + 128, :], mg[:], reads=[r_mg])
                fw.barrier()

            with ExitStack() as es4:
                P = Pool(nc, es4)
                wstg = P.sb([128, 4096], F32, "wstg")
                wpq, r_wpq = P.sb([128, 8, 2048], BF16, "wpq")
                gfc, r_gfc = P.sb([128, 8], F32, "gfc")
                fw.dma("sp", gfc[:], g_ffn_c, writes=[r_gfc])
                load_weight(wstg, wpq, r_wpq, w_rows(w_peer_q), 0, 2048, gfc, r_gfc)
                gff, r_gff = P.sb([128, D], F32, "gff")
                gfin, r_gfin = P.sb([128, D], F32, "gfin")
                fw.dma("sp", gff[:], g_ffn, writes=[r_gff])
                fw.dma("sp", gfin[:], g_final, writes=[r_gfin])
                skT, r_skT = P.sb([128, 16, 128], BF16, "skT")
                skf, r_skf = P.sb([128, 16, 128], F32, "skf")
                skb, r_skb = P.sb([128, 16, 128], BF16, "skb")
                fw.dma("sp", skf[:], sub_keys.rearrange("b k d -> k b d"), writes=[r_skf])
                fw.op("dve", lambda e: e.tensor_copy(out=skb[:], in_=skf[:]), reads=[r_skf], writes=[r_skb])
                for half in range(2):
                    bkt, r_bkt = banks[half]
                    bktb = bkt[:].bitcast(BF16)
                    for i in range(8):
                        fw.op("pe", lambda e, i=i, half=half, bktb=bktb: e.transpose(out=bktb[:, i * 128:(i + 1) * 128], in_=skb[:, half * 8 + i, :], identity=ident_b),
                              reads=[r_skb, r_cstb], writes=[r_bkt], skip_self=True)
                    fw.op("act", lambda e, half=half, bktb=bktb: e.activation(out=skT[:, half * 8:(half + 1) * 8, :], in_=bktb.rearrange("p (c t) -> p c t", c=8), func=ACT.Copy), reads=[r_bkt], writes=[r_skT])
                iota16, r_iota = P.sb([128, 16], F32, "iota16")
                for i in range(16):
                    fw.op("pool", lambda e, i=i: e.memset(iota16[:, i:i + 1], float(i)), writes=[r_iota])
                h2, r_h2 = P.sb([128, D], F32, "h2")
                a3, r_a3 = P.sb([128, D], F32, "a3")
                bfb, r_bfb = P.sb([128, D], BF16, "bfb4")
                tT, r_tT = P.sb([128, 8, 128], BF16, "tT4")
                qpT, r_qpT = P.sb([128, 16, 128], BF16, "qpT")
                sc, r_sc = P.sb([128, 16, 128], F32, "sc")
                scw, r_scw = P.sb([128, 16, 128], F32, "scw")
                tops, r_tops = P.sb([128, 16, 16], F32, "tops")
                topi, r_topi = P.sb([128, 16, 16], U32, "topi")
                topf, r_topf = P.sb([128, 16, 16], F32, "topf")
                cand, r_cand = P.sb([128, 8, 256], F32, "cand")
                candw, r_candw = P.sb([128, 8, 256], F32, "candw")
                selv, r_selv = P.sb([128, 8, 16], F32, "selv")
                seli, r_seli = P.sb([128, 8, 16], U32, "seli")
                selt, r_selt = P.sb([128, 8, 16], U32, "selt")
                sif, r_sif = P.sb([128, 8, 16], F32, "sif")
                sjf, r_sjf = P.sb([128, 8, 16], F32, "sjf")
                oh, r_oh = P.sb([128, 8, 16, 16], F32, "oh")
                eaf, r_eaf = P.sb([128, 8, 16], F32, "eaf")
                ebf, r_ebf = P.sb([128, 8, 16], F32, "ebf")
                eidx, r_eidx = P.sb([128, 128], I32, "eidx")
                gw, r_gw = P.sb([128, 8, 16], F32, "gw")
                g8, r_g8 = P.sb([128, 16], F32, "g8")
                dots, r_dots = P.sb([128, 128], F32, "dots")
                wts, r_wts = P.sb([128, 128], F32, "wts")
                Ub = [P.sb([128, D], F32, "Ub") for _ in range(8)]
                prod, r_prod = P.sb([128, D], F32, "prod")
                acc, r_acc = P.sb([128, D], F32, "acc")
                sq, r_sq = P.sb([128, 8], F32, "sq4")
                junk, r_junk = P.sb([128, D], F32, "junk4")
                for it in range(16):
                    t0 = it * 128
                    fw.dma("sp", h2[:], h2_d[t0:t0 + 128, :], writes=[r_h2])
                    fw.op("act", lambda e: e.activation(out=junk[:], in_=h2[:], func=ACT.Square, accum_out=sq[:, 0:1]), reads=[r_h2], writes=[r_junk, r_sq])
                    fw.op("dve", lambda e: e.tensor_scalar(out=sq[:, 1:2], in0=sq[:, 0:1], scalar1=1.0 / D, scalar2=EPS, op0=ALU.mult, op1=ALU.add), reads=[r_sq], writes=[r_sq])
                    fw.op("act", lambda e: e.activation(out=sq[:, 2:3], in_=sq[:, 1:2], func=ACT.Ln), reads=[r_sq], writes=[r_sq])
                    fw.op("act", lambda e: e.activation(out=sq[:, 3:4], in_=sq[:, 2:3], func=ACT.Exp, scale=-0.5), reads=[r_sq], writes=[r_sq])
                    fw.op("dve", lambda e: e.tensor_scalar(out=bfb[:], in0=h2[:], scalar1=sq[:, 3:4], scalar2=None, op0=ALU.mult), reads=[r_h2, r_sq], writes=[r_bfb])
                    fw.op("dve", lambda e: e.scalar_tensor_tensor(out=a3[:], in0=h2[:], scalar=sq[:, 3:4], in1=gff[:], op0=ALU.mult, op1=ALU.mult), reads=[r_h2, r_sq, r_gff], writes=[r_a3])
                    bkt, r_bkt = banks[0]
                    bktb = bkt[:].bitcast(BF16)
                    for ch in range(8):
                        fw.op("pe", lambda e, ch=ch: e.transpose(out=bktb[:, ch * 128:(ch + 1) * 128], in_=bfb[:, ch * 128:(ch + 1) * 128], identity=ident_b),
                              reads=[r_bfb, r_cstb], writes=[r_bkt], skip_self=True)
                    fw.op("act", lambda e: e.activation(out=tT[:], in_=bktb.rearrange("p (c t) -> p c t", c=8), func=ACT.Copy), reads=[r_bkt], writes=[r_tT])
                    for q4 in range(4):
                        bk, r_bk = banks[1 + (q4 % 2)]
                        for b4 in range(4):
                            blk = q4 * 4 + b4
                            for ch in range(8):
                                fw.op("pe", lambda e, ch=ch, blk=blk, b4=b4, bk=bk: e.matmul(bk[:, b4 * 128:(b4 + 1) * 128], lhsT=wpq[:, ch, blk * 128:(blk + 1) * 128], rhs=tT[:, ch, :], start=(ch == 0), stop=(ch == 7)),
                                      reads=[r_wpq, r_tT], writes=[r_bk], skip_self=True)
                        fw.op("act", lambda e, q4=q4, bk=bk: e.activation(out=qpT[:, q4 * 4:(q4 + 1) * 4, :], in_=bk[:].rearrange("p (b t) -> p b t", b=4), func=ACT.Copy), reads=[r_bk], writes=[r_qpT])
                    for q4 in range(4):
                        bk, r_bk = banks[3 + (q4 % 2)]
                        for b4 in range(4):
                            blk = q4 * 4 + b4
                            fw.op("pe", lambda e, blk=blk, b4=b4, bk=bk: e.matmul(bk[:, b4 * 128:(b4 + 1) * 128], lhsT=qpT[:, blk, :], rhs=skT[:, blk, :], start=True, stop=True),
                                  reads=[r_qpT, r_skT], writes=[r_bk], skip_self=True)
                        fw.op("act", lambda e, q4=q4, bk=bk: e.activation(out=sc[:, q4 * 4:(q4 + 1) * 4, :], in_=bk[:].rearrange("p (b t) -> p b t", b=4), func=ACT.Copy), reads=[r_bk], writes=[r_sc])
                    for blk in range(16):
                        fw.op("dve", lambda e, blk=blk: e.max(out=tops[:, blk, 0:8], in_=sc[:, blk, :]), reads=[r_sc], writes=[r_tops])
                        fw.op("dve", lambda e, blk=blk: e.max_index(out=topi[:, blk, 0:8], in_max=tops[:, blk, 0:8], in_values=sc[:, blk, :]), reads=[r_sc, r_tops], writes=[r_topi])
                        fw.op("dve", lambda e, blk=blk: e.match_replace(out=scw[:, blk, :], in_to_replace=tops[:, blk, 0:8], in_values=sc[:, blk, :], imm_value=NEG), reads=[r_sc, r_tops], writes=[r_scw])
                        fw.op("dve", lambda e, blk=blk: e.max(out=tops[:, blk, 8:16], in_=scw[:, blk, :]), reads=[r_scw], writes=[r_tops])
                        fw.op("dve", lambda e, blk=blk: e.max_index(out=topi[:, blk, 8:16], in_max=tops[:, blk, 8:16], in_values=scw[:, blk, :]), reads=[r_scw, r_tops], writes=[r_topi])
                    fw.op("dve", lambda e: e.tensor_copy(out=topf[:], in_=topi[:]), reads=[r_topi], writes=[r_topf])
                    tv = tops[:].rearrange("p (h q) k -> p h q k", q=2)
                    fw.op("dve", lambda e: e.tensor_tensor(out=cand[:].rearrange("p h (i j) -> p h i j", j=16), in0=tv[:, :, 0, :].unsqueeze(3).to_broadcast([128, 8, 16, 16]),
                                                           in1=tv[:, :, 1, :].unsqueeze(2).to_broadcast([128, 8, 16, 16]), op=ALU.add), reads=[r_tops], writes=[r_cand])
                    for h in range(8):
                        fw.op("dve", lambda e, h=h: e.max(out=selv[:, h, 0:8], in_=cand[:, h, :]), reads=[r_cand], writes=[r_selv])
                        fw.op("dve", lambda e, h=h: e.max_index(out=seli[:, h, 0:8], in_max=selv[:, h, 0:8], in_values=cand[:, h, :]), reads=[r_cand, r_selv], writes=[r_seli])
                        fw.op("dve", lambda e, h=h: e.match_replace(out=candw[:, h, :], in_to_replace=selv[:, h, 0:8], in_values=cand[:, h, :], imm_value=NEG), reads=[r_cand, r_selv], writes=[r_candw])
                        fw.op("dve", lambda e, h=h: e.max(out=selv[:, h, 8:16], in_=candw[:, h, :]), reads=[r_candw], writes=[r_selv])
                        fw.op("dve", lambda e, h=h: e.max_index(out=seli[:, h, 8:16], in_max=selv[:, h, 8:16], in_values=candw[:, h, :]), reads=[r_candw, r_selv], writes=[r_seli])
                    fw.op("dve", lambda e: e.tensor_single_scalar(out=selt[:], in_=seli[:], scalar=4, op=ALU.logical_shift_right), reads=[r_seli], writes=[r_selt])
                    fw.op("dve", lambda e: e.tensor_copy(out=sif[:], in_=selt[:]), reads=[r_selt], writes=[r_sif])
                    fw.op("dve", lambda e: e.tensor_single_scalar(out=selt[:], in_=seli[:], scalar=15, op=ALU.bitwise_and), reads=[r_seli], writes=[r_selt])
                    fw.op("dve", lambda e: e.tensor_copy(out=sjf[:], in_=selt[:]), reads=[r_selt], writes=[r_sjf])
                    tf = topf[:].rearrange("p (h q) k -> p h q k", q=2)
                    for (srcf, r_srcf, q, dst, r_dst) in ((sif, r_sif, 0, eaf, r_eaf), (sjf, r_sjf, 1, ebf, r_ebf)):
                        fw.op("dve", lambda e, srcf=srcf: e.tensor_tensor(out=oh[:], in0=srcf[:].unsqueeze(3).to_broadcast([128, 8, 16, 16]),
                                                                         in1=iota16[:].unsqueeze(1).unsqueeze(2).to_broadcast([128, 8, 16, 16]), op=ALU.is_equal),
                              reads=[r_srcf, r_iota], writes=[r_oh])
                        fw.op("dve", lambda e, q=q: e.tensor_tensor(out=oh[:], in0=oh[:], in1=tf[:, :, q, :].unsqueeze(2).to_broadcast([128, 8, 16, 16]), op=ALU.mult),
                              reads=[r_oh, r_topf], writes=[r_oh])
                        fw.op("dve", lambda e, dst=dst: e.tensor_reduce(out=dst[:], in_=oh[:], axis=AX.X, op=ALU.add), reads=[r_oh], writes=[r_dst])
                    fw.op("dve", lambda e: e.scalar_tensor_tensor(out=eaf[:], in0=eaf[:], scalar=128.0, in1=ebf[:], op0=ALU.mult, op1=ALU.add), reads=[r_eaf, r_ebf], writes=[r_eaf])
                    fw.op("dve", lambda e: e.tensor_copy(out=eidx[:], in_=eaf[:].rearrange("p h k -> p (h k)")), reads=[r_eaf], writes=[r_eidx])
                    fw.op("dve", lambda e: e.tensor_tensor(out=gw[:], in0=selv[:], in1=selv[:, :, 0:1].to_broadcast([128, 8, 16]), op=ALU.subtract), reads=[r_selv], writes=[r_gw])
                    fw.op("act", lambda e: e.activation(out=gw[:], in_=gw[:], func=ACT.Exp), reads=[r_gw], writes=[r_gw])
                    fw.op("dve", lambda e: e.tensor_reduce(out=g8[:, 0:8], in_=gw[:], axis=AX.X, op=ALU.add), reads=[r_gw], writes=[r_g8])
                    fw.op("dve", lambda e: e.reciprocal(out=g8[:, 8:16], in_=g8[:, 0:8]), reads=[r_g8], writes=[r_g8])
                    fw.op("dve", lambda e: e.tensor_tensor(out=gw[:], in0=gw[:], in1=g8[:, 8:16].unsqueeze(2).to_broadcast([128, 8, 16]), op=ALU.mult), reads=[r_gw, r_g8], writes=[r_gw])
                    for sl in range(128):
                        ub, r_ub = Ub[sl % 8]
                        fw.dma("pool", ub[:], peer_u, reads=[r_eidx], writes=[r_ub], indirect=bass.IndirectOffsetOnAxis(ap=eidx[:, sl:sl + 1], axis=0))
                        fw.op("dve", lambda e, ub=ub: e.tensor_tensor(out=prod[:], in0=ub[:], in1=a3[:], op=ALU.mult), reads=[r_ub, r_a3], writes=[r_prod])
                        fw.op("act", lambda e, sl=sl: e.activation(out=junk[:], in_=prod[:], func=ACT.Copy, accum_out=dots[:, sl:sl + 1]), reads=[r_prod], writes=[r_junk, r_dots])
                    fw.op("act", lambda e: e.activation(out=wts[:], in_=dots[:], func=ACT.Gelu), reads=[r_dots], writes=[r_wts])
                    fw.op("dve", lambda e: e.tensor_tensor(out=wts[:], in0=wts[:], in1=gw[:].rearrange("p h k -> p (h k)"), op=ALU.mult), reads=[r_wts, r_gw], writes=[r_wts])
                    for sl in range(128):
                        ub, r_ub = Ub[sl % 8]
                        fw.dma("pool", ub[:], peer_v, reads=[r_eidx], writes=[r_ub], indirect=bass.IndirectOffsetOnAxis(ap=eidx[:, sl:sl + 1], axis=0))
                        if sl == 0:
                            fw.op("dve", lambda e, ub=ub: e.tensor_scalar(out=acc[:], in0=ub[:], scalar1=wts[:, 0:1], scalar2=None, op0=ALU.mult), reads=[r_ub, r_wts], writes=[r_acc])
                        else:
                            fw.op("dve", lambda e, ub=ub, sl=sl: e.scalar_tensor_tensor(out=acc[:], in0=ub[:], scalar=wts[:, sl:sl + 1], in1=acc[:], op0=ALU.mult, op1=ALU.add),
                                  reads=[r_ub, r_wts, r_acc], writes=[r_acc])
                    fw.op("dve", lambda e: e.tensor_tensor(out=acc[:], in0=acc[:], in1=h2[:], op=ALU.add), reads=[r_acc, r_h2], writes=[r_acc])
                    fw.op("act", lambda e: e.activation(out=junk[:], in_=acc[:], func=ACT.Square, accum_out=sq[:, 4:5]), reads=[r_acc], writes=[r_junk, r_sq])
                    fw.op("dve", lambda e: e.tensor_scalar(out=sq[:, 5:6], in0=sq[:, 4:5], scalar1=1.0 / D, scalar2=EPS, op0=ALU.mult, op1=ALU.add), reads=[r_sq], writes=[r_sq])
                    fw.op("act", lambda e: e.activation(out=sq[:, 6:7], in_=sq[:, 5:6], func=ACT.Ln), reads=[r_sq], writes=[r_sq])
                    fw.op("act", lambda e: e.activation(out=sq[:, 7:8], in_=sq[:, 6:7], func=ACT.Exp, scale=-0.5), reads=[r_sq], writes=[r_sq])
                    fw.op("dve", lambda e: e.scalar_tensor_tensor(out=prod[:], in0=acc[:], scalar=sq[:, 7:8], in1=gfin[:], op0=ALU.mult, op1=ALU.mult), reads=[r_acc, r_sq, r_gfin], writes=[r_prod])
                    fw.dma("sp", y_out[t0:t0 + 128, :], prod[:], reads=[r_prod])
                fw.barrier()

        if 3 not in phases:
            zt, r_zt = P0.sb([128, D], F32, "zt")
            fw.op("pool", lambda e: e.memset(zt[:], 0.0), writes=[r_zt])
            for ti in range(16):
                fw.dma("sp", y_out[ti * 128:(ti + 1) * 128, :], zt[:], reads=[r_zt])
        fw.barrier()
    return nc


def _prep_inputs(inputs):
    f32 = np.float32
    x = np.asarray(inputs["x"], f32)
    pos = np.asarray(inputs["positions"])
    ident = np.eye(128, dtype=f32)
    k_ = np.arange(128)
    tri = (k_[:, None] <= k_[None, :]).astype(f32)
    ones = np.ones((128, 128), f32)
    tri_pen = np.where(k_[None, :] <= k_[:, None], 0.0, NEG).astype(f32)
    consts = np.concatenate([ident, tri, ones, tri_pen, np.zeros((128, 128), f32)], axis=1)
    inv16 = (500000.0 ** (-2.0 * np.arange(16, dtype=f32) / 32)).astype(f32)
    inv8 = (500000.0 ** (-2.0 * np.arange(8, dtype=f32) / 16)).astype(f32)
    invf = np.tile(np.concatenate([inv16, inv8])[None, :], (128, 1)).astype(f32)

    def pc(v, n):
        return np.ascontiguousarray(np.asarray(v, f32).reshape(n, 128).T)

    def bc(v):
        return np.ascontiguousarray(np.tile(np.asarray(v, f32).reshape(1, -1), (128, 1)))

    cw = np.asarray(inputs["conv_w"], f32)[0, :, 0, :]
    convw = np.ascontiguousarray(cw.reshape(4, 24, 128).transpose(2, 1, 0).reshape(128, 96))
    shared = {
        "consts": consts, "invf": invf,
        "w_in": np.asarray(inputs["w_in"], f32)[0],
        "g_mix": pc(inputs["norm_mix_g"][0], 8),
        "convw": convw, "convb": pc(inputs["conv_b"][0], 24),
        "hp3": np.concatenate([bc(inputs["dt_bias"][0]), bc(inputs["a_log"][0]), bc(inputs["d_skip"][0])], axis=1),
        "g_ssd": pc(inputs["ssd_norm_g"][0], 16),
        "w_attn_branch": np.asarray(inputs["w_attn_branch"], f32)[0],
        "w_ssd_branch": np.asarray(inputs["w_ssd_branch"], f32)[0],
        "w_out": np.asarray(inputs["w_out"], f32)[0],
        "g_cross": pc(inputs["norm_cross_g"][0], 8), "g_mem": pc(inputs["norm_mem_g"][0], 8),
        "w_cross_q": np.asarray(inputs["w_cross_q"], f32)[0],
        "w_cross_kv": np.asarray(inputs["w_cross_kv"], f32)[0],
        "w_cross_out": np.asarray(inputs["w_cross_out"], f32)[0],
        "g_ffn": bc(inputs["norm_ffn_g"][0]), "g_ffn_c": pc(inputs["norm_ffn_g"][0], 8),
        "w_peer_q": np.asarray(inputs["w_peer_q"], f32)[0],
        "sub_keys": np.ascontiguousarray(np.asarray(inputs["peer_sub_keys"], f32)[0].reshape(16, 128, 128)),
        "peer_u": np.asarray(inputs["peer_u"], f32)[0],
        "peer_v": np.asarray(inputs["peer_v"], f32)[0],
        "g_final": bc(inputs["norm_final_g"]),
    }
    maps = []
    for c in range(NCORE):
        b, j = c // 4, c % 4
        xs = np.zeros((NSEG, SEGT, D), f32)
        ps = np.zeros((NSEG, SEGT), f32)
        vs = np.zeros((NSEG, SEGT), f32)
        pen = np.full((6,), NEG, f32)
        for s in range(NSEG):
            t0 = 2048 * j - NCTX + s * SEGT
            if t0 >= 0:
                xs[s] = x[b, t0:t0 + SEGT]
                ps[s] = pos[b, t0:t0 + SEGT].astype(f32)
                vs[s] = 1.0
                if s < 6:
                    pen[s] = 0.0
        m = dict(shared)
        m["x_seg"] = xs
        m["pos_seg"] = np.ascontiguousarray(ps.reshape(NSEG * 8, 128).T)
        m["valid_seg"] = np.ascontiguousarray(vs.reshape(NSEG * 8, 128).T)
        m["pen_ctx"] = np.ascontiguousarray(np.tile(pen[None, :], (128, 1)))
        m["mem"] = np.asarray(inputs["mem"], f32)[b]
        maps.append(m)
    return maps


def kernel(**inputs):
    nc = build()
    maps = _prep_inputs(inputs)
    res = run_bass_kernel_spmd(nc, maps, core_ids=list(range(NCORE)))
    out = np.zeros((2, 8192, D), np.float32)
    for c in range(NCORE):
        b, j = c // 4, c % 4
        out[b, 2048 * j:2048 * (j + 1)] = res.results[c]["y_out"]
    return out
```
